# Optimizing a Trainium2 kernel written in Bass

```python
import jax
import jax.numpy as jnp
from jax import lax
import numpy as np

D_MODEL = 1024
BATCH = 8
SEQ = 8192
DEPTH = 1

GRID_W = 64
CTX_LEN = 256
D_MIX = D_MODEL
NA_HEADS = 8
NA_HEAD_DIM = 64
NA_WIN_H = 8
NA_WIN_W = 16
NA_W = NA_HEADS * NA_HEAD_DIM
ML_HEADS = 4
ML_QK_DIM = 64
ML_V_DIM = 128
ML_QK_W = ML_HEADS * ML_QK_DIM
ML_V_W = ML_HEADS * ML_V_DIM
ML_CHUNK = 128
ML_N_GATES = 4 * ML_HEADS
CONV_K = 5
ROPE_BASE = 10000.0
N_EXPERTS = 16
CAPACITY_FACTOR = 2
D_FF = 2816
LN_EPS = 1e-5
DEEPNORM_ALPHA = (2.0 * DEPTH) ** 0.25
DEEPNORM_BETA = (8.0 * DEPTH) ** -0.25
SPLITS = (NA_W, 2 * NA_W, 3 * NA_W, 3 * NA_W + ML_QK_W, 3 * NA_W + 2 * ML_QK_W, 3 * NA_W + 2 * ML_QK_W + ML_V_W, 3 * NA_W + 2 * ML_QK_W + 2 * ML_V_W)
D_IN = 3 * NA_W + 2 * ML_QK_W + 2 * ML_V_W + ML_N_GATES

kernel_name = "hybrid_natten_mlstm_ec_moe_dit_layer"


def _layer_norm(x, g, b):
    xf = x.astype(jnp.float32)
    mu = xf.mean(-1, keepdims=True)
    var = jnp.mean(jnp.square(xf - mu), -1, keepdims=True)
    return ((xf - mu) * lax.rsqrt(var + LN_EPS)).astype(x.dtype) * g + b


def _ada(cvec, w_ada, b_ada):
    m = (jax.nn.silu(cvec) @ w_ada + b_ada)[..., None, :]
    return jnp.split(m, 6, axis=-1)


def _dwconv(x, w):
    k = w.shape[0]
    return lax.conv_general_dilated(x, w[:, None, :], window_strides=(1,), padding=[(k // 2, k // 2)],
                                    dimension_numbers=('NWC', 'WIO', 'NWC'), feature_group_count=x.shape[-1])


def _rope_2d(x, rows, cols):
    dh = x.shape[-1]
    half = dh // 2
    nf = half // 2
    inv = 1.0 / (ROPE_BASE ** (jnp.arange(nf, dtype=jnp.float32) / nf))

    def rot(xa, pos):
        ang = pos.astype(jnp.float32)[:, None] * inv
        cos = jnp.cos(ang)[None, :, None, :].astype(x.dtype)
        sin = jnp.sin(ang)[None, :, None, :].astype(x.dtype)
        x1, x2 = xa[..., :nf], xa[..., nf:]
        return jnp.concatenate([x1 * cos - x2 * sin, x1 * sin + x2 * cos], axis=-1)

    return jnp.concatenate([rot(x[..., :half], rows), rot(x[..., half:], cols)], axis=-1)


def _neighbourhood_attention(q, k, v, k_ctx, v_ctx, bias_table):
    B, T, H, Dh = q.shape
    rows = T // GRID_W
    kh = min(NA_WIN_H, rows)
    qg = (q * Dh ** -0.5).reshape(B, rows, GRID_W, H, Dh)
    kg = k.reshape(B, rows, GRID_W, H, Dh)
    vg = v.reshape(B, rows, GRID_W, H, Dh)
    col_start = np.clip(np.arange(GRID_W) - NA_WIN_W // 2, 0, GRID_W - NA_WIN_W)
    col_idx = col_start[:, None] + np.arange(NA_WIN_W)
    col_off = col_idx - np.arange(GRID_W)[:, None] + (NA_WIN_W - 1)
    bias_cols = bias_table[:, :, col_off]

    def row_block(r):
        rs = jnp.clip(r - kh // 2, 0, rows - kh)
        k_win = lax.dynamic_slice_in_dim(kg, rs, kh, axis=1)[:, :, col_idx]
        v_win = lax.dynamic_slice_in_dim(vg, rs, kh, axis=1)[:, :, col_idx]
        q_r = lax.dynamic_index_in_dim(qg, r, axis=1, keepdims=False)
        row_off = rs + jnp.arange(kh) - r + (NA_WIN_H - 1)
        bias = jnp.transpose(jnp.take(bias_cols, row_off, axis=1), (0, 2, 1, 3))
        s_lat = jnp.einsum('bchd,bicjhd->bhcij', q_r, k_win).astype(jnp.float32) + bias[None].astype(jnp.float32)
        s_ctx = jnp.einsum('bchd,blhd->bhcl', q_r, k_ctx).astype(jnp.float32)
        s = jnp.concatenate([s_lat.reshape(B, H, GRID_W, kh * NA_WIN_W), s_ctx], axis=-1)
        p = jax.nn.softmax(s, axis=-1).astype(v.dtype)
        p_lat = p[..., :kh * NA_WIN_W].reshape(B, H, GRID_W, kh, NA_WIN_W)
        p_ctx = p[..., kh * NA_WIN_W:]
        return (jnp.einsum('bhcij,bicjhd->bchd', p_lat, v_win)
                + jnp.einsum('bhcl,blhd->bchd', p_ctx, v_ctx))

    out = lax.map(row_block, jnp.arange(rows))
    return jnp.moveaxis(out, 0, 1).reshape(B, T, H * Dh)


def _context_attention(q, k, v):
    s = jnp.einsum('bqhd,bkhd->bhqk', q * q.shape[-1] ** -0.5, k).astype(jnp.float32)
    p = jax.nn.softmax(s, axis=-1).astype(v.dtype)
    o = jnp.einsum('bhqk,bkhd->bqhd', p, v)
    return o.reshape(o.shape[0], o.shape[1], -1)


def _mlstm_chunkwise(q, k, v, i_pre, logf, state):
    B, H, T, dk = q.shape
    dv = v.shape[-1]
    L = ML_CHUNK
    nc = T // L
    qc = q.reshape(B, H, nc, L, dk)
    kc = k.reshape(B, H, nc, L, dk)
    vc = v.reshape(B, H, nc, L, dv)
    ic = i_pre.reshape(B, H, nc, L)
    bc = jnp.cumsum(logf.reshape(B, H, nc, L), axis=-1)
    b_last = bc[..., -1]
    a = b_last[..., None] - bc + ic
    m_loc = a.max(-1)
    w = jnp.exp(a - m_loc[..., None])
    c_loc = jnp.einsum('bhnl,bhnlv,bhnlk->bhnvk', w, vc, kc)
    n_loc = jnp.einsum('bhnl,bhnlk->bhnk', w, kc)

    def step(carry, inp):
        c_prev, n_prev, m_prev = carry
        bl, ml, cl, nl = inp
        m_new = jnp.maximum(bl + m_prev, ml)
        s_prev = jnp.exp(bl + m_prev - m_new)
        s_loc = jnp.exp(ml - m_new)
        c_new = s_prev[..., None, None] * c_prev + s_loc[..., None, None] * cl
        n_new = s_prev[..., None] * n_prev + s_loc[..., None] * nl
        return (c_new, n_new, m_new), (c_prev, n_prev, m_prev)

    xs = (jnp.moveaxis(b_last, 2, 0), jnp.moveaxis(m_loc, 2, 0), jnp.moveaxis(c_loc, 2, 0), jnp.moveaxis(n_loc, 2, 0))
    final, (c_in, n_in, m_in) = lax.scan(step, state, xs)
    c_in = jnp.moveaxis(c_in, 0, 2)
    n_in = jnp.moveaxis(n_in, 0, 2)
    m_in = jnp.moveaxis(m_in, 0, 2)

    lower = np.tril(np.ones((L, L), dtype=bool))
    dmat = jnp.where(lower, bc[..., :, None] - bc[..., None, :] + ic[..., None, :], -jnp.inf)
    m_prev_term = bc + m_in[..., None]
    m_t = jnp.maximum(dmat.max(-1), m_prev_term)
    s = jnp.einsum('bhntk,bhnsk->bhnts', qc, kc) * jnp.exp(dmat - m_t[..., None])
    inter = jnp.exp(m_prev_term - m_t)
    num = (jnp.einsum('bhnts,bhnsv->bhntv', s, vc)
           + inter[..., None] * jnp.einsum('bhnvk,bhntk->bhntv', c_in, qc))
    den = s.sum(-1) + inter * jnp.einsum('bhnk,bhntk->bhnt', n_in, qc)
    h = num / jnp.maximum(jnp.abs(den), jnp.exp(-m_t))[..., None]
    return h.reshape(B, H, T, dv), final


def _mlstm_inputs(qm, km, vm, gates, b_gate, conv_w, pos):
    B, T, _ = qm.shape
    qk = jax.nn.silu(_dwconv(jnp.concatenate([qm, km], axis=-1), conv_w))
    q = qk[..., :ML_QK_W].reshape(B, T, ML_HEADS, ML_QK_DIM)
    k = qk[..., ML_QK_W:].reshape(B, T, ML_HEADS, ML_QK_DIM)
    if pos is not None:
        q = _rope_2d(q, pos[0], pos[1])
        k = _rope_2d(k, pos[0], pos[1])
    v = vm.reshape(B, T, ML_HEADS, ML_V_DIM)
    g = (gates + b_gate).astype(jnp.float32).reshape(B, T, 4, ML_HEADS)
    g = jnp.transpose(g, (2, 0, 3, 1))
    tr = lambda a: jnp.transpose(a, (0, 2, 1, 3))
    return tr(q) * ML_QK_DIM ** -0.5, tr(k), tr(v), g


def _mlstm_bidirectional(lat, ctx_in):
    q, k, v, g = lat
    qc, kc, vc, gc = ctx_in
    B, H = q.shape[:2]
    zero = (jnp.zeros((B, H, ML_V_DIM, ML_QK_DIM), jnp.float32), jnp.zeros((B, H, ML_QK_DIM), jnp.float32),
            jnp.zeros((B, H), jnp.float32))
    flip = lambda a: jnp.flip(a, axis=2)
    hc_f, st_f = _mlstm_chunkwise(qc, kc, vc, gc[0], jax.nn.log_sigmoid(gc[1]), zero)
    h_f, _ = _mlstm_chunkwise(q, k, v, g[0], jax.nn.log_sigmoid(g[1]), st_f)
    hc_b, st_b = _mlstm_chunkwise(flip(qc), flip(kc), flip(vc), flip(gc[2]), flip(jax.nn.log_sigmoid(gc[3])), zero)
    h_b, _ = _mlstm_chunkwise(flip(q), flip(k), flip(v), flip(g[2]), flip(jax.nn.log_sigmoid(g[3])), st_b)
    return h_f + flip(h_b), hc_f + flip(hc_b)


def _head_norm(h, g):
    h = jnp.transpose(h, (0, 2, 1, 3)).astype(jnp.float32)
    mu = h.mean(-1, keepdims=True)
    var = jnp.mean(jnp.square(h - mu), -1, keepdims=True)
    hn = (h - mu) * lax.rsqrt(var + LN_EPS)
    return hn.reshape(hn.shape[0], hn.shape[1], -1) * g


def _expert_choice_ffn(h, w_router, w_gate, w_up, w_down):
    B, T, D = h.shape
    cap = CAPACITY_FACTOR * T // N_EXPERTS
    aff = jax.nn.softmax(jnp.einsum('btd,de->bte', h, w_router).astype(jnp.float32), axis=-1)
    g, idx = lax.top_k(jnp.swapaxes(aff, 1, 2), cap)
    g = jnp.moveaxis(g, 1, 0).astype(h.dtype)
    idx = jnp.moveaxis(idx, 1, 0)

    def expert(args):
        idx_e, g_e, wg, wu, wd = args
        xe = jax.vmap(lambda hb, ib: hb[ib])(h, idx_e)
        return ((jax.nn.silu(xe @ wg) * (xe @ wu)) @ wd) * g_e[..., None]

    y = lax.map(expert, (idx, g, w_gate, w_up, w_down))
    return jnp.zeros_like(h).at[jnp.arange(B)[None, :, None], idx].add(y)


def _layer(x, ctx, c, c_ctx, w_ada, b_ada, w_in, b_gate, conv_qk, na_rel_bias, ml_norm_g, w_out,
           ln1_g, ln1_b, w_router, w_expert_gate, w_expert_up, w_expert_down, ln2_g, ln2_b, update_ctx):
    B, T, _ = x.shape
    Lc = ctx.shape[1]
    t = jnp.arange(T)
    pos = (t // GRID_W, t % GRID_W)
    sh1, sc1, g1, sh2, sc2, g2 = _ada(c, w_ada, b_ada)
    csh1, csc1, cg1, csh2, csc2, cg2 = _ada(c_ctx, w_ada, b_ada)

    ux = (x * (1 + sc1) + sh1) @ w_in
    uc = (ctx * (1 + csc1) + csh1) @ w_in
    qa, ka, va, qm, km, vm, om, gt = jnp.split(ux, SPLITS, axis=-1)
    qa_c, ka_c, va_c, qm_c, km_c, vm_c, om_c, gt_c = jnp.split(uc, SPLITS, axis=-1)
    heads = lambda a: a.reshape(a.shape[0], a.shape[1], NA_HEADS, NA_HEAD_DIM)
    ka_c, va_c = heads(ka_c), heads(va_c)

    att = _neighbourhood_attention(heads(qa), heads(ka), heads(va), ka_c, va_c, na_rel_bias)

    h_lat, h_ctx = _mlstm_bidirectional(_mlstm_inputs(qm, km, vm, gt, b_gate, conv_qk, pos),
                                        _mlstm_inputs(qm_c, km_c, vm_c, gt_c, b_gate, conv_qk, None))
    ml = _head_norm(h_lat, ml_norm_g).astype(x.dtype) * jax.nn.sigmoid(om)

    mix = jnp.concatenate([att, ml], axis=-1) @ w_out
    x_new = _layer_norm(DEEPNORM_ALPHA * x + g1 * mix, ln1_g, ln1_b)
    moe = _expert_choice_ffn(x_new * (1 + sc2) + sh2, w_router, w_expert_gate, w_expert_up, w_expert_down)
    x_new = _layer_norm(DEEPNORM_ALPHA * x_new + g2 * moe, ln2_g, ln2_b)

    if update_ctx:
        att_c = _context_attention(heads(qa_c), ka_c, va_c)
        ml_c = _head_norm(h_ctx, ml_norm_g).astype(ctx.dtype) * jax.nn.sigmoid(om_c)
        mix_c = jnp.concatenate([att_c, ml_c], axis=-1) @ w_out
        ctx = _layer_norm(DEEPNORM_ALPHA * ctx + cg1 * mix_c, ln1_g, ln1_b)
        moe_c = _expert_choice_ffn(ctx * (1 + csc2) + csh2, w_router, w_expert_gate, w_expert_up, w_expert_down)
        ctx = _layer_norm(DEEPNORM_ALPHA * ctx + cg2 * moe_c, ln2_g, ln2_b)
    return x_new, ctx


def setup_inputs(seed: int = 0) -> dict:
    key = jax.random.key(seed)
    ks = jax.random.split(key, 20)
    f32 = jnp.float32
    D = D_MODEL
    nrm = lambda k, shape, scale: jax.random.normal(k, shape, f32) * scale
    gate_base = np.concatenate([np.zeros(ML_HEADS), np.linspace(3.0, 6.0, ML_HEADS),
                                np.zeros(ML_HEADS), np.linspace(3.0, 6.0, ML_HEADS)]).astype(np.float32)
    return {
        "x": nrm(ks[0], (BATCH, SEQ, D), 1.0),
        "c": nrm(ks[1], (BATCH, D), 1.0),
        "ctx": nrm(ks[2], (BATCH, CTX_LEN, D), 1.0),
        "c_ctx": nrm(ks[3], (D,), 1.0),
        "w_ada": nrm(ks[4], (DEPTH, D, 6 * D), 0.5 * D ** -0.5),
        "b_ada": nrm(ks[5], (DEPTH, 6 * D), 0.02),
        "w_in": nrm(ks[6], (DEPTH, D, D_IN), D ** -0.5),
        "b_gate": jnp.asarray(gate_base)[None, :] + nrm(ks[7], (DEPTH, ML_N_GATES), 0.1),
        "conv_qk": nrm(ks[8], (DEPTH, CONV_K, 2 * ML_QK_W), CONV_K ** -0.5),
        "na_rel_bias": nrm(ks[9], (DEPTH, NA_HEADS, 2 * NA_WIN_H - 1, 2 * NA_WIN_W - 1), 0.1),
        "ml_norm_g": 1.0 + nrm(ks[10], (DEPTH, ML_V_W), 0.02),
        "w_out": nrm(ks[11], (DEPTH, D_MIX, D), DEEPNORM_BETA * D_MIX ** -0.5),
        "ln1_g": 1.0 + nrm(ks[12], (DEPTH, D), 0.02),
        "ln1_b": nrm(ks[13], (DEPTH, D), 0.02),
        "w_router": nrm(ks[14], (DEPTH, D, N_EXPERTS), D ** -0.5),
        "w_expert_gate": nrm(ks[15], (DEPTH, N_EXPERTS, D, D_FF), D ** -0.5),
        "w_expert_up": nrm(ks[16], (DEPTH, N_EXPERTS, D, D_FF), D ** -0.5),
        "w_expert_down": nrm(ks[17], (DEPTH, N_EXPERTS, D_FF, D), DEEPNORM_BETA * D_FF ** -0.5),
        "ln2_g": 1.0 + nrm(ks[18], (DEPTH, D), 0.02),
        "ln2_b": nrm(ks[19], (DEPTH, D), 0.02),
    }


def reference(x, c, ctx, c_ctx, w_ada, b_ada, w_in, b_gate, conv_qk, na_rel_bias, ml_norm_g, w_out,
              ln1_g, ln1_b, w_router, w_expert_gate, w_expert_up, w_expert_down, ln2_g, ln2_b):
    for layer in range(DEPTH):
        x, ctx = _layer(x, ctx, c, c_ctx, w_ada[layer], b_ada[layer], w_in[layer], b_gate[layer], conv_qk[layer],
                        na_rel_bias[layer], ml_norm_g[layer], w_out[layer], ln1_g[layer], ln1_b[layer],
                        w_router[layer], w_expert_gate[layer], w_expert_up[layer], w_expert_down[layer],
                        ln2_g[layer], ln2_b[layer], update_ctx=layer < DEPTH - 1)
    return x
```

```python
import os
import numpy as np
import ml_dtypes
import concourse.bass as bass
import concourse.mybir as mybir
from concourse.bass_utils import run_bass_kernel_spmd

F32 = mybir.dt.float32
BF16 = mybir.dt.bfloat16
I32 = mybir.dt.int32
U32 = mybir.dt.uint32
ALU = mybir.AluOpType
ACT = mybir.ActivationFunctionType
AX = mybir.AxisListType


class Cfg:
    def __init__(self, T=8192, DFF=2816, NE=16, debug=()):
        self.T = T
        self.DFF = DFF
        self.NE = NE
        self.D = 1024
        self.GW = 64
        self.LC = 256
        self.TX = T + self.LC
        self.NT = T // 128
        self.NTX = self.TX // 128
        self.ROWS = T // 64
        self.CAP = 2 * T // NE
        self.NSL = self.CAP // 128
        self.NFC = DFF // 128
        self.DIN = 3088
        self.debug = set(debug)


class Buf:
    __slots__ = ("name", "w", "r")

    def __init__(self, name=""):
        self.name = name
        self.w = None
        self.r = []


class Prog:
    ENGS = ("pe", "act", "dve", "pool", "sp")

    def __init__(self, nc, stack, n_dma_sems=20):
        self.nc = nc
        self.lists = {e: [] for e in self.ENGS}
        self.esem = {e: stack.enter_context(nc.semaphore("es_" + e)) for e in self.ENGS}
        self.cnt = {e: 0 for e in self.ENGS}
        self.seen = {e: {} for e in self.ENGS}
        self.dsem = {}
        self.dtot = {}
        self.drr = {}
        for q in ("sp", "pool", "act"):
            self.dsem[q] = [stack.enter_context(nc.semaphore("ds_%s%d" % (q, i))) for i in range(n_dma_sems)]
            self.dtot[q] = [0] * n_dma_sems
            self.drr[q] = 0
        self.nops = 0

    def _need(self, eng, tok, waits):
        if tok is None:
            return
        kind, key, val = tok
        sk = (kind, key)
        if self.seen[eng].get(sk, 0) >= val:
            return
        if waits.get(sk, 0) < val:
            waits[sk] = val

    def _sem(self, sk):
        kind, key = sk
        if kind == "E":
            return self.esem[key]
        return self.dsem[key[0]][key[1]]

    def op(self, eng, fn, rd=(), wr=(), sig=True, dma=False, acc=False, wr_add=False):
        waits = {}
        for b in rd:
            for t in (b.w or ()):
                if t[0] == "E" and t[1] == eng and eng == "pe":
                    continue
                self._need(eng, t, waits)
        for b in wr:
            if not wr_add:
                for t in (b.w or ()):
                    if not (t[0] == "E" and t[1] == eng):
                        self._need(eng, t, waits)
            for t in b.r:
                if not (t[0] == "E" and t[1] == eng and not dma):
                    self._need(eng, t, waits)
        lst = self.lists[eng]
        if dma:
            q = eng
            i = self.drr[q]
            self.drr[q] = (i + 1) % len(self.dsem[q])
            if self.dtot[q][i] > 0:
                self._need(eng, ("D", (q, i), self.dtot[q][i]), waits)
            self.dtot[q][i] += 16
            tok = ("D", (q, i), self.dtot[q][i])
            for sk, v in waits.items():
                lst.append(("w", self._sem(sk), v))
                self.seen[eng][sk] = v
            lst.append(("o", fn, self.dsem[q][i], 16))
        else:
            for sk, v in waits.items():
                lst.append(("w", self._sem(sk), v))
                self.seen[eng][sk] = v
            if True:
                self.cnt[eng] += 1
                tok = ("E", eng, self.cnt[eng])
                lst.append(("o", fn, self.esem[eng], 1))
            else:
                tok = ("E", eng, self.cnt[eng] + 1)
                lst.append(("o", fn, None, 0))
        for b in rd:
            b.r.append(tok)
            if len(b.r) > 24:
                b.r = b.r[-24:] if False else b.r
        for b in wr:
            if wr_add and b.w:
                b.w = b.w + [tok]
            else:
                b.w = [tok]
                b.r = []
        self.nops += 1
        return tok

    def pe(self, meth, rd=(), wr=(), **kw):
        return self.op("pe", (meth, kw), rd, wr)

    def act(self, meth, rd=(), wr=(), **kw):
        return self.op("act", (meth, kw), rd, wr)

    def dve(self, meth, rd=(), wr=(), **kw):
        return self.op("dve", (meth, kw), rd, wr)

    def pool(self, meth, rd=(), wr=(), **kw):
        return self.op("pool", (meth, kw), rd, wr)

    def dma(self, q, out, in_, rd=(), wr=(), wr_add=False, **kw):
        return self.op(q, ("dma_start", dict(out=out, in_=in_, **kw)), rd, wr, dma=True, wr_add=wr_add)

    def barrier(self, bufs=()):
        toks = []
        for e in self.ENGS:
            if self.cnt[e] > 0:
                toks.append(("E", e, self.cnt[e]))
        for q in self.dsem:
            for i, t in enumerate(self.dtot[q]):
                if t > 0:
                    toks.append(("D", (q, i), t))
        for e in self.ENGS:
            for (kind, key, val) in toks:
                sk = (kind, key)
                if kind == "E" and key == e:
                    continue
                if self.seen[e].get(sk, 0) >= val:
                    continue
                self.lists[e].append(("w", self._sem(sk), val))
                self.seen[e][sk] = val
        for b in bufs:
            b.w = None
            b.r = []

    def finish(self):
        nc = self.nc
        self.barrier()
        lists = self.lists

        def replay(engine, items):
            for it in items:
                if it[0] == "w":
                    engine.wait_ge(it[1], it[2])
                else:
                    f = it[1]
                    ins = getattr(engine, f[0])(**f[1]) if isinstance(f, tuple) else f(engine)
                    if it[2] is not None:
                        ins.then_inc(it[2], it[3])

        with nc.Block() as block:
            @block.tensor
            def _(e):
                replay(e, lists["pe"])

            @block.scalar
            def _(e):
                replay(e, lists["act"])

            @block.vector
            def _(e):
                replay(e, lists["dve"])

            @block.gpsimd
            def _(e):
                replay(e, lists["pool"])

            @block.sync
            def _(e):
                replay(e, lists["sp"])


class Arena:
    def __init__(self, nc):
        self.nc = nc
        self.base = (nc.sbuf_base + 63) // 64 * 64
        self.top = nc.sbuf_top
        self.cur = self.base
        self.n = 0

    def alloc(self, shape, dtype, name="t"):
        esz = {F32: 4, BF16: 2, I32: 4, U32: 4}[dtype]
        per = int(np.prod(shape[1:])) * esz
        per = (per + 63) // 64 * 64
        off = self.cur
        assert off + per <= self.top, "SBUF arena overflow: %s %s need %d have %d" % (name, shape, per, self.top - off)
        self.cur += per
        self.n += 1
        return self.nc.alloc_sbuf_tensor_at("%s_%d" % (name, self.n), list(shape), dtype, offset=off)

    def mark(self):
        return self.cur

    def release(self, m):
        self.cur = m


def _na_bias_layout(bias_table):
    H = bias_table.shape[0]
    NEG = np.float32(-30000.0)
    out = np.full((H, 3, 6, 128, 256), NEG, np.float32)
    c = np.arange(64)
    cs = np.clip(c - 8, 0, 48)
    for gt in range(3):
        for ql in range(4):
            for kl in range(12 if gt == 1 else 8):
                if gt == 0:
                    r, kr, rs = ql, kl, 0
                elif gt == 1:
                    r, kr, rs = 4 + ql, kl, ql
                else:
                    r, kr, rs = 4 + ql, kl, 0
                if not (rs <= kr < rs + 8):
                    continue
                dr = kr - r + 7
                for qc in range(64):
                    kcs = np.arange(cs[qc], cs[qc] + 16)
                    dc = kcs - qc + 15
                    tile = kr // 2
                    keyp = (kr % 2) * 64 + kcs
                    out[:, gt, tile, keyp, ql * 64 + qc] = bias_table[:, dr, :][:, dc]
    return out


def _rope_tables(T):
    nf = 16
    inv = (1.0 / (10000.0 ** (np.arange(nf, dtype=np.float32) / nf))).astype(np.float32)
    t = np.arange(T)
    rows = (t // 64).astype(np.float32)
    cols = (t % 64).astype(np.float32)
    cos = np.zeros((128, T), np.float32)
    sin = np.zeros((128, T), np.float32)
    for p in range(128):
        d = p % 64
        pos = rows if d < 32 else cols
        j = d % 16
        ang = (pos * inv[j]).astype(np.float32)
        cos[p] = np.cos(ang)
        sgn = -1.0 if (d % 32) < 16 else 1.0
        sin[p] = sgn * np.sin(ang)
    return cos, sin


def _perm_matrix():
    P = np.zeros((128, 128), np.float32)
    for m in range(128):
        d = m % 64
        base = m - d
        pd = d + 16 if (d % 32) < 16 else d - 16
        P[base + pd, m] = 1.0
    return P


class Ring:
    def __init__(self, items):
        self.items = items
        self.i = 0

    def next(self):
        it = self.items[self.i]
        self.i = (self.i + 1) % len(self.items)
        return it


def sb_ring(ar, n, shape, dtype, name):
    return Ring([(ar.alloc(shape, dtype, name), Buf(name)) for _ in range(n)])


class Ctx:
    pass


def build_program(cfg):
    from contextlib import ExitStack
    nc = bass.Bass("TRN2", target_bir_lowering=False)
    T, TX, NT, NTX, D, DFF, NE = cfg.T, cfg.TX, cfg.NT, cfg.NTX, cfg.D, cfg.DFF, cfg.NE
    c = Ctx()
    c.cfg = cfg
    c.nc = nc

    def din(name, shape, dt=F32):
        return nc.dram_tensor(name, list(shape), dt, kind="ExternalInput").ap()

    def dscr(name, shape, dt=F32):
        kind = "ExternalOutput" if name in cfg.debug else "Internal"
        return nc.dram_tensor(name, list(shape), dt, kind=kind).ap()

    I = Ctx()
    c.I = I
    I.x = din("x", [T, D])
    I.ctx = din("ctx", [cfg.LC, D])
    I.cvec = din("cvec", [128, 16])
    I.w_ada = din("w_ada", [D, 6 * D])
    I.b_ada = din("b_ada", [1, 6 * D])
    I.w_in = din("w_in", [D, cfg.DIN])
    I.b_gate = din("b_gate", [1, 16])
    I.convw = din("convw", [128, 4, 5])
    I.nabias = din("nabias", [8, 3, 6, 128, 256])
    I.ml_norm_g = din("ml_norm_g", [1, 512])
    I.w_out = din("w_out", [D, D])
    I.ln1_g = din("ln1_g", [1, D])
    I.ln1_b = din("ln1_b", [1, D])
    I.w_router = din("w_router", [D, NE])
    I.weg = din("weg", [NE, D, DFF])
    I.weu = din("weu", [NE, D, DFF])
    I.wed = din("wed", [NE, DFF, D])
    I.ln2_g = din("ln2_g", [1, D])
    I.ln2_b = din("ln2_b", [1, D])
    I.ropec = din("ropec", [128, T])
    I.ropes = din("ropes", [128, T])
    I.cmat = din("cmat", [5, 128, 128])
    I.iota = din("iota", [128, 128])
    out = nc.dram_tensor("out", [T, D], F32, kind="ExternalOutput").ap()
    c.out = out

    S = Ctx()
    c.S = S
    S.QA_T = dscr("QA_T", [4, 128, T], BF16)
    S.KA_T = dscr("KA_T", [4, 128, TX], BF16)
    S.VA = dscr("VA", [TX, 512], BF16)
    S.QM_T = dscr("QM_T", [2, 128, T])
    S.KM_T = dscr("KM_T", [2, 128, TX])
    S.VM = dscr("VM", [TX, 512], BF16)
    S.OM = dscr("OM", [T, 512])
    S.GT = dscr("GT", [TX, 16])
    S.MIX_T = dscr("MIX_T", [8, 128, T], BF16)
    S.HF = dscr("HF", [T, 512])
    S.HB = dscr("HB", [T, 512])
    S.ACC = dscr("ACC", [T, D])
    S.H2 = dscr("H2", [T, D], BF16)
    S.AFF = dscr("AFF", [T, NE])

    with ExitStack() as stack:
        k = Prog(nc, stack)
        c.k = k
        ar = Arena(nc)
        c.ar = ar
        c.ps = [(nc.alloc_psum_tensor("ps%d" % i, [128, 512], F32), Buf("ps%d" % i)) for i in range(8)]
        c.cm = ar.alloc([128, 5, 128], F32, "cmat")
        c.cm_b = Buf("cmat")
        k.dma("sp", c.cm[:, :, :], I.cmat.rearrange("a p q -> p a q"), wr=[c.cm_b])
        c.cmb = ar.alloc([128, 5, 128], BF16, "cmatb")
        c.cmb_b = Buf("cmatb")
        k.dve("tensor_copy", [c.cm_b], [c.cmb_b], out=c.cmb[:, :, :], in_=c.cm[:, :, :])
        c.ident, c.perm, c.triF, c.triB, c.ones = [c.cm[:, i, :] for i in range(5)]
        c.identb, c.permb, c.triFb, c.triBb, c.onesb = [c.cmb[:, i, :] for i in range(5)]

        c.modp = ar.alloc([128, 4, 8], F32, "modp")
        c.modp_b = Buf("modp")
        c.G2t = ar.alloc([128, 1024], F32, "G2t")
        c.g2_b = Buf("G2t")
        c.AFFt = ar.alloc([128, cfg.NT, cfg.NE], F32, "AFFt")
        c.aff_b = Buf("AFFt")
        c.IDX = ar.alloc([128, cfg.NE, cfg.NSL], U32, "IDX")
        c.idx_b = Buf("IDX")
        c.mb_mark = ar.mark()
        for nm, fn in (("A", stage_a), ("B", stage_b), ("C", stage_c), ("D", stage_d), ("E", stage_e),
                       ("F", stage_f), ("G", stage_g), ("H", stage_h)):
            fn(c)
            if any(d_.startswith("stop" + nm) for d_ in cfg.debug):
                break
        k.finish()
    return nc


def dump(c, name, ap, shape, buf, dt=F32):
    if name in c.cfg.debug:
        d = c.nc.dram_tensor(name, list(shape), dt, kind="ExternalOutput").ap()
        c.k.dma("sp", d, ap, rd=[buf])


class Evac:
    def __init__(self, k):
        self.k = k
        self.n = 0

    def __call__(self, out_ap, in_ap, rd, wr, scale=None, eng=None):
        k = self.k
        self.n += 1
        use_act = (self.n % 2 == 0) if eng is None else (eng == "act")
        if use_act:
            if scale is None:
                k.act("activation", rd, wr, out=out_ap, in_=in_ap, func=ACT.Copy)
            else:
                k.act("activation", rd, wr, out=out_ap, in_=in_ap, func=ACT.Copy, scale=scale)
        else:
            if scale is None:
                k.dve("tensor_copy", rd, wr, out=out_ap, in_=in_ap)
            else:
                k.dve("tensor_scalar_mul", rd, wr, out=out_ap, in0=in_ap, scalar1=scale)


def stage_a(c):
    k, ar, I, nc = c.k, c.ar, c.I, c.nc
    D = 1024
    c.MB = ar.alloc([128, 6 * D], F32, "MB")
    c.MB_b = Buf("MB")
    m0 = ar.mark()
    MBC = ar.alloc([128, 2 * D], F32, "MBC")
    MBC_b = Buf("MBC")
    cv = ar.alloc([128, 16], F32, "cv")
    cv_b = Buf()
    sv = ar.alloc([128, 16], F32, "sv")
    sv_b = Buf()
    SR = ar.alloc([128, 16, 128], F32, "SR")
    SR_b = Buf()
    bab = ar.alloc([128, 6 * D], F32, "bab")
    bab_b = Buf()
    wring = sb_ring(ar, 3, [128, 2048], F32, "wada")
    k.dma("sp", cv[:, :], I.cvec[:, :], wr=[cv_b])
    k.dma("sp", bab[:, :], I.b_ada.partition_broadcast(128), wr=[bab_b])
    k.act("activation", [cv_b], [sv_b], out=sv[:, :], in_=cv[:, :], func=ACT.Silu)
    k.dve("tensor_copy", [sv_b], [SR_b], out=SR[:, :, :], in_=sv[:, :].unsqueeze(2).to_broadcast([128, 16, 128]))
    for piece in range(3):
        nb = 8 if piece == 0 else 4
        for kk in range(8):
            wt, wb = wring.next()
            k.dma("sp", wt[:, :], I.w_ada[kk * 128:(kk + 1) * 128, piece * 2048:(piece + 1) * 2048], wr=[wb])
            for j in range(nb):
                pt, pb = c.ps[j]
                lhs = SR[:, kk, :] if j < 4 else SR[:, 8 + kk, :]
                jj = j % 4
                k.pe("matmul", [SR_b, wb], [pb], out=pt[:, :], lhsT=lhs, rhs=wt[:, jj * 512:(jj + 1) * 512],
                     start=(kk == 0), stop=(kk == 7))
        for j in range(nb):
            pt, pb = c.ps[j]
            jj = j % 4
            col = piece * 2048 + jj * 512
            if j < 4:
                k.dve("tensor_tensor", [pb, bab_b], [c.MB_b], out=c.MB[:, col:col + 512], in0=pt[:, :],
                      in1=bab[:, col:col + 512], op=ALU.add)
            else:
                k.dve("tensor_tensor", [pb, bab_b], [MBC_b], out=MBC[:, col:col + 512], in0=pt[:, :],
                      in1=bab[:, col:col + 512], op=ALU.add)
    srcs = [(c.MB, c.MB_b, 1024, 0, 1.0), (c.MB, c.MB_b, 0, 1, 0.0), (MBC, MBC_b, 1024, 2, 1.0), (MBC, MBC_b, 0, 3, 0.0)]
    n = 0
    for (src, sb, off, slot, addc) in srcs:
        for half in range(2):
            pt, pb = c.ps[n % 8]
            n += 1
            for q in range(4):
                ch = half * 4 + q
                k.pe("transpose", [sb, c.cm_b], [pb], out=pt[:, q * 128:(q + 1) * 128],
                     in_=src[:, off + ch * 128: off + (ch + 1) * 128], identity=c.ident)
            k.dve("tensor_scalar_add", [pb], [c.modp_b], out=c.modp[:, slot, half * 4:(half + 1) * 4],
                  in0=pt[:, :].rearrange("p (q t) -> p q t", t=128)[:, :, 0], scalar1=addc)
    k.dve("tensor_copy", [c.MB_b], [c.g2_b], out=c.G2t[:, :], in_=c.MB[:, 5120:6144])
    dump(c, "MB", c.MB[:, :], [128, 6144], c.MB_b)
    dump(c, "MODP", c.modp[:, :, :], [128, 4, 8], c.modp_b)
    k.barrier()
    ar.release(m0)


def stage_b(c):
    k, ar, I, S, nc, cfg = c.k, c.ar, c.I, c.S, c.nc, c.cfg
    T, TX, NT, NTX = cfg.T, cfg.TX, cfg.NT, cfg.NTX
    m0 = ar.mark()
    W = ar.alloc([128, 8, cfg.DIN], BF16, "win")
    W_b = Buf("win")
    wst = sb_ring(ar, 2, [128, cfg.DIN], F32, "winst")
    evac = Evac(k)
    for kk in range(8):
        wt, wb = wst.next()
        k.dma("sp", wt[:, :], I.w_in[kk * 128:(kk + 1) * 128, :], wr=[wb])
        evac(W[:, kk, :], wt[:, :], [wb], [W_b])
    xring = sb_ring(ar, 3, [128, 1024], F32, "xin")
    xmring = sb_ring(ar, 2, [128, 8, 512], BF16, "xm")
    st_bf = sb_ring(ar, 4, [128, 512], BF16, "stbf")
    st_f = sb_ring(ar, 4, [128, 512], F32, "stf")
    st_g = sb_ring(ar, 3, [128, 16], F32, "stg")
    ps_tp = Ring([c.ps[0], c.ps[1]])
    ps_tm = Ring([c.ps[2], c.ps[3], c.ps[4]])
    ps_fm = Ring([c.ps[5], c.ps[6], c.ps[7]])
    ngroups = (NTX + 3) // 4
    for g in range(ngroups):
        tiles = list(range(4 * g, min(4 * g + 4, NTX)))
        ntok = 128 * len(tiles)
        xm, xm_b = xmring.next()
        is_ctx = tiles[0] >= NT
        sl_sc, sl_sh = (2, 3) if is_ctx else (0, 1)
        for ti, i in enumerate(tiles):
            xt, xb = xring.next()
            src = I.ctx[(i - NT) * 128:(i - NT + 1) * 128, :] if is_ctx else I.x[i * 128:(i + 1) * 128, :]
            k.dma("sp", xt[:, :], src, wr=[xb])
            for half in range(2):
                pt, pb = ps_tp.next()
                for q in range(4):
                    ch = half * 4 + q
                    k.pe("transpose", [xb, c.cm_b], [pb], out=pt[:, q * 128:(q + 1) * 128],
                         in_=xt[:, ch * 128:(ch + 1) * 128], identity=c.ident)
                for q in range(4):
                    ch = half * 4 + q
                    o_ap = xm[:, ch, ti * 128:(ti + 1) * 128]
                    i_ap = pt[:, q * 128:(q + 1) * 128]
                    s1 = c.modp[:, sl_sc, ch:ch + 1]
                    s2 = c.modp[:, sl_sh, ch:ch + 1]
                    if q % 2 == 0:
                        k.dve("tensor_scalar", [pb, c.modp_b], [xm_b], out=o_ap, in0=i_ap, scalar1=s1, scalar2=s2,
                              op0=ALU.mult, op1=ALU.add)
                    else:
                        k.act("activation", [pb, c.modp_b], [xm_b], out=o_ap, in_=i_ap, func=ACT.Identity,
                              bias=s2, scale=s1)
            tsl = slice(ti * 128, (ti + 1) * 128)
            rows = slice(i * 128, (i + 1) * 128)
            for (c0, ncol, kind) in ((1024, 512, "va"), (2048, 512, "vm"), (2560, 512, "om"), (3072, 16, "gt")):
                if kind == "om" and is_ctx:
                    continue
                pt, pb = ps_tm.next()
                for kk in range(8):
                    k.pe("matmul", [xm_b, W_b], [pb], out=pt[:, 0:ncol], lhsT=xm[:, kk, tsl],
                         rhs=W[:, kk, c0:c0 + ncol], start=(kk == 0), stop=(kk == 7))
                if kind in ("va", "vm"):
                    st, stb = st_bf.next()
                    evac(st[:, :], pt[:, :], [pb], [stb])
                    dst = S.VA if kind == "va" else S.VM
                    k.dma("sp", dst[rows, :], st[:, :], rd=[stb])
                elif kind == "om":
                    st, stb = st_f.next()
                    evac(st[:, :], pt[:, :], [pb], [stb])
                    k.dma("sp", S.OM[rows, :], st[:, :], rd=[stb])
                else:
                    st, stb = st_g.next()
                    evac(st[:, :], pt[:, 0:16], [pb], [stb])
                    k.dma("sp", S.GT[rows, :], st[:, :], rd=[stb])
        tok0 = tiles[0] * 128
        for ci in range(12):
            kind = ("qa", "ka", "qm", "km")[0 if ci < 4 else 1 if ci < 8 else 2 if ci < 10 else 3]
            if is_ctx and kind in ("qa", "qm"):
                continue
            c0 = {"qa": 0, "ka": 512, "qm": 1536, "km": 1792}[kind]
            j = ci if ci < 4 else ci - 4 if ci < 8 else ci - 8 if ci < 10 else ci - 10
            pt, pb = ps_fm.next()
            for kk in range(8):
                k.pe("matmul", [xm_b, W_b], [pb], out=pt[:, 0:ntok], lhsT=W[:, kk, c0 + j * 128:c0 + (j + 1) * 128],
                     rhs=xm[:, kk, 0:ntok], start=(kk == 0), stop=(kk == 7))
            if kind in ("qa", "ka"):
                st, stb = st_bf.next()
                evac(st[:, 0:ntok], pt[:, 0:ntok], [pb], [stb], scale=(0.125 if kind == "qa" else None))
                dst = S.QA_T if kind == "qa" else S.KA_T
                k.dma("sp", dst[j, :, tok0:tok0 + ntok], st[:, 0:ntok], rd=[stb])
            else:
                st, stb = st_f.next()
                evac(st[:, 0:ntok], pt[:, 0:ntok], [pb], [stb])
                dst = S.QM_T if kind == "qm" else S.KM_T
                k.dma("sp", dst[j, :, tok0:tok0 + ntok], st[:, 0:ntok], rd=[stb])
    k.barrier()
    ar.release(m0)


def stage_c(c):
    k, ar, I, S, nc, cfg = c.k, c.ar, c.I, c.S, c.nc, c.cfg
    T, TX, NT, NTX = cfg.T, cfg.TX, cfg.NT, cfg.NTX
    G = cfg.ROWS // 4
    m0 = ar.mark()
    qring = sb_ring(ar, 2, [128, T], BF16, "qT")
    kring = sb_ring(ar, 2, [128, TX], BF16, "kT")
    vring = sb_ring(ar, 2, [128, NTX, 128], BF16, "Vp")
    bias = [(ar.alloc([128, 3, 6, 256], F32, "nab"), Buf("nab")) for _ in range(2)]
    ptring = sb_ring(ar, 3, [128, 8, 256], BF16, "PT")
    tmpring = sb_ring(ar, 3, [128, 256], F32, "stmp")
    rdring = sb_ring(ar, 2, [128, 256], F32, "rden")
    attring = sb_ring(ar, 2, [128, 256], BF16, "attT")
    ps_s = Ring([c.ps[i] for i in range(5)])
    ps_o = Ring([c.ps[5], c.ps[6], c.ps[7]])
    for j in range(4):
        qT, q_b = qring.next()
        kT, k_b = kring.next()
        V, v_b = vring.next()
        k.dma("sp", qT[:, :], S.QA_T[j], wr=[q_b])
        k.dma("sp", kT[:, :], S.KA_T[j], wr=[k_b])
        for n0 in range(0, NTX, 4):
            n1 = min(NTX, n0 + 4)
            k.dma("sp", V[:, n0:n1, :], S.VA[n0 * 128:n1 * 128, j * 128:(j + 1) * 128].rearrange("(n p) c -> p n c", p=128),
                  wr=[v_b], wr_add=(n0 > 0))
        for hh in range(2):
            first = True
            for t_ in range(3):
                for a0 in (0, 3):
                    k.dma("sp", bias[hh][0][:, t_, a0:a0 + 3, :], I.nabias[2 * j + hh, t_, a0:a0 + 3].rearrange("a p q -> p a q"),
                          wr=[bias[hh][1]], wr_add=(not first))
                    first = False
        for g in range(G):
            q0 = g * 256
            if g == 0:
                gt, t0, nl = 0, 0, 4
            elif g == G - 1:
                gt, t0, nl = 2, NT - 4, 4
            else:
                gt, t0, nl = 1, 2 * g - 2, 6
            tiles = [t0 + a for a in range(nl)] + [NT, NT + 1]
            att, att_b = attring.next()
            for hh in range(2):
                pr = slice(hh * 64, hh * 64 + 64)
                bt, bb = bias[hh]
                PT, pt_b = ptring.next()
                for a, tile in enumerate(tiles):
                    pst, psb = ps_s.next()
                    k.pe("matmul", [k_b, q_b], [psb], out=pst[:, 0:256], lhsT=kT[pr, tile * 128:(tile + 1) * 128],
                         rhs=qT[pr, q0:q0 + 256], start=True, stop=True)
                    if a < nl:
                        tmp, tmp_b = tmpring.next()
                        k.dve("tensor_tensor", [psb, bb], [tmp_b], out=tmp[:, :], in0=pst[:, 0:256],
                              in1=bt[:, gt, a, :], op=ALU.add)
                        k.act("activation", [tmp_b], [pt_b], out=PT[:, a, :], in_=tmp[:, :], func=ACT.Exp)
                    else:
                        k.act("activation", [psb], [pt_b], out=PT[:, a, :], in_=pst[:, 0:256], func=ACT.Exp)
                po, pob = ps_o.next()
                na = len(tiles)
                for a, tile in enumerate(tiles):
                    k.pe("matmul", [v_b, pt_b], [pob], out=po[pr, 0:256], lhsT=V[:, tile, pr], rhs=PT[:, a, :],
                         start=(a == 0), stop=(a == na - 1))
                for a, tile in enumerate(tiles):
                    k.pe("matmul", [c.cmb_b, pt_b], [pob], out=po[pr, 256:512], lhsT=c.onesb[:, pr], rhs=PT[:, a, :],
                         start=(a == 0), stop=(a == na - 1))
                rd, rd_b = rdring.next()
                k.dve("reciprocal", [pob], [rd_b], out=rd[pr, :], in_=po[pr, 256:512])
                k.dve("tensor_tensor", [pob, rd_b], [att_b], out=att[pr, :], in0=po[pr, 0:256], in1=rd[pr, :], op=ALU.mult)
            k.dma("sp", S.MIX_T[j, :, q0:q0 + 256], att[:, :], rd=[att_b])
    k.barrier()
    ar.release(m0)


def stage_d(c):
    k, ar, I, S, nc, cfg = c.k, c.ar, c.I, c.S, c.nc, c.cfg
    T, TX, NT, NTX = cfg.T, cfg.TX, cfg.NT, cfg.NTX
    m0 = ar.mark()
    evac = Evac(k)
    GTt = ar.alloc([128, NTX, 16], F32, "gt")
    g_b = Buf("gt")
    bg = ar.alloc([128, 16], F32, "bg")
    bg_b = Buf("bg")
    for n0 in range(0, NTX, 4):
        n1 = min(NTX, n0 + 4)
        k.dma("sp", GTt[:, n0:n1, :], S.GT[n0 * 128:n1 * 128, :].rearrange("(n p) g -> p n g", p=128), wr=[g_b],
              wr_add=(n0 > 0))
    k.dma("sp", bg[:, :], I.b_gate.partition_broadcast(128), wr=[bg_b])
    k.dve("tensor_tensor", [g_b, bg_b], [g_b], out=GTt[:, :, :], in0=GTt[:, :, :],
          in1=bg[:, :].unsqueeze(1).to_broadcast([128, NTX, 16]), op=ALU.add)
    LF = ar.alloc([128, NTX, 8], F32, "LF")
    lf_b = Buf("LF")
    IG = ar.alloc([128, NTX, 8], F32, "IG")
    ig_b = Buf("IG")
    for d in range(2):
        k.act("activation", [g_b], [lf_b], out=LF[:, :, d * 4:(d + 1) * 4], in_=GTt[:, :, 8 * d + 4:8 * d + 8],
              func=ACT.Exp, scale=-1.0)
        k.dve("tensor_copy", [g_b], [ig_b], out=IG[:, :, d * 4:(d + 1) * 4], in_=GTt[:, :, 8 * d:8 * d + 4])
    k.act("activation", [lf_b], [lf_b], out=LF[:, :, :], in_=LF[:, :, :], func=ACT.Ln, bias=1.0)
    k.dve("tensor_scalar_mul", [lf_b], [lf_b], out=LF[:, :, :], in0=LF[:, :, :], scalar1=-1.0)
    BC = ar.alloc([128, NTX, 8], F32, "BC")
    bc_b = Buf("BC")
    eT = ar.alloc([128, NTX, 8], F32, "eT")
    et_b = Buf("eT")
    eS = ar.alloc([128, NTX, 8], F32, "eS")
    es_b = Buf("eS")
    eL = ar.alloc([128, NTX, 8], F32, "eL")
    el_b = Buf("eL")
    NC4 = NTX * 4
    for d in range(2):
        pt, pb = c.ps[d]
        k.pe("matmul", [lf_b, c.cm_b], [pb], out=pt[:, 0:NC4], lhsT=(c.triF if d == 0 else c.triB),
             rhs=LF[:, :, d * 4:(d + 1) * 4], start=True, stop=True)
        k.dve("tensor_copy", [pb], [bc_b], out=BC[:, :, d * 4:(d + 1) * 4],
              in_=pt[:, 0:NC4].rearrange("p (n h) -> p n h", h=4))
    pt, pb = c.ps[2]
    pt2, pb2 = c.ps[3]
    half = NTX * 8 // 2
    LF2 = LF[:, :, :].rearrange("p n h -> p (n h)")
    k.pe("matmul", [lf_b, c.cm_b], [pb], out=pt[:, 0:half], lhsT=c.ones, rhs=LF2[:, 0:half], start=True, stop=True)
    k.pe("matmul", [lf_b, c.cm_b], [pb2], out=pt2[:, 0:half], lhsT=c.ones, rhs=LF2[:, half:2 * half], start=True, stop=True)
    eL2 = eL[:, :, :].rearrange("p n h -> p (n h)")
    k.act("activation", [pb], [el_b], out=eL2[:, 0:half], in_=pt[:, 0:half], func=ACT.Exp)
    k.act("activation", [pb2], [el_b], out=eL2[:, half:2 * half], in_=pt2[:, 0:half], func=ACT.Exp)
    k.act("activation", [bc_b], [et_b], out=eT[:, :, :], in_=BC[:, :, :], func=ACT.Exp)
    k.dve("tensor_tensor", [ig_b, bc_b], [es_b], out=eS[:, :, :], in0=IG[:, :, :], in1=BC[:, :, :], op=ALU.subtract)
    k.act("activation", [es_b], [es_b], out=eS[:, :, :], in_=eS[:, :, :], func=ACT.Exp)
    dump(c, "DBG_BC", BC[:, :, :], [128, NTX, 8], bc_b)
    qT = [(ar.alloc([128, T], BF16, "mqT"), Buf("mqT")) for _ in range(2)]
    kT = [(ar.alloc([128, TX], BF16, "mkT"), Buf("mkT")) for _ in range(2)]
    KTM = ar.alloc([128, NTX, 256], BF16, "ktm")
    ktm_b = Buf("ktm")
    cw = ar.alloc([128, 4, 5], F32, "cw")
    cw_b = Buf("cw")
    k.dma("sp", cw[:, :, :], I.convw[:, :, :], wr=[cw_b])
    m1 = ar.mark()
    BLK = min(int(os.environ.get('KBLK', 1024)), T)
    rawr = sb_ring(ar, 2, [128, BLK + 4], F32, "raw")
    accr = sb_ring(ar, 2, [128, BLK], F32, "acc")
    sr = sb_ring(ar, 2, [128, BLK], F32, "sil")
    cosr = sb_ring(ar, 2, [128, BLK], F32, "cos")
    sinr = sb_ring(ar, 2, [128, BLK], F32, "sin")
    t1r = sb_ring(ar, 2, [128, 512], F32, "t1")
    t2r = sb_ring(ar, 2, [128, 512], F32, "t2")
    ps_r = Ring([c.ps[4], c.ps[5], c.ps[6], c.ps[7]])
    for ch in range(4):
        isq = ch < 2
        jj = ch % 2
        src = S.QM_T[jj] if isq else S.KM_T[jj]
        dstT, dst_b = (qT[jj] if isq else kT[jj])
        scale = 0.125 if isq else 1.0
        segs = [(t0, min(BLK, T - t0), 0, T) for t0 in range(0, T, BLK)]
        if not isq:
            segs.append((T, cfg.LC, T, TX))
        for (t0, n, lo, hi) in segs:
            raw, raw_b = rawr.next()
            a0 = max(lo, t0 - 2)
            a1 = min(hi, t0 + n + 2)
            if a0 > t0 - 2 or a1 < t0 + n + 2:
                k.pool("memset", [], [raw_b], ap=raw[:, 0:n + 4], constant=0.0)
            k.dma("sp", raw[:, a0 - (t0 - 2):a1 - (t0 - 2)], src[:, a0:a1], wr=[raw_b])
            acc, acc_b = accr.next()
            k.dve("tensor_scalar_mul", [raw_b, cw_b], [acc_b], out=acc[:, 0:n], in0=raw[:, 0:n], scalar1=cw[:, ch, 0:1])
            for j in range(1, 5):
                k.dve("scalar_tensor_tensor", [raw_b, cw_b, acc_b], [acc_b], out=acc[:, 0:n], in0=raw[:, j:j + n],
                      scalar=cw[:, ch, j:j + 1], in1=acc[:, 0:n], op0=ALU.mult, op1=ALU.add)
            if lo == T:
                k.act("activation", [acc_b], [dst_b], out=dstT[:, t0:t0 + n], in_=acc[:, 0:n], func=ACT.Silu)
                continue
            s_, s_b = sr.next()
            k.act("activation", [acc_b], [s_b], out=s_[:, 0:n], in_=acc[:, 0:n], func=ACT.Silu)
            cs, cs_b = cosr.next()
            sn, sn_b = sinr.next()
            k.dma("sp", cs[:, 0:n], I.ropec[:, t0:t0 + n], wr=[cs_b])
            k.dma("sp", sn[:, 0:n], I.ropes[:, t0:t0 + n], wr=[sn_b])
            for p0 in range(0, n, 512):
                pn = min(512, n - p0)
                pt, pb = ps_r.next()
                k.pe("matmul", [s_b, c.cm_b], [pb], out=pt[:, 0:pn], lhsT=c.perm, rhs=s_[:, p0:p0 + pn], start=True, stop=True)
                t1, t1_b = t1r.next()
                t2, t2_b = t2r.next()
                k.dve("scalar_tensor_tensor", [s_b, cs_b], [t1_b], out=t1[:, 0:pn], in0=s_[:, p0:p0 + pn], scalar=scale,
                       in1=cs[:, p0:p0 + pn], op0=ALU.mult, op1=ALU.mult)
                k.dve("scalar_tensor_tensor", [pb, sn_b], [t2_b], out=t2[:, 0:pn], in0=pt[:, 0:pn], scalar=scale,
                      in1=sn[:, p0:p0 + pn], op0=ALU.mult, op1=ALU.mult)
                k.dve("tensor_tensor", [t1_b, t2_b], [dst_b], out=dstT[:, t0 + p0:t0 + p0 + pn], in0=t1[:, 0:pn],
                      in1=t2[:, 0:pn], op=ALU.add)
    dump(c, "DBG_QT", qT[0][0][:, :], [128, T], qT[0][1], BF16)
    dump(c, "DBG_KT", kT[1][0][:, :], [128, TX], kT[1][1], BF16)
    for jj in range(2):
        kt, kt_b = kT[jj]
        for n0 in range(0, NTX, 4):
            nn = min(4, NTX - n0)
            pt, pb = ps_r.next()
            ptb = pt[:, :].bitcast(BF16)
            for a in range(nn):
                k.pe("transpose", [kt_b, c.cmb_b], [pb], out=ptb[:, a * 128:(a + 1) * 128],
                     in_=kt[:, (n0 + a) * 128:(n0 + a + 1) * 128], identity=c.identb)
            evac(KTM[:, n0:n0 + nn, jj * 128:(jj + 1) * 128], ptb[:, 0:nn * 128].rearrange("p (a x) -> p a x", x=128),
                 [pb], [ktm_b])
    k.barrier()
    ar.release(m1)
    C32 = ar.alloc([128, 4, 129], F32, "C32")
    Cbf = ar.alloc([128, 4, 129], BF16, "Cbf")
    cbuf = {}
    for d in range(2):
        for h in range(4):
            cbuf[(d, h)] = (Buf("c32"), Buf("cbf"))
    k.pool("memset", [], [cbuf[(d, h)][0] for d in range(2) for h in range(4)], ap=C32[:, :, :], constant=0.0)
    k.pool("memset", [], [cbuf[(d, h)][1] for d in range(2) for h in range(4)], ap=Cbf[:, :, :], constant=0.0)
    vmr = sb_ring(ar, 6, [128, 4, 129], BF16, "vma")
    for (vt, vb) in vmr.items:
        k.pool("memset", [], [vb], ap=vt[:, :, :], constant=1.0)
    vpr = sb_ring(ar, 4, [128, 4, 129], BF16, "vp4")
    ptmr = sb_ring(ar, 4, [128, 128], BF16, "ptm")
    hsr = sb_ring(ar, 4, [128, 129], F32, "hs")
    ddr = sb_ring(ar, 4, [128, 1], F32, "dd")
    hor = sb_ring(ar, 4, [128, 512], F32, "hout")
    tmpr = sb_ring(ar, 4, [128, 129], F32, "ctmp")
    ps_s = Ring([c.ps[0], c.ps[1], c.ps[2]])
    ps_o = Ring([c.ps[3], c.ps[4], c.ps[5]])
    ps_u = Ring([c.ps[6], c.ps[7]])
    seq = {0: [NT, NT + 1] + list(range(NT)), 1: [NT + 1, NT] + list(range(NT - 1, -1, -1))}
    for step in range(NT + 2):
        for d in range(2):
            n = seq[d][step]
            latent = n < NT
            tok = slice(n * 128, (n + 1) * 128)
            vt, vb = vmr.next()
            k.dma("sp", vt[:, :, 0:128], S.VM[n * 128:(n + 1) * 128, :].rearrange("p (h v) -> p h v", v=128), wr=[vb])
            vp, vp_b = vpr.next()
            k.dve("tensor_tensor", [vb, es_b], [vp_b], out=vp[:, :, :], in0=vt[:, :, :],
                  in1=eS[:, n, d * 4:(d + 1) * 4].unsqueeze(2).to_broadcast([128, 4, 129]), op=ALU.mult)
            if latent:
                ho, ho_b = hor.next()
            for h in range(4):
                jj, hh = h // 2, h % 2
                pr = slice(hh * 64, hh * 64 + 64)
                slot = d * 2 + jj
                c32_b, cbf_b = cbuf[(d, h)]
                col = d * 4 + h
                if latent:
                    q_, q_b = qT[jj]
                    kt, kt_b = kT[jj]
                    pst, psb = ps_s.next()
                    k.pe("matmul", [kt_b, q_b], [psb], out=pst[:, 0:128], lhsT=kt[pr, tok], rhs=q_[pr, tok], start=True, stop=True)
                    ptm, ptm_b = ptmr.next()
                    k.dve("tensor_tensor", [psb, c.cm_b], [ptm_b], out=ptm[:, :], in0=pst[:, 0:128],
                          in1=(c.triF if d == 0 else c.triB), op=ALU.mult)
                    po, pob = ps_o.next()
                    k.pe("matmul", [ptm_b, vp_b], [pob], out=po[:, 0:129], lhsT=ptm[:, :], rhs=vp[:, h, :], start=True, stop=False)
                    k.pe("matmul", [q_b, cbf_b], [pob], out=po[:, 0:129], lhsT=q_[pr, tok], rhs=Cbf[pr, slot, :], start=False, stop=True)
                    hs, hs_b = hsr.next()
                    k.act("activation", [pob, et_b], [hs_b], out=hs[:, :], in_=po[:, 0:129], func=ACT.Copy, scale=eT[:, n, col:col + 1])
                    dd, dd_b = ddr.next()
                    k.dve("tensor_scalar", [hs_b], [dd_b], out=dd[:, :], in0=hs[:, 128:129], scalar1=-1.0, scalar2=1.0,
                          op0=ALU.mult, op1=ALU.max)
                    k.dve("tensor_tensor", [hs_b, dd_b], [dd_b], out=dd[:, :], in0=dd[:, :], in1=hs[:, 128:129], op=ALU.max)
                    k.dve("reciprocal", [dd_b], [dd_b], out=dd[:, :], in_=dd[:, :])
                    k.dve("tensor_scalar_mul", [hs_b, dd_b], [ho_b], out=ho[:, h * 128:(h + 1) * 128], in0=hs[:, 0:128],
                          scalar1=dd[:, 0:1])
                pu, pub = ps_u.next()
                k.pe("matmul", [ktm_b, vp_b], [pub], out=pu[pr, 0:129], lhsT=KTM[:, n, h * 64:(h + 1) * 64], rhs=vp[:, h, :],
                     start=True, stop=True)
                tmp, tmp_b = tmpr.next()
                k.dve("tensor_tensor", [pub, c32_b], [tmp_b], out=tmp[pr, :], in0=pu[pr, 0:129], in1=C32[pr, slot, :], op=ALU.add)
                k.dve("tensor_scalar_mul", [tmp_b, el_b], [c32_b], out=C32[pr, slot, :], in0=tmp[pr, :], scalar1=eL[pr, n, col:col + 1])
                k.act("activation", [tmp_b, el_b], [cbf_b], out=Cbf[pr, slot, :], in_=tmp[pr, :], func=ACT.Copy,
                      scale=eL[pr, n, col:col + 1])
            if latent:
                dst = S.HF if d == 0 else S.HB
                k.dma("sp", dst[n * 128:(n + 1) * 128, :], ho[:, :], rd=[ho_b])
    k.barrier()
    ar.release(m0)


LN_EPS = 1e-5
ALPHA = 2.0 ** 0.25


def bcast_load(c, ar, src_row, n, name):
    t = ar.alloc([128, n], F32, name)
    b = Buf(name)
    c.k.dma("sp", t[:, :], src_row.partition_broadcast(128), wr=[b])
    return t, b


def stage_e(c):
    k, ar, I, S, nc, cfg = c.k, c.ar, c.I, c.S, c.nc, c.cfg
    T, NT, NE = cfg.T, cfg.NT, cfg.NE
    m0 = ar.mark()
    evac = Evac(k)
    WO = ar.alloc([128, 8, 1024], BF16, "wo")
    wo_b = Buf("wo")
    wst = sb_ring(ar, 2, [128, 1024], F32, "wost")
    for kk in range(8):
        wt, wb = wst.next()
        k.dma("sp", wt[:, :], I.w_out[kk * 128:(kk + 1) * 128, :], wr=[wb])
        evac(WO[:, kk, :], wt[:, :], [wb], [wo_b])
    WR = ar.alloc([128, 8, NE], F32, "wr")
    wr_b = Buf("wr")
    for a0 in (0, 4):
        k.dma("sp", WR[:, a0:a0 + 4, :], I.w_router[a0 * 128:(a0 + 4) * 128, :].rearrange("(a p) e -> p a e", p=128),
              wr=[wr_b], wr_add=(a0 > 0))
    g1t, g1_b = bcast_load(c, ar, I.ln1_g, 1024, "ln1g")
    b1t, b1_b = bcast_load(c, ar, I.ln1_b, 1024, "ln1b")
    mgt, mg_b = bcast_load(c, ar, I.ml_norm_g, 512, "mlg")
    P1 = ar.alloc([128, 1024], F32, "p1sc2")
    p1_b = Buf("p1")
    k.dve("tensor_scalar_add", [c.MB_b], [p1_b], out=P1[:, :], in0=c.MB[:, 4096:5120], scalar1=1.0)
    G1 = c.MB[:, 2048:3072]
    SH2 = c.MB[:, 3072:4096]
    hfr = sb_ring(ar, 2, [128, 512], F32, "hf")
    hbr = sb_ring(ar, 2, [128, 512], F32, "hb")
    omr = sb_ring(ar, 2, [128, 512], F32, "om")
    xr = sb_ring(ar, 2, [128, 1024], F32, "xe")
    mixr = sb_ring(ar, 2, [128, 8, 128], BF16, "mixT")
    hr = sb_ring(ar, 2, [128, 512], F32, "h")
    sqr = sb_ring(ar, 2, [128, 512], F32, "sq")
    sgr = sb_ring(ar, 2, [128, 512], F32, "sg")
    mlr = sb_ring(ar, 2, [128, 512], BF16, "mlb")
    st4 = sb_ring(ar, 4, [128, 4], F32, "st4")
    st1 = sb_ring(ar, 8, [128, 1], F32, "st1")
    yr = sb_ring(ar, 2, [128, 1024], F32, "y")
    scr = sb_ring(ar, 2, [128, 1024], F32, "scr")
    accr = sb_ring(ar, 2, [128, 1024], F32, "acc")
    h2r = sb_ring(ar, 2, [128, 1024], F32, "h2")
    h2br = sb_ring(ar, 2, [128, 1024], BF16, "h2b")
    h2tr = sb_ring(ar, 2, [128, 8, 128], F32, "h2T")
    lgr = sb_ring(ar, 2, [128, NE], F32, "lg")
    ps_t = Ring([c.ps[0], c.ps[1]])
    ps_m = Ring([c.ps[2], c.ps[3], c.ps[4], c.ps[5]])
    ps_r = Ring([c.ps[6], c.ps[7]])
    for i in range(NT):
        rows = slice(i * 128, (i + 1) * 128)
        hf, hf_b = hfr.next()
        hb, hb_b = hbr.next()
        om, om_b = omr.next()
        xt, x_b = xr.next()
        mixT, mx_b = mixr.next()
        k.dma("sp", hf[:, :], S.HF[rows, :], wr=[hf_b])
        k.dma("sp", hb[:, :], S.HB[rows, :], wr=[hb_b])
        k.dma("sp", om[:, :], S.OM[rows, :], wr=[om_b])
        k.dma("sp", xt[:, :], I.x[rows, :], wr=[x_b])
        k.dma("sp", mixT[:, 0:4, :], S.MIX_T[0:4, :, rows].rearrange("j p t -> p j t"), wr=[mx_b])
        h, h_b = hr.next()
        k.pool("tensor_tensor", [hf_b, hb_b], [h_b], out=h[:, :], in0=hf[:, :], in1=hb[:, :], op=ALU.add)
        h3 = h[:, :].rearrange("p (a v) -> p a v", v=128)
        mu, mu_b = st4.next()
        k.dve("tensor_reduce", [h_b], [mu_b], out=mu[:, :], in_=h3, axis=AX.X, op=ALU.add)
        k.dve("tensor_scalar_mul", [mu_b], [mu_b], out=mu[:, :], in0=mu[:, :], scalar1=1.0 / 128)
        k.dve("tensor_tensor", [h_b, mu_b], [h_b], out=h3, in0=h3, in1=mu[:, :].unsqueeze(2).to_broadcast([128, 4, 128]),
              op=ALU.subtract)
        sq, sq_b = sqr.next()
        k.pool("tensor_tensor", [h_b], [sq_b], out=sq[:, :], in0=h[:, :], in1=h[:, :], op=ALU.mult)
        var, var_b = st4.next()
        k.dve("tensor_reduce", [sq_b], [var_b], out=var[:, :], in_=sq[:, :].rearrange("p (a v) -> p a v", v=128), axis=AX.X,
              op=ALU.add)
        k.dve("tensor_scalar", [var_b], [var_b], out=var[:, :], in0=var[:, :], scalar1=1.0 / 128, scalar2=LN_EPS,
              op0=ALU.mult, op1=ALU.add)
        k.act("activation", [var_b], [var_b], out=var[:, :], in_=var[:, :], func=ACT.Sqrt)
        k.dve("reciprocal", [var_b], [var_b], out=var[:, :], in_=var[:, :])
        k.dve("tensor_tensor", [h_b, var_b], [h_b], out=h3, in0=h3, in1=var[:, :].unsqueeze(2).to_broadcast([128, 4, 128]),
              op=ALU.mult)
        sg, sg_b = sgr.next()
        k.act("activation", [om_b], [sg_b], out=sg[:, :], in_=om[:, :], func=ACT.Sigmoid)
        k.pool("tensor_tensor", [h_b, mg_b], [h_b], out=h[:, :], in0=h[:, :], in1=mgt[:, :], op=ALU.mult)
        mlb, ml_b = mlr.next()
        k.dve("tensor_tensor", [h_b, sg_b], [ml_b], out=mlb[:, :], in0=h[:, :], in1=sg[:, :], op=ALU.mult)
        pt, pb = ps_t.next()
        ptb = pt[:, :].bitcast(BF16)
        for a in range(4):
            k.pe("transpose", [ml_b, c.cmb_b], [pb], out=ptb[:, a * 128:(a + 1) * 128], in_=mlb[:, a * 128:(a + 1) * 128],
                 identity=c.identb)
        evac(mixT[:, 4:8, :], ptb[:, 0:512].rearrange("p (a x) -> p a x", x=128), [pb], [mx_b])
        y, y_b = yr.next()
        for half in range(2):
            pm, pmb = ps_m.next()
            for kk in range(8):
                k.pe("matmul", [mx_b, wo_b], [pmb], out=pm[:, :], lhsT=mixT[:, kk, :], rhs=WO[:, kk, half * 512:(half + 1) * 512],
                     start=(kk == 0), stop=(kk == 7))
            k.dve("tensor_tensor", [pmb, c.MB_b], [y_b], out=y[:, half * 512:(half + 1) * 512], in0=pm[:, :],
                  in1=G1[:, half * 512:(half + 1) * 512], op=ALU.mult)
        k.dve("scalar_tensor_tensor", [x_b, y_b], [y_b], out=y[:, :], in0=xt[:, :], scalar=ALPHA, in1=y[:, :], op0=ALU.mult,
              op1=ALU.add)
        x1, x1_b = layer_norm(c, y, y_b, g1t, g1_b, b1t, b1_b, st1, scr)
        acc, acc_b = accr.next()
        k.act("activation", [x1_b], [acc_b], out=acc[:, :], in_=x1[:, :], func=ACT.Copy, scale=ALPHA)
        k.dma("sp", S.ACC[rows, :], acc[:, :], rd=[acc_b])
        h2, h2_b = h2r.next()
        k.pool("tensor_tensor", [x1_b, p1_b], [h2_b], out=h2[:, :], in0=x1[:, :], in1=P1[:, :], op=ALU.mult)
        k.pool("tensor_tensor", [h2_b, c.MB_b], [h2_b], out=h2[:, :], in0=h2[:, :], in1=SH2, op=ALU.add)
        h2b, h2b_b = h2br.next()
        k.act("activation", [h2_b], [h2b_b], out=h2b[:, :], in_=h2[:, :], func=ACT.Copy)
        k.dma("sp", S.H2[rows, :], h2b[:, :], rd=[h2b_b])
        h2T, h2T_b = h2tr.next()
        for half in range(2):
            pt, pb = ps_t.next()
            for q in range(4):
                ch = half * 4 + q
                k.pe("transpose", [h2_b, c.cm_b], [pb], out=pt[:, q * 128:(q + 1) * 128], in_=h2[:, ch * 128:(ch + 1) * 128],
                     identity=c.ident)
            evac(h2T[:, half * 4:(half + 1) * 4, :], pt[:, :].rearrange("p (a x) -> p a x", x=128), [pb], [h2T_b])
        pr_, prb = ps_r.next()
        for kk in range(8):
            k.pe("matmul", [h2T_b, wr_b], [prb], out=pr_[:, 0:NE], lhsT=h2T[:, kk, :], rhs=WR[:, kk, :], start=(kk == 0),
                 stop=(kk == 7))
        mxv, mxv_b = st1.next()
        k.dve("tensor_reduce", [prb], [mxv_b], out=mxv[:, :], in_=pr_[:, 0:NE], axis=AX.X, op=ALU.max)
        k.dve("tensor_scalar_mul", [mxv_b], [mxv_b], out=mxv[:, :], in0=mxv[:, :], scalar1=-1.0)
        lg, lg_b = lgr.next()
        ssum, ssum_b = st1.next()
        k.act("activation", [prb, mxv_b], [lg_b, ssum_b], out=lg[:, :], in_=pr_[:, 0:NE], func=ACT.Exp, bias=mxv[:, 0:1],
              accum_out=ssum[:, 0:1])
        k.dve("reciprocal", [ssum_b], [ssum_b], out=ssum[:, :], in_=ssum[:, :])
        k.dve("tensor_scalar_mul", [lg_b, ssum_b], [c.aff_b], out=c.AFFt[:, i, :], in0=lg[:, :], scalar1=ssum[:, 0:1])
        k.dma("sp", S.AFF[rows, :], c.AFFt[:, i, :], rd=[c.aff_b])
    k.barrier()
    ar.release(m0)


def layer_norm(c, y, y_b, gt, g_b, bt, b_b, st1, scr):
    k = c.k
    mu, mu_b = st1.next()
    k.dve("tensor_reduce", [y_b], [mu_b], out=mu[:, :], in_=y[:, :], axis=AX.X, op=ALU.add)
    k.dve("tensor_scalar_mul", [mu_b], [mu_b], out=mu[:, :], in0=mu[:, :], scalar1=-1.0 / 1024)
    k.dve("tensor_scalar_add", [y_b, mu_b], [y_b], out=y[:, :], in0=y[:, :], scalar1=mu[:, 0:1])
    sc, sc_b = scr.next()
    ss, ss_b = st1.next()
    k.act("activation", [y_b], [sc_b, ss_b], out=sc[:, :], in_=y[:, :], func=ACT.Square, accum_out=ss[:, 0:1])
    k.dve("tensor_scalar", [ss_b], [ss_b], out=ss[:, :], in0=ss[:, :], scalar1=1.0 / 1024, scalar2=LN_EPS, op0=ALU.mult,
          op1=ALU.add)
    k.act("activation", [ss_b], [ss_b], out=ss[:, :], in_=ss[:, :], func=ACT.Sqrt)
    k.dve("reciprocal", [ss_b], [ss_b], out=ss[:, :], in_=ss[:, :])
    k.dve("scalar_tensor_tensor", [y_b, ss_b, g_b], [y_b], out=y[:, :], in0=y[:, :], scalar=ss[:, 0:1], in1=gt[:, :],
          op0=ALU.mult, op1=ALU.mult)
    k.dve("tensor_tensor", [y_b, b_b], [y_b], out=y[:, :], in0=y[:, :], in1=bt[:, :], op=ALU.add)
    return y, y_b


def stage_f(c):
    k, ar, I, S, nc, cfg = c.k, c.ar, c.I, c.S, c.nc, c.cfg
    T, NT, NE, NSL, CAP = cfg.T, cfg.NT, cfg.NE, cfg.NSL, cfg.CAP
    ar.release(c.mb_mark)
    m0 = ar.mark()
    NN = NT * NE
    A = c.AFFt
    a_b = c.aff_b
    io = ar.alloc([128, 128], F32, "iota")
    io_b = Buf("iota")
    k.dma("sp", io[:, :], I.iota[:, :], wr=[io_b])
    lo = ar.alloc([128, NE], F32, "lo")
    hi = ar.alloc([128, NE], F32, "hi")
    mid = ar.alloc([128, NE], F32, "mid")
    dl = ar.alloc([128, NE], F32, "dl")
    sel = ar.alloc([128, NE], F32, "sel")
    pc = ar.alloc([128, NE], BF16, "pc")
    M = ar.alloc([128, NT, NE], F32, "M")
    lo_b, hi_b, mid_b, dl_b, sel_b, pc_b, M_b = [Buf(n) for n in ("lo", "hi", "mid", "dl", "sel", "pc", "M")]
    k.dve("memset", [], [lo_b], ap=lo[:, :], constant=0.0)
    k.dve("memset", [], [hi_b], ap=hi[:, :], constant=1.0)
    psr = Ring([c.ps[0], c.ps[1]])
    for it in range(30):
        k.dve("tensor_tensor", [lo_b, hi_b], [mid_b], out=mid[:, :], in0=lo[:, :], in1=hi[:, :], op=ALU.add)
        k.dve("tensor_scalar_mul", [mid_b], [mid_b], out=mid[:, :], in0=mid[:, :], scalar1=0.5)
        k.dve("tensor_tensor", [a_b, mid_b], [M_b], out=M[:, :, :], in0=A[:, :, :],
              in1=mid[:, :].unsqueeze(1).to_broadcast([128, NT, NE]), op=ALU.is_gt)
        k.dve("tensor_reduce", [M_b], [dl_b], out=dl[:, :], in_=M[:, :, :].rearrange("p n e -> p e n"), axis=AX.X, op=ALU.add)
        k.dve("tensor_copy", [dl_b], [pc_b], out=pc[:, :], in_=dl[:, :])
        pt, pb = psr.next()
        k.pe("matmul", [pc_b, c.cmb_b], [pb], out=pt[:, 0:NE], lhsT=c.onesb, rhs=pc[:, :], start=True, stop=True)
        k.dve("tensor_single_scalar", [pb], [sel_b], out=sel[:, :], in_=pt[:, 0:NE], scalar=float(CAP), op=ALU.is_ge)
        k.dve("tensor_tensor", [mid_b, lo_b], [dl_b], out=dl[:, :], in0=mid[:, :], in1=lo[:, :], op=ALU.subtract)
        k.dve("tensor_tensor", [dl_b, sel_b], [dl_b], out=dl[:, :], in0=dl[:, :], in1=sel[:, :], op=ALU.mult)
        k.dve("tensor_tensor", [lo_b, dl_b], [lo_b], out=lo[:, :], in0=lo[:, :], in1=dl[:, :], op=ALU.add)
        k.dve("tensor_tensor", [hi_b, mid_b], [dl_b], out=dl[:, :], in0=hi[:, :], in1=mid[:, :], op=ALU.subtract)
        k.dve("tensor_tensor", [dl_b, sel_b], [dl_b], out=dl[:, :], in0=dl[:, :], in1=sel[:, :], op=ALU.mult)
        k.dve("tensor_tensor", [mid_b, dl_b], [hi_b], out=hi[:, :], in0=mid[:, :], in1=dl[:, :], op=ALU.add)
    if "stopF1" in cfg.debug:
        dump(c, "DBG_LO", lo[:, :], [128, NE], lo_b)
        k.barrier()
        ar.release(m0)
        return
    Mb = ar.alloc([128, NN], BF16, "Mb")
    Mb_b = Buf("Mb")
    k.dve("tensor_tensor", [a_b, lo_b], [Mb_b], out=Mb[:, :].rearrange("p (n e) -> p n e", e=NE), in0=A[:, :, :],
          in1=lo[:, :].unsqueeze(1).to_broadcast([128, NT, NE]), op=ALU.is_gt)
    INCL = ar.alloc([128, NN], F32, "INCL")
    incl_b = Buf("INCL")
    TA = ar.alloc([128, NN], F32, "TA")
    ta_b = Buf("TA")
    TB = ar.alloc([128, NN], F32, "TB")
    tb_b = Buf("TB")
    TOT = ar.alloc([128, NN], F32, "TOT")
    tot_b = Buf("TOT")
    for p0 in range(0, NN, 512):
        w = min(512, NN - p0)
        pt, pb = psr.next()
        k.pe("matmul", [Mb_b, c.cmb_b], [pb], out=pt[:, 0:w], lhsT=c.triFb, rhs=Mb[:, p0:p0 + w], start=True, stop=True)
        k.dve("tensor_copy", [pb], [incl_b], out=INCL[:, p0:p0 + w], in_=pt[:, 0:w])
        pt, pb = psr.next()
        k.pe("matmul", [Mb_b, c.cmb_b], [pb], out=pt[:, 0:w], lhsT=c.onesb, rhs=Mb[:, p0:p0 + w], start=True, stop=True)
        k.dve("tensor_copy", [pb], [tot_b], out=TOT[:, p0:p0 + w], in_=pt[:, 0:w])
        k.dve("tensor_copy", [tot_b], [ta_b], out=TA[:, p0:p0 + w], in_=TOT[:, p0:p0 + w])
    cur, cur_b, oth, oth_b = TA, ta_b, TB, tb_b
    s = 1
    while s < NT:
        sw = s * NE
        k.dve("tensor_copy", [cur_b], [oth_b], out=oth[:, 0:sw], in_=cur[:, 0:sw])
        k.dve("tensor_tensor", [cur_b], [oth_b], out=oth[:, sw:NN], in0=cur[:, sw:NN], in1=cur[:, 0:NN - sw], op=ALU.add)
        cur, cur_b, oth, oth_b = oth, oth_b, cur, cur_b
        s *= 2
    k.dve("tensor_tensor", [cur_b, tot_b], [cur_b], out=cur[:, :], in0=cur[:, :], in1=TOT[:, :], op=ALU.subtract)
    k.dve("tensor_tensor", [incl_b, cur_b], [incl_b], out=INCL[:, :], in0=INCL[:, :], in1=cur[:, :], op=ALU.add)
    if "stopF2" in cfg.debug:
        dump(c, "DBG_INCL", INCL[:, :], [128, NN], incl_b)
        k.barrier()
        ar.release(m0)
        return
    K1 = NSL + 1
    K128 = ar.alloc([128, K1], F32, "K128")
    k128_b = Buf("K128")
    k.dve("tensor_scalar_mul", [io_b], [k128_b], out=K128[:, :], in0=io[:, 0:K1], scalar1=128.0)
    Ge = ar.alloc([128, NN, K1], F32, "Ge")
    ge_b = Buf("Ge")
    k.dve("tensor_tensor", [incl_b, k128_b], [ge_b], out=Ge[:, :, :], in0=INCL[:, :].unsqueeze(2).to_broadcast([128, NN, K1]),
          in1=K128[:, :].unsqueeze(1).to_broadcast([128, NN, K1]), op=ALU.is_ge)
    Bv = ar.alloc([128, NN], F32, "Bv")
    bv_b = Buf("Bv")
    k.dve("tensor_reduce", [ge_b], [bv_b], out=Bv[:, :], in_=Ge[:, :, 1:K1], axis=AX.X, op=ALU.add)
    k.dve("scalar_tensor_tensor", [bv_b, incl_b], [bv_b], out=Bv[:, :], in0=Bv[:, :], scalar=-128.0, in1=INCL[:, :],
          op0=ALU.mult, op1=ALU.add)
    Am = ar.alloc([128, NN, NSL], BF16, "Am")
    am_b = Buf("Am")
    Al = ar.alloc([128, NN, NSL], BF16, "Al")
    al_b = Buf("Al")
    k.dve("tensor_tensor", [ge_b], [am_b], out=Am[:, :, :], in0=Ge[:, :, 0:NSL], in1=Ge[:, :, 1:K1], op=ALU.subtract)
    k.dve("tensor_scalar", [ge_b], [al_b], out=Al[:, :, :], in0=Ge[:, :, 0:NSL], scalar1=-1.0, scalar2=1.0, op0=ALU.mult,
          op1=ALU.add)
    dump(c, "DBG_INCL", INCL[:, :], [128, NN], incl_b)
    if "stopF3" in cfg.debug:
        dump(c, "DBG_BV", Bv[:, :], [128, NN], bv_b)
        k.barrier()
        ar.release(m0)
        return
    thr = sb_ring(ar, 4, [128, 128], BF16, "Th")
    ps4 = Ring([c.ps[2], c.ps[3], c.ps[4], c.ps[5]])
    for e in range(NE):
        pt, pb = ps4.next()
        for n in range(NT):
            th, th_b = thr.next()
            col = n * NE + e
            eng = k.dve
            eng("tensor_scalar", [io_b, bv_b], [th_b], out=th[:, :], in0=io[:, :], scalar1=Bv[:, col:col + 1], scalar2=None,
                op0=ALU.is_ge)
            k.pe("matmul", [th_b, am_b], [pb], out=pt[:, 0:NSL], lhsT=th[:, :], rhs=Am[:, col, :], start=(n == 0), stop=False)
            k.pe("matmul", [c.cmb_b, al_b], [pb], out=pt[:, 0:NSL], lhsT=c.onesb, rhs=Al[:, col, :], start=False,
                 stop=(n == NT - 1))
        k.dve("tensor_scalar_min", [pb], [c.idx_b], out=c.IDX[:, e, :], in0=pt[:, 0:NSL], scalar1=float(T - 1))
    dump(c, "DBG_IDX", c.IDX[:, :, :], [128, NE, NSL], c.idx_b, U32)
    k.barrier()
    ar.release(m0)


def stage_g(c):
    k, ar, I, S, nc, cfg = c.k, c.ar, c.I, c.S, c.nc, c.cfg
    T, NT, NE, NSL, CAP, NFC, DFF = cfg.T, cfg.NT, cfg.NE, cfg.NSL, cfg.CAP, cfg.NFC, cfg.DFF
    m0 = ar.mark()
    evac = Evac(k)
    FG = 2
    NG = NFC // FG
    HW_ = min(int(os.environ.get('KHW', 512)), CAP)
    NH = CAP // HW_
    xer = sb_ring(ar, 1, [128, NSL, 1024], BF16, "XE")
    xtr = sb_ring(ar, 1, [128, 8, CAP], BF16, "XT")
    g16r = sb_ring(ar, 2, [128, NSL, NE], F32, "g16")
    wgs = sb_ring(ar, 2, [128, 8, FG * 128], F32, "wgs")
    wus = sb_ring(ar, 2, [128, 8, FG * 128], F32, "wus")
    wds = sb_ring(ar, 1, [128, FG, 1024], F32, "wds")
    wgb = sb_ring(ar, 2, [128, 8, FG * 128], BF16, "wgb")
    wub = sb_ring(ar, 2, [128, 8, FG * 128], BF16, "wub")
    WD = ar.alloc([128, NFC, 1024], BF16, "WD")
    wd_b = Buf("WD")
    HT = ar.alloc([128, NFC, CAP], BF16, "HT")
    ht_b = Buf("HT")
    sgr = sb_ring(ar, 2, [128, HW_], F32, "sg")
    ysr = sb_ring(ar, 1, [128, 1024], F32, "ys")
    acc_b = Buf("ACCdram")
    ps_t = Ring([c.ps[0], c.ps[1]])
    ps_g = Ring([c.ps[2], c.ps[3]])
    ps_u = Ring([c.ps[4], c.ps[5]])
    ps_d = Ring([c.ps[6], c.ps[7]])
    cast_n = [0]

    def cast(out_ap, in_ap, rd, wr):
        cast_n[0] += 1
        m = cast_n[0] % 3
        if m == 0:
            k.act("activation", rd, wr, out=out_ap, in_=in_ap, func=ACT.Copy)
        elif m == 1:
            k.dve("tensor_copy", rd, wr, out=out_ap, in_=in_ap)
        else:
            k.pool("tensor_copy", rd, wr, out=out_ap, in_=in_ap)

    for e in range(NE):
        XE, xe_b = xer.next()
        XT, xt_b = xtr.next()
        g16, g16_b = g16r.next()
        for kq in range(NSL):
            off = bass.IndirectOffsetOnAxis(ap=c.IDX[:, e, kq:kq + 1], axis=0)
            k.op("pool", ("indirect_dma_start", dict(out=XE[:, kq, :], out_offset=None, in_=S.H2[:, :], in_offset=off)),
                 [c.idx_b], [xe_b], dma=True)
            off2 = bass.IndirectOffsetOnAxis(ap=c.IDX[:, e, kq:kq + 1], axis=0)
            k.op("pool", ("indirect_dma_start", dict(out=g16[:, kq, :], out_offset=None, in_=S.AFF[:, :], in_offset=off2)),
                 [c.idx_b], [g16_b], dma=True)
        for kq in range(NSL):
            for half in range(2):
                pt, pb = ps_t.next()
                ptb = pt[:, :].bitcast(BF16)
                for q in range(4):
                    ch = half * 4 + q
                    k.pe("transpose", [xe_b, c.cmb_b], [pb], out=ptb[:, q * 128:(q + 1) * 128],
                         in_=XE[:, kq, ch * 128:(ch + 1) * 128], identity=c.identb)
                evac(XT[:, half * 4:(half + 1) * 4, kq * 128:(kq + 1) * 128],
                     ptb[:, 0:512].rearrange("p (a x) -> p a x", x=128), [pb], [xt_b])
        for g in range(NG):
            f0 = g * FG * 128
            ws, ws_b = wgs.next()
            for a0 in (0, 4):
                k.dma("sp", ws[:, a0:a0 + 4, :], I.weg[e, a0 * 128:(a0 + 4) * 128, f0:f0 + FG * 128].rearrange(
                    "(a p) f -> p a f", p=128), wr=[ws_b], wr_add=(a0 > 0))
            wg, wg_b = wgb.next()
            cast(wg[:, :, :], ws[:, :, :], [ws_b], [wg_b])
            ws2, ws2_b = wus.next()
            for a0 in (0, 4):
                k.dma("sp", ws2[:, a0:a0 + 4, :], I.weu[e, a0 * 128:(a0 + 4) * 128, f0:f0 + FG * 128].rearrange(
                    "(a p) f -> p a f", p=128), wr=[ws2_b], wr_add=(a0 > 0))
            wu, wu_b = wub.next()
            cast(wu[:, :, :], ws2[:, :, :], [ws2_b], [wu_b])
            ws3, ws3_b = wds.next()
            k.dma("sp", ws3[:, :, :], I.wed[e, f0:f0 + FG * 128, :].rearrange("(a p) d -> p a d", p=128), wr=[ws3_b])
            cast(WD[:, g * FG:(g + 1) * FG, :], ws3[:, :, :], [ws3_b], [wd_b])
            for fl in range(FG):
                fc = g * FG + fl
                for hf in range(NH):
                    sl = slice(hf * HW_, (hf + 1) * HW_)
                    pg, pgb = ps_g.next()
                    pu, pub = ps_u.next()
                    for kk in range(8):
                        k.pe("matmul", [wg_b, xt_b], [pgb], out=pg[:, 0:HW_], lhsT=wg[:, kk, fl * 128:(fl + 1) * 128],
                             rhs=XT[:, kk, sl], start=(kk == 0), stop=(kk == 7))
                    for kk in range(8):
                        k.pe("matmul", [wu_b, xt_b], [pub], out=pu[:, 0:HW_], lhsT=wu[:, kk, fl * 128:(fl + 1) * 128],
                             rhs=XT[:, kk, sl], start=(kk == 0), stop=(kk == 7))
                    sg, sg_b = sgr.next()
                    k.act("activation", [pgb], [sg_b], out=sg[:, :], in_=pg[:, 0:HW_], func=ACT.Silu)
                    k.dve("tensor_tensor", [sg_b, pub], [ht_b], out=HT[:, fc, sl], in0=sg[:, :], in1=pu[:, 0:HW_], op=ALU.mult)
        for kq in range(NSL):
            ys, ys_b = ysr.next()
            for dh in range(2):
                pd, pdb = ps_d.next()
                for fc in range(NFC):
                    k.pe("matmul", [ht_b, wd_b], [pdb], out=pd[:, :], lhsT=HT[:, fc, kq * 128:(kq + 1) * 128],
                         rhs=WD[:, fc, dh * 512:(dh + 1) * 512], start=(fc == 0), stop=(fc == NFC - 1))
                k.dve("scalar_tensor_tensor", [pdb, g16_b, c.g2_b], [ys_b], out=ys[:, dh * 512:(dh + 1) * 512], in0=pd[:, :],
                      scalar=g16[:, kq, e:e + 1], in1=c.G2t[:, dh * 512:(dh + 1) * 512], op0=ALU.mult, op1=ALU.mult)
            off = bass.IndirectOffsetOnAxis(ap=c.IDX[:, e, kq:kq + 1], axis=0)
            k.op("pool", ("indirect_dma_start", dict(out=S.ACC[:, :], out_offset=off, in_=ys[:, :], in_offset=None,
                                                     compute_op=ALU.add)),
                 [ys_b, c.idx_b], [acc_b], dma=True)
    k.barrier()
    ar.release(m0)


def stage_h(c):
    k, ar, I, S, nc, cfg = c.k, c.ar, c.I, c.S, c.nc, c.cfg
    NT = cfg.NT
    m0 = ar.mark()
    g2t, g2_b = bcast_load(c, ar, I.ln2_g, 1024, "ln2g")
    b2t, b2_b = bcast_load(c, ar, I.ln2_b, 1024, "ln2b")
    yr = sb_ring(ar, 3, [128, 1024], F32, "yh")
    scr = sb_ring(ar, 2, [128, 1024], F32, "scrh")
    st1 = sb_ring(ar, 8, [128, 1], F32, "st1h")
    for i in range(NT):
        rows = slice(i * 128, (i + 1) * 128)
        y, y_b = yr.next()
        k.dma("sp", y[:, :], S.ACC[rows, :], wr=[y_b])
        layer_norm(c, y, y_b, g2t, g2_b, b2t, b2_b, st1, scr)
        k.dma("sp", c.out[rows, :], y[:, :], rd=[y_b])
    k.barrier()
    ar.release(m0)


_CONST_CACHE = {}


def _consts(T):
    if T in _CONST_CACHE:
        return _CONST_CACHE[T]
    cos, sin = _rope_tables(T)
    ident = np.eye(128, dtype=np.float32)
    perm = _perm_matrix()
    s = np.arange(128)[:, None]
    t = np.arange(128)[None, :]
    triF = (s <= t).astype(np.float32)
    triB = (s >= t).astype(np.float32)
    ones = np.ones((128, 128), np.float32)
    cmat = np.stack([ident, perm, triF, triB, ones]).astype(np.float32)
    iota = np.tile(np.arange(128, dtype=np.float32)[None, :], (128, 1))
    _CONST_CACHE[T] = dict(ropec=cos, ropes=sin, cmat=cmat, iota=iota)
    return _CONST_CACHE[T]


def prep_shared(cfg, inp):
    f = lambda a: np.ascontiguousarray(np.asarray(a, dtype=np.float32))
    sh = {}
    sh["w_ada"] = f(inp["w_ada"][0])
    sh["b_ada"] = f(inp["b_ada"][0]).reshape(1, -1)
    sh["w_in"] = f(inp["w_in"][0])
    sh["b_gate"] = f(inp["b_gate"][0]).reshape(1, 16)
    cq = f(inp["conv_qk"][0])
    sh["convw"] = np.ascontiguousarray(cq.T.reshape(4, 128, 5).transpose(1, 0, 2))
    sh["nabias"] = _na_bias_layout(f(inp["na_rel_bias"][0]))
    sh["ml_norm_g"] = f(inp["ml_norm_g"][0]).reshape(1, -1)
    sh["w_out"] = f(inp["w_out"][0])
    sh["ln1_g"] = f(inp["ln1_g"][0]).reshape(1, -1)
    sh["ln1_b"] = f(inp["ln1_b"][0]).reshape(1, -1)
    sh["w_router"] = f(inp["w_router"][0])
    sh["weg"] = f(inp["w_expert_gate"][0])
    sh["weu"] = f(inp["w_expert_up"][0])
    sh["wed"] = f(inp["w_expert_down"][0])
    sh["ln2_g"] = f(inp["ln2_g"][0]).reshape(1, -1)
    sh["ln2_b"] = f(inp["ln2_b"][0]).reshape(1, -1)
    sh.update(_consts(cfg.T))
    return sh


def prep_core(cfg, inp, sh, b):
    f = lambda a: np.ascontiguousarray(np.asarray(a, dtype=np.float32))
    m = dict(sh)
    m["x"] = f(inp["x"][b])
    m["ctx"] = f(inp["ctx"][b])
    cv = np.concatenate([f(inp["c"][b]).reshape(8, 128).T, f(inp["c_ctx"]).reshape(8, 128).T], axis=1)
    m["cvec"] = np.ascontiguousarray(cv)
    return m


_PROG_CACHE = {}


def kernel(**inputs):
    cfg = Cfg()
    if "full" not in _PROG_CACHE:
        _PROG_CACHE["full"] = build_program(cfg)
    nc = _PROG_CACHE["full"]
    sh = prep_shared(cfg, inputs)
    B = 8
    in_maps = [prep_core(cfg, inputs, sh, b) for b in range(B)]
    res = run_bass_kernel_spmd(nc, in_maps, core_ids=list(range(B)))
    return np.stack([np.asarray(r["out"], dtype=np.float32) for r in res.results], axis=0)
```

```python
import os
import numpy as np
import ml_dtypes
import concourse.bass as bass
import concourse.mybir as mybir
from concourse.bass_utils import run_bass_kernel_spmd

F32 = mybir.dt.float32
BF16 = mybir.dt.bfloat16
I32 = mybir.dt.int32
U32 = mybir.dt.uint32
ALU = mybir.AluOpType
ACT = mybir.ActivationFunctionType
AX = mybir.AxisListType


class Cfg:
    def __init__(self, T=8192, DFF=2816, NE=16, debug=()):
        self.T = T
        self.DFF = DFF
        self.NE = NE
        self.D = 1024
        self.GW = 64
        self.LC = 256
        self.TX = T + self.LC
        self.NT = T // 128
        self.NTX = self.TX // 128
        self.ROWS = T // 64
        self.CAP = 2 * T // NE
        self.NSL = self.CAP // 128
        self.NFC = DFF // 128
        self.DIN = 3088
        self.debug = set(debug)


class Buf:
    __slots__ = ("name", "w", "r")

    def __init__(self, name=""):
        self.name = name
        self.w = None
        self.r = []


class Prog:
    ENGS = ("pe", "act", "dve", "pool", "sp")

    def __init__(self, nc, stack, n_dma_sems=20):
        self.nc = nc
        self.lists = {e: [] for e in self.ENGS}
        self.esem = {e: stack.enter_context(nc.semaphore("es_" + e)) for e in self.ENGS}
        self.cnt = {e: 0 for e in self.ENGS}
        self.seen = {e: {} for e in self.ENGS}
        self.dsem = {}
        self.dtot = {}
        self.drr = {}
        for q in ("sp", "pool", "act"):
            self.dsem[q] = [stack.enter_context(nc.semaphore("ds_%s%d" % (q, i))) for i in range(n_dma_sems)]
            self.dtot[q] = [0] * n_dma_sems
            self.drr[q] = 0
        self.nops = 0

    def _need(self, eng, tok, waits):
        if tok is None:
            return
        kind, key, val = tok
        sk = (kind, key)
        if self.seen[eng].get(sk, 0) >= val:
            return
        if waits.get(sk, 0) < val:
            waits[sk] = val

    def _sem(self, sk):
        kind, key = sk
        if kind == "E":
            return self.esem[key]
        return self.dsem[key[0]][key[1]]

    def op(self, eng, fn, rd=(), wr=(), sig=True, dma=False, acc=False, wr_add=False):
        waits = {}
        for b in rd:
            for t in (b.w or ()):
                if t[0] == "E" and t[1] == eng and eng == "pe":
                    continue
                self._need(eng, t, waits)
        for b in wr:
            if not wr_add:
                for t in (b.w or ()):
                    if not (t[0] == "E" and t[1] == eng):
                        self._need(eng, t, waits)
            for t in b.r:
                if not (t[0] == "E" and t[1] == eng and not dma):
                    self._need(eng, t, waits)
        lst = self.lists[eng]
        if dma:
            q = eng
            i = self.drr[q]
            self.drr[q] = (i + 1) % len(self.dsem[q])
            if self.dtot[q][i] > 0:
                self._need(eng, ("D", (q, i), self.dtot[q][i]), waits)
            self.dtot[q][i] += 16
            tok = ("D", (q, i), self.dtot[q][i])
            for sk, v in waits.items():
                lst.append(("w", self._sem(sk), v))
                self.seen[eng][sk] = v
            lst.append(("o", fn, self.dsem[q][i], 16))
        else:
            for sk, v in waits.items():
                lst.append(("w", self._sem(sk), v))
                self.seen[eng][sk] = v
            if True:
                self.cnt[eng] += 1
                tok = ("E", eng, self.cnt[eng])
                lst.append(("o", fn, self.esem[eng], 1))
            else:
                tok = ("E", eng, self.cnt[eng] + 1)
                lst.append(("o", fn, None, 0))
        for b in rd:
            b.r.append(tok)
            if len(b.r) > 24:
                b.r = b.r[-24:] if False else b.r
        for b in wr:
            if wr_add and b.w:
                b.w = b.w + [tok]
            else:
                b.w = [tok]
                b.r = []
        self.nops += 1
        return tok

    def pe(self, meth, rd=(), wr=(), **kw):
        return self.op("pe", (meth, kw), rd, wr)

    def act(self, meth, rd=(), wr=(), **kw):
        return self.op("act", (meth, kw), rd, wr)

    def dve(self, meth, rd=(), wr=(), **kw):
        return self.op("dve", (meth, kw), rd, wr)

    def pool(self, meth, rd=(), wr=(), **kw):
        return self.op("pool", (meth, kw), rd, wr)

    def dma(self, q, out, in_, rd=(), wr=(), wr_add=False, **kw):
        return self.op(q, ("dma_start", dict(out=out, in_=in_, **kw)), rd, wr, dma=True, wr_add=wr_add)

    def barrier(self, bufs=()):
        toks = []
        for e in self.ENGS:
            if self.cnt[e] > 0:
                toks.append(("E", e, self.cnt[e]))
        for q in self.dsem:
            for i, t in enumerate(self.dtot[q]):
                if t > 0:
                    toks.append(("D", (q, i), t))
        for e in self.ENGS:
            for (kind, key, val) in toks:
                sk = (kind, key)
                if kind == "E" and key == e:
                    continue
                if self.seen[e].get(sk, 0) >= val:
                    continue
                self.lists[e].append(("w", self._sem(sk), val))
                self.seen[e][sk] = val
        for b in bufs:
            b.w = None
            b.r = []

    def finish(self):
        nc = self.nc
        self.barrier()
        lists = self.lists

        def replay(engine, items):
            for it in items:
                if it[0] == "w":
                    engine.wait_ge(it[1], it[2])
                else:
                    f = it[1]
                    ins = getattr(engine, f[0])(**f[1]) if isinstance(f, tuple) else f(engine)
                    if it[2] is not None:
                        ins.then_inc(it[2], it[3])

        with nc.Block() as block:
            @block.tensor
            def _(e):
                replay(e, lists["pe"])

            @block.scalar
            def _(e):
                replay(e, lists["act"])

            @block.vector
            def _(e):
                replay(e, lists["dve"])

            @block.gpsimd
            def _(e):
                replay(e, lists["pool"])

            @block.sync
            def _(e):
                replay(e, lists["sp"])


class Arena:
    def __init__(self, nc):
        self.nc = nc
        self.base = (nc.sbuf_base + 63) // 64 * 64
        self.top = nc.sbuf_top
        self.cur = self.base
        self.n = 0

    def alloc(self, shape, dtype, name="t"):
        esz = {F32: 4, BF16: 2, I32: 4, U32: 4}[dtype]
        per = int(np.prod(shape[1:])) * esz
        per = (per + 63) // 64 * 64
        off = self.cur
        assert off + per <= self.top, "SBUF arena overflow: %s %s need %d have %d" % (name, shape, per, self.top - off)
        self.cur += per
        self.n += 1
        return self.nc.alloc_sbuf_tensor_at("%s_%d" % (name, self.n), list(shape), dtype, offset=off)

    def mark(self):
        return self.cur

    def release(self, m):
        self.cur = m


def _na_bias_layout(bias_table):
    H = bias_table.shape[0]
    NEG = np.float32(-30000.0)
    out = np.full((H, 3, 6, 128, 256), NEG, np.float32)
    c = np.arange(64)
    cs = np.clip(c - 8, 0, 48)
    for gt in range(3):
        for ql in range(4):
            for kl in range(12 if gt == 1 else 8):
                if gt == 0:
                    r, kr, rs = ql, kl, 0
                elif gt == 1:
                    r, kr, rs = 4 + ql, kl, ql
                else:
                    r, kr, rs = 4 + ql, kl, 0
                if not (rs <= kr < rs + 8):
                    continue
                dr = kr - r + 7
                for qc in range(64):
                    kcs = np.arange(cs[qc], cs[qc] + 16)
                    dc = kcs - qc + 15
                    tile = kr // 2
                    keyp = (kr % 2) * 64 + kcs
                    out[:, gt, tile, keyp, ql * 64 + qc] = bias_table[:, dr, :][:, dc]
    return out


def _rope_tables(T):
    nf = 16
    inv = (1.0 / (10000.0 ** (np.arange(nf, dtype=np.float32) / nf))).astype(np.float32)
    t = np.arange(T)
    rows = (t // 64).astype(np.float32)
    cols = (t % 64).astype(np.float32)
    cos = np.zeros((128, T), np.float32)
    sin = np.zeros((128, T), np.float32)
    for p in range(128):
        d = p % 64
        pos = rows if d < 32 else cols
        j = d % 16
        ang = (pos * inv[j]).astype(np.float32)
        cos[p] = np.cos(ang)
        sgn = -1.0 if (d % 32) < 16 else 1.0
        sin[p] = sgn * np.sin(ang)
    return cos, sin


def _perm_matrix():
    P = np.zeros((128, 128), np.float32)
    for m in range(128):
        d = m % 64
        base = m - d
        pd = d + 16 if (d % 32) < 16 else d - 16
        P[base + pd, m] = 1.0
    return P


class Ring:
    def __init__(self, items):
        self.items = items
        self.i = 0

    def next(self):
        it = self.items[self.i]
        self.i = (self.i + 1) % len(self.items)
        return it


def sb_ring(ar, n, shape, dtype, name):
    return Ring([(ar.alloc(shape, dtype, name), Buf(name)) for _ in range(n)])


class Ctx:
    pass


def build_program(cfg):
    from contextlib import ExitStack
    nc = bass.Bass("TRN2", target_bir_lowering=False)
    T, TX, NT, NTX, D, DFF, NE = cfg.T, cfg.TX, cfg.NT, cfg.NTX, cfg.D, cfg.DFF, cfg.NE
    c = Ctx()
    c.cfg = cfg
    c.nc = nc

    def din(name, shape, dt=F32):
        return nc.dram_tensor(name, list(shape), dt, kind="ExternalInput").ap()

    def dscr(name, shape, dt=F32):
        kind = "ExternalOutput" if name in cfg.debug else "Internal"
        return nc.dram_tensor(name, list(shape), dt, kind=kind).ap()

    I = Ctx()
    c.I = I
    I.x = din("x", [T, D])
    I.ctx = din("ctx", [cfg.LC, D])
    I.cvec = din("cvec", [128, 16])
    I.w_ada = din("w_ada", [D, 6 * D])
    I.b_ada = din("b_ada", [1, 6 * D])
    I.w_in = din("w_in", [D, cfg.DIN])
    I.b_gate = din("b_gate", [1, 16])
    I.convw = din("convw", [128, 4, 5])
    I.nabias = din("nabias", [8, 3, 6, 128, 256])
    I.ml_norm_g = din("ml_norm_g", [1, 512])
    I.w_out = din("w_out", [D, D])
    I.ln1_g = din("ln1_g", [1, D])
    I.ln1_b = din("ln1_b", [1, D])
    I.w_router = din("w_router", [D, NE])
    I.weg = din("weg", [NE, D, DFF])
    I.weu = din("weu", [NE, D, DFF])
    I.wed = din("wed", [NE, DFF, D])
    I.ln2_g = din("ln2_g", [1, D])
    I.ln2_b = din("ln2_b", [1, D])
    I.ropec = din("ropec", [128, T])
    I.ropes = din("ropes", [128, T])
    I.cmat = din("cmat", [5, 128, 128])
    I.iota = din("iota", [128, 128])
    out = nc.dram_tensor("out", [T, D], F32, kind="ExternalOutput").ap()
    c.out = out

    S = Ctx()
    c.S = S
    S.QA_T = dscr("QA_T", [4, 128, T], BF16)
    S.KA_T = dscr("KA_T", [4, 128, TX], BF16)
    S.VA = dscr("VA", [TX, 512], BF16)
    S.QM_T = dscr("QM_T", [2, 128, T])
    S.KM_T = dscr("KM_T", [2, 128, TX])
    S.VM = dscr("VM", [TX, 512], BF16)
    S.OM = dscr("OM", [T, 512])
    S.GT = dscr("GT", [TX, 16])
    S.MIX_T = dscr("MIX_T", [8, 128, T], BF16)
    S.HF = dscr("HF", [T, 512])
    S.HB = dscr("HB", [T, 512])
    S.ACC = dscr("ACC", [T, D])
    S.H2 = dscr("H2", [T, D], BF16)
    S.AFF = dscr("AFF", [T, NE])

    with ExitStack() as stack:
        k = Prog(nc, stack)
        c.k = k
        ar = Arena(nc)
        c.ar = ar
        c.ps = [(nc.alloc_psum_tensor("ps%d" % i, [128, 512], F32), Buf("ps%d" % i)) for i in range(8)]
        c.cm = ar.alloc([128, 5, 128], F32, "cmat")
        c.cm_b = Buf("cmat")
        k.dma("sp", c.cm[:, :, :], I.cmat.rearrange("a p q -> p a q"), wr=[c.cm_b])
        c.cmb = ar.alloc([128, 5, 128], BF16, "cmatb")
        c.cmb_b = Buf("cmatb")
        k.dve("tensor_copy", [c.cm_b], [c.cmb_b], out=c.cmb[:, :, :], in_=c.cm[:, :, :])
        c.ident, c.perm, c.triF, c.triB, c.ones = [c.cm[:, i, :] for i in range(5)]
        c.identb, c.permb, c.triFb, c.triBb, c.onesb = [c.cmb[:, i, :] for i in range(5)]

        c.modp = ar.alloc([128, 4, 8], F32, "modp")
        c.modp_b = Buf("modp")
        c.G2t = ar.alloc([128, 1024], F32, "G2t")
        c.g2_b = Buf("G2t")
        c.AFFt = ar.alloc([128, cfg.NT, cfg.NE], F32, "AFFt")
        c.aff_b = Buf("AFFt")
        c.IDX = ar.alloc([128, cfg.NE, cfg.NSL], U32, "IDX")
        c.idx_b = Buf("IDX")
        c.mb_mark = ar.mark()
        for nm, fn in (("A", stage_a), ("B", stage_b), ("C", stage_c), ("D", stage_d), ("E", stage_e),
                       ("F", stage_f), ("G", stage_g), ("H", stage_h)):
            fn(c)
            if any(d_.startswith("stop" + nm) for d_ in cfg.debug):
                break
        k.finish()
    return nc


def dump(c, name, ap, shape, buf, dt=F32):
    if name in c.cfg.debug:
        d = c.nc.dram_tensor(name, list(shape), dt, kind="ExternalOutput").ap()
        c.k.dma("sp", d, ap, rd=[buf])


class Evac:
    def __init__(self, k):
        self.k = k
        self.n = 0

    def __call__(self, out_ap, in_ap, rd, wr, scale=None, eng=None):
        k = self.k
        self.n += 1
        use_act = (self.n % 2 == 0) if eng is None else (eng == "act")
        if use_act:
            if scale is None:
                k.act("activation", rd, wr, out=out_ap, in_=in_ap, func=ACT.Copy)
            else:
                k.act("activation", rd, wr, out=out_ap, in_=in_ap, func=ACT.Copy, scale=scale)
        else:
            if scale is None:
                k.dve("tensor_copy", rd, wr, out=out_ap, in_=in_ap)
            else:
                k.dve("tensor_scalar_mul", rd, wr, out=out_ap, in0=in_ap, scalar1=scale)


def stage_a(c):
    k, ar, I, nc = c.k, c.ar, c.I, c.nc
    D = 1024
    c.MB = ar.alloc([128, 6 * D], F32, "MB")
    c.MB_b = Buf("MB")
    m0 = ar.mark()
    MBC = ar.alloc([128, 2 * D], F32, "MBC")
    MBC_b = Buf("MBC")
    cv = ar.alloc([128, 16], F32, "cv")
    cv_b = Buf()
    sv = ar.alloc([128, 16], F32, "sv")
    sv_b = Buf()
    SR = ar.alloc([128, 16, 128], F32, "SR")
    SR_b = Buf()
    bab = ar.alloc([128, 6 * D], F32, "bab")
    bab_b = Buf()
    wring = sb_ring(ar, 3, [128, 2048], F32, "wada")
    k.dma("sp", cv[:, :], I.cvec[:, :], wr=[cv_b])
    k.dma("sp", bab[:, :], I.b_ada.partition_broadcast(128), wr=[bab_b])
    k.act("activation", [cv_b], [sv_b], out=sv[:, :], in_=cv[:, :], func=ACT.Silu)
    k.dve("tensor_copy", [sv_b], [SR_b], out=SR[:, :, :], in_=sv[:, :].unsqueeze(2).to_broadcast([128, 16, 128]))
    for piece in range(3):
        nb = 8 if piece == 0 else 4
        for kk in range(8):
            wt, wb = wring.next()
            k.dma("sp", wt[:, :], I.w_ada[kk * 128:(kk + 1) * 128, piece * 2048:(piece + 1) * 2048], wr=[wb])
            for j in range(nb):
                pt, pb = c.ps[j]
                lhs = SR[:, kk, :] if j < 4 else SR[:, 8 + kk, :]
                jj = j % 4
                k.pe("matmul", [SR_b, wb], [pb], out=pt[:, :], lhsT=lhs, rhs=wt[:, jj * 512:(jj + 1) * 512],
                     start=(kk == 0), stop=(kk == 7))
        for j in range(nb):
            pt, pb = c.ps[j]
            jj = j % 4
            col = piece * 2048 + jj * 512
            if j < 4:
                k.dve("tensor_tensor", [pb, bab_b], [c.MB_b], out=c.MB[:, col:col + 512], in0=pt[:, :],
                      in1=bab[:, col:col + 512], op=ALU.add)
            else:
                k.dve("tensor_tensor", [pb, bab_b], [MBC_b], out=MBC[:, col:col + 512], in0=pt[:, :],
                      in1=bab[:, col:col + 512], op=ALU.add)
    srcs = [(c.MB, c.MB_b, 1024, 0, 1.0), (c.MB, c.MB_b, 0, 1, 0.0), (MBC, MBC_b, 1024, 2, 1.0), (MBC, MBC_b, 0, 3, 0.0)]
    n = 0
    for (src, sb, off, slot, addc) in srcs:
        for half in range(2):
            pt, pb = c.ps[n % 8]
            n += 1
            for q in range(4):
                ch = half * 4 + q
                k.pe("transpose", [sb, c.cm_b], [pb], out=pt[:, q * 128:(q + 1) * 128],
                     in_=src[:, off + ch * 128: off + (ch + 1) * 128], identity=c.ident)
            k.dve("tensor_scalar_add", [pb], [c.modp_b], out=c.modp[:, slot, half * 4:(half + 1) * 4],
                  in0=pt[:, :].rearrange("p (q t) -> p q t", t=128)[:, :, 0], scalar1=addc)
    k.dve("tensor_copy", [c.MB_b], [c.g2_b], out=c.G2t[:, :], in_=c.MB[:, 5120:6144])
    dump(c, "MB", c.MB[:, :], [128, 6144], c.MB_b)
    dump(c, "MODP", c.modp[:, :, :], [128, 4, 8], c.modp_b)
    k.barrier()
    ar.release(m0)


def stage_b(c):
    k, ar, I, S, nc, cfg = c.k, c.ar, c.I, c.S, c.nc, c.cfg
    T, TX, NT, NTX = cfg.T, cfg.TX, cfg.NT, cfg.NTX
    m0 = ar.mark()
    W = ar.alloc([128, 8, cfg.DIN], BF16, "win")
    W_b = Buf("win")
    wst = sb_ring(ar, 2, [128, cfg.DIN], F32, "winst")
    evac = Evac(k)
    for kk in range(8):
        wt, wb = wst.next()
        k.dma("sp", wt[:, :], I.w_in[kk * 128:(kk + 1) * 128, :], wr=[wb])
        evac(W[:, kk, :], wt[:, :], [wb], [W_b])
    xring = sb_ring(ar, 3, [128, 1024], F32, "xin")
    xmring = sb_ring(ar, 2, [128, 8, 512], BF16, "xm")
    st_bf = sb_ring(ar, 4, [128, 512], BF16, "stbf")
    st_f = sb_ring(ar, 4, [128, 512], F32, "stf")
    st_g = sb_ring(ar, 3, [128, 16], F32, "stg")
    ps_tp = Ring([c.ps[0], c.ps[1]])
    ps_tm = Ring([c.ps[2], c.ps[3], c.ps[4]])
    ps_fm = Ring([c.ps[5], c.ps[6], c.ps[7]])
    ngroups = (NTX + 3) // 4
    for g in range(ngroups):
        tiles = list(range(4 * g, min(4 * g + 4, NTX)))
        ntok = 128 * len(tiles)
        xm, xm_b = xmring.next()
        is_ctx = tiles[0] >= NT
        sl_sc, sl_sh = (2, 3) if is_ctx else (0, 1)
        for ti, i in enumerate(tiles):
            xt, xb = xring.next()
            src = I.ctx[(i - NT) * 128:(i - NT + 1) * 128, :] if is_ctx else I.x[i * 128:(i + 1) * 128, :]
            k.dma("sp", xt[:, :], src, wr=[xb])
            for half in range(2):
                pt, pb = ps_tp.next()
                for q in range(4):
                    ch = half * 4 + q
                    k.pe("transpose", [xb, c.cm_b], [pb], out=pt[:, q * 128:(q + 1) * 128],
                         in_=xt[:, ch * 128:(ch + 1) * 128], identity=c.ident)
                for q in range(4):
                    ch = half * 4 + q
                    o_ap = xm[:, ch, ti * 128:(ti + 1) * 128]
                    i_ap = pt[:, q * 128:(q + 1) * 128]
                    s1 = c.modp[:, sl_sc, ch:ch + 1]
                    s2 = c.modp[:, sl_sh, ch:ch + 1]
                    if q % 2 == 0:
                        k.dve("tensor_scalar", [pb, c.modp_b], [xm_b], out=o_ap, in0=i_ap, scalar1=s1, scalar2=s2,
                              op0=ALU.mult, op1=ALU.add)
                    else:
                        k.act("activation", [pb, c.modp_b], [xm_b], out=o_ap, in_=i_ap, func=ACT.Identity,
                              bias=s2, scale=s1)
            tsl = slice(ti * 128, (ti + 1) * 128)
            rows = slice(i * 128, (i + 1) * 128)
            for (c0, ncol, kind) in ((1024, 512, "va"), (2048, 512, "vm"), (2560, 512, "om"), (3072, 16, "gt")):
                if kind == "om" and is_ctx:
                    continue
                pt, pb = ps_tm.next()
                for kk in range(8):
                    k.pe("matmul", [xm_b, W_b], [pb], out=pt[:, 0:ncol], lhsT=xm[:, kk, tsl],
                         rhs=W[:, kk, c0:c0 + ncol], start=(kk == 0), stop=(kk == 7))
                if kind in ("va", "vm"):
                    st, stb = st_bf.next()
                    evac(st[:, :], pt[:, :], [pb], [stb])
                    dst = S.VA if kind == "va" else S.VM
                    k.dma("sp", dst[rows, :], st[:, :], rd=[stb])
                elif kind == "om":
                    st, stb = st_f.next()
                    evac(st[:, :], pt[:, :], [pb], [stb])
                    k.dma("sp", S.OM[rows, :], st[:, :], rd=[stb])
                else:
                    st, stb = st_g.next()
                    evac(st[:, :], pt[:, 0:16], [pb], [stb])
                    k.dma("sp", S.GT[rows, :], st[:, :], rd=[stb])
        tok0 = tiles[0] * 128
        for ci in range(12):
            kind = ("qa", "ka", "qm", "km")[0 if ci < 4 else 1 if ci < 8 else 2 if ci < 10 else 3]
            if is_ctx and kind in ("qa", "qm"):
                continue
            c0 = {"qa": 0, "ka": 512, "qm": 1536, "km": 1792}[kind]
            j = ci if ci < 4 else ci - 4 if ci < 8 else ci - 8 if ci < 10 else ci - 10
            pt, pb = ps_fm.next()
            for kk in range(8):
                k.pe("matmul", [xm_b, W_b], [pb], out=pt[:, 0:ntok], lhsT=W[:, kk, c0 + j * 128:c0 + (j + 1) * 128],
                     rhs=xm[:, kk, 0:ntok], start=(kk == 0), stop=(kk == 7))
            if kind in ("qa", "ka"):
                st, stb = st_bf.next()
                evac(st[:, 0:ntok], pt[:, 0:ntok], [pb], [stb], scale=(0.125 if kind == "qa" else None))
                dst = S.QA_T if kind == "qa" else S.KA_T
                k.dma("sp", dst[j, :, tok0:tok0 + ntok], st[:, 0:ntok], rd=[stb])
            else:
                st, stb = st_f.next()
                evac(st[:, 0:ntok], pt[:, 0:ntok], [pb], [stb])
                dst = S.QM_T if kind == "qm" else S.KM_T
                k.dma("sp", dst[j, :, tok0:tok0 + ntok], st[:, 0:ntok], rd=[stb])
    k.barrier()
    ar.release(m0)


def stage_c(c):
    k, ar, I, S, nc, cfg = c.k, c.ar, c.I, c.S, c.nc, c.cfg
    T, TX, NT, NTX = cfg.T, cfg.TX, cfg.NT, cfg.NTX
    G = cfg.ROWS // 4
    m0 = ar.mark()
    qring = sb_ring(ar, 2, [128, T], BF16, "qT")
    kring = sb_ring(ar, 2, [128, TX], BF16, "kT")
    vring = sb_ring(ar, 2, [128, NTX, 128], BF16, "Vp")
    bias = [(ar.alloc([128, 3, 6, 256], F32, "nab"), Buf("nab")) for _ in range(2)]
    ptring = sb_ring(ar, 3, [128, 8, 256], BF16, "PT")
    tmpring = sb_ring(ar, 3, [128, 256], F32, "stmp")
    rdring = sb_ring(ar, 2, [128, 256], F32, "rden")
    attring = sb_ring(ar, 2, [128, 256], BF16, "attT")
    ps_s = Ring([c.ps[i] for i in range(5)])
    ps_o = Ring([c.ps[5], c.ps[6], c.ps[7]])
    for j in range(4):
        qT, q_b = qring.next()
        kT, k_b = kring.next()
        V, v_b = vring.next()
        k.dma("sp", qT[:, :], S.QA_T[j], wr=[q_b])
        k.dma("sp", kT[:, :], S.KA_T[j], wr=[k_b])
        for n0 in range(0, NTX, 4):
            n1 = min(NTX, n0 + 4)
            k.dma("sp", V[:, n0:n1, :], S.VA[n0 * 128:n1 * 128, j * 128:(j + 1) * 128].rearrange("(n p) c -> p n c", p=128),
                  wr=[v_b], wr_add=(n0 > 0))
        for hh in range(2):
            first = True
            for t_ in range(3):
                for a0 in (0, 3):
                    k.dma("sp", bias[hh][0][:, t_, a0:a0 + 3, :], I.nabias[2 * j + hh, t_, a0:a0 + 3].rearrange("a p q -> p a q"),
                          wr=[bias[hh][1]], wr_add=(not first))
                    first = False
        for g in range(G):
            q0 = g * 256
            if g == 0:
                gt, t0, nl = 0, 0, 4
            elif g == G - 1:
                gt, t0, nl = 2, NT - 4, 4
            else:
                gt, t0, nl = 1, 2 * g - 2, 6
            tiles = [t0 + a for a in range(nl)] + [NT, NT + 1]
            att, att_b = attring.next()
            for hh in range(2):
                pr = slice(hh * 64, hh * 64 + 64)
                bt, bb = bias[hh]
                PT, pt_b = ptring.next()
                for a, tile in enumerate(tiles):
                    pst, psb = ps_s.next()
                    k.pe("matmul", [k_b, q_b], [psb], out=pst[:, 0:256], lhsT=kT[pr, tile * 128:(tile + 1) * 128],
                         rhs=qT[pr, q0:q0 + 256], start=True, stop=True)
                    if a < nl:
                        tmp, tmp_b = tmpring.next()
                        k.dve("tensor_tensor", [psb, bb], [tmp_b], out=tmp[:, :], in0=pst[:, 0:256],
                              in1=bt[:, gt, a, :], op=ALU.add)
                        k.act("activation", [tmp_b], [pt_b], out=PT[:, a, :], in_=tmp[:, :], func=ACT.Exp)
                    else:
                        k.act("activation", [psb], [pt_b], out=PT[:, a, :], in_=pst[:, 0:256], func=ACT.Exp)
                po, pob = ps_o.next()
                na = len(tiles)
                for a, tile in enumerate(tiles):
                    k.pe("matmul", [v_b, pt_b], [pob], out=po[pr, 0:256], lhsT=V[:, tile, pr], rhs=PT[:, a, :],
                         start=(a == 0), stop=(a == na - 1))
                for a, tile in enumerate(tiles):
                    k.pe("matmul", [c.cmb_b, pt_b], [pob], out=po[pr, 256:512], lhsT=c.onesb[:, pr], rhs=PT[:, a, :],
                         start=(a == 0), stop=(a == na - 1))
                rd, rd_b = rdring.next()
                k.dve("reciprocal", [pob], [rd_b], out=rd[pr, :], in_=po[pr, 256:512])
                k.dve("tensor_tensor", [pob, rd_b], [att_b], out=att[pr, :], in0=po[pr, 0:256], in1=rd[pr, :], op=ALU.mult)
            k.dma("sp", S.MIX_T[j, :, q0:q0 + 256], att[:, :], rd=[att_b])
    k.barrier()
    ar.release(m0)


def stage_d(c):
    k, ar, I, S, nc, cfg = c.k, c.ar, c.I, c.S, c.nc, c.cfg
    T, TX, NT, NTX = cfg.T, cfg.TX, cfg.NT, cfg.NTX
    m0 = ar.mark()
    evac = Evac(k)
    GTt = ar.alloc([128, NTX, 16], F32, "gt")
    g_b = Buf("gt")
    bg = ar.alloc([128, 16], F32, "bg")
    bg_b = Buf("bg")
    for n0 in range(0, NTX, 4):
        n1 = min(NTX, n0 + 4)
        k.dma("sp", GTt[:, n0:n1, :], S.GT[n0 * 128:n1 * 128, :].rearrange("(n p) g -> p n g", p=128), wr=[g_b],
              wr_add=(n0 > 0))
    k.dma("sp", bg[:, :], I.b_gate.partition_broadcast(128), wr=[bg_b])
    k.dve("tensor_tensor", [g_b, bg_b], [g_b], out=GTt[:, :, :], in0=GTt[:, :, :],
          in1=bg[:, :].unsqueeze(1).to_broadcast([128, NTX, 16]), op=ALU.add)
    LF = ar.alloc([128, NTX, 8], F32, "LF")
    lf_b = Buf("LF")
    IG = ar.alloc([128, NTX, 8], F32, "IG")
    ig_b = Buf("IG")
    for d in range(2):
        k.act("activation", [g_b], [lf_b], out=LF[:, :, d * 4:(d + 1) * 4], in_=GTt[:, :, 8 * d + 4:8 * d + 8],
              func=ACT.Exp, scale=-1.0)
        k.dve("tensor_copy", [g_b], [ig_b], out=IG[:, :, d * 4:(d + 1) * 4], in_=GTt[:, :, 8 * d:8 * d + 4])
    k.act("activation", [lf_b], [lf_b], out=LF[:, :, :], in_=LF[:, :, :], func=ACT.Ln, bias=1.0)
    k.dve("tensor_scalar_mul", [lf_b], [lf_b], out=LF[:, :, :], in0=LF[:, :, :], scalar1=-1.0)
    BC = ar.alloc([128, NTX, 8], F32, "BC")
    bc_b = Buf("BC")
    eT = ar.alloc([128, NTX, 8], F32, "eT")
    et_b = Buf("eT")
    eS = ar.alloc([128, NTX, 8], F32, "eS")
    es_b = Buf("eS")
    eL = ar.alloc([128, NTX, 8], F32, "eL")
    el_b = Buf("eL")
    NC4 = NTX * 4
    for d in range(2):
        pt, pb = c.ps[d]
        k.pe("matmul", [lf_b, c.cm_b], [pb], out=pt[:, 0:NC4], lhsT=(c.triF if d == 0 else c.triB),
             rhs=LF[:, :, d * 4:(d + 1) * 4], start=True, stop=True)
        k.dve("tensor_copy", [pb], [bc_b], out=BC[:, :, d * 4:(d + 1) * 4],
              in_=pt[:, 0:NC4].rearrange("p (n h) -> p n h", h=4))
    pt, pb = c.ps[2]
    pt2, pb2 = c.ps[3]
    half = NTX * 8 // 2
    LF2 = LF[:, :, :].rearrange("p n h -> p (n h)")
    k.pe("matmul", [lf_b, c.cm_b], [pb], out=pt[:, 0:half], lhsT=c.ones, rhs=LF2[:, 0:half], start=True, stop=True)
    k.pe("matmul", [lf_b, c.cm_b], [pb2], out=pt2[:, 0:half], lhsT=c.ones, rhs=LF2[:, half:2 * half], start=True, stop=True)
    eL2 = eL[:, :, :].rearrange("p n h -> p (n h)")
    k.act("activation", [pb], [el_b], out=eL2[:, 0:half], in_=pt[:, 0:half], func=ACT.Exp)
    k.act("activation", [pb2], [el_b], out=eL2[:, half:2 * half], in_=pt2[:, 0:half], func=ACT.Exp)
    k.act("activation", [bc_b], [et_b], out=eT[:, :, :], in_=BC[:, :, :], func=ACT.Exp)
    k.dve("tensor_tensor", [ig_b, bc_b], [es_b], out=eS[:, :, :], in0=IG[:, :, :], in1=BC[:, :, :], op=ALU.subtract)
    k.act("activation", [es_b], [es_b], out=eS[:, :, :], in_=eS[:, :, :], func=ACT.Exp)
    dump(c, "DBG_BC", BC[:, :, :], [128, NTX, 8], bc_b)
    qT = [(ar.alloc([128, T], BF16, "mqT"), Buf("mqT")) for _ in range(2)]
    kT = [(ar.alloc([128, TX], BF16, "mkT"), Buf("mkT")) for _ in range(2)]
    KTM = ar.alloc([128, NTX, 256], BF16, "ktm")
    ktm_b = Buf("ktm")
    cw = ar.alloc([128, 4, 5], F32, "cw")
    cw_b = Buf("cw")
    k.dma("sp", cw[:, :, :], I.convw[:, :, :], wr=[cw_b])
    m1 = ar.mark()
    BLK = min(int(os.environ.get('KBLK', 1024)), T)
    rawr = sb_ring(ar, 2, [128, BLK + 4], F32, "raw")
    accr = sb_ring(ar, 2, [128, BLK], F32, "acc")
    sr = sb_ring(ar, 2, [128, BLK], F32, "sil")
    cosr = sb_ring(ar, 2, [128, BLK], F32, "cos")
    sinr = sb_ring(ar, 2, [128, BLK], F32, "sin")
    t1r = sb_ring(ar, 2, [128, 512], F32, "t1")
    t2r = sb_ring(ar, 2, [128, 512], F32, "t2")
    ps_r = Ring([c.ps[4], c.ps[5], c.ps[6], c.ps[7]])
    for ch in range(4):
        isq = ch < 2
        jj = ch % 2
        src = S.QM_T[jj] if isq else S.KM_T[jj]
        dstT, dst_b = (qT[jj] if isq else kT[jj])
        scale = 0.125 if isq else 1.0
        segs = [(t0, min(BLK, T - t0), 0, T) for t0 in range(0, T, BLK)]
        if not isq:
            segs.append((T, cfg.LC, T, TX))
        for (t0, n, lo, hi) in segs:
            raw, raw_b = rawr.next()
            a0 = max(lo, t0 - 2)
            a1 = min(hi, t0 + n + 2)
            if a0 > t0 - 2 or a1 < t0 + n + 2:
                k.pool("memset", [], [raw_b], ap=raw[:, 0:n + 4], constant=0.0)
            k.dma("sp", raw[:, a0 - (t0 - 2):a1 - (t0 - 2)], src[:, a0:a1], wr=[raw_b])
            acc, acc_b = accr.next()
            k.dve("tensor_scalar_mul", [raw_b, cw_b], [acc_b], out=acc[:, 0:n], in0=raw[:, 0:n], scalar1=cw[:, ch, 0:1])
            for j in range(1, 5):
                k.dve("scalar_tensor_tensor", [raw_b, cw_b, acc_b], [acc_b], out=acc[:, 0:n], in0=raw[:, j:j + n],
                      scalar=cw[:, ch, j:j + 1], in1=acc[:, 0:n], op0=ALU.mult, op1=ALU.add)
            if lo == T:
                k.act("activation", [acc_b], [dst_b], out=dstT[:, t0:t0 + n], in_=acc[:, 0:n], func=ACT.Silu)
                continue
            s_, s_b = sr.next()
            k.act("activation", [acc_b], [s_b], out=s_[:, 0:n], in_=acc[:, 0:n], func=ACT.Silu)
            cs, cs_b = cosr.next()
            sn, sn_b = sinr.next()
            k.dma("sp", cs[:, 0:n], I.ropec[:, t0:t0 + n], wr=[cs_b])
            k.dma("sp", sn[:, 0:n], I.ropes[:, t0:t0 + n], wr=[sn_b])
            for p0 in range(0, n, 512):
                pn = min(512, n - p0)
                pt, pb = ps_r.next()
                k.pe("matmul", [s_b, c.cm_b], [pb], out=pt[:, 0:pn], lhsT=c.perm, rhs=s_[:, p0:p0 + pn], start=True, stop=True)
                t1, t1_b = t1r.next()
                t2, t2_b = t2r.next()
                k.dve("scalar_tensor_tensor", [s_b, cs_b], [t1_b], out=t1[:, 0:pn], in0=s_[:, p0:p0 + pn], scalar=scale,
                       in1=cs[:, p0:p0 + pn], op0=ALU.mult, op1=ALU.mult)
                k.dve("scalar_tensor_tensor", [pb, sn_b], [t2_b], out=t2[:, 0:pn], in0=pt[:, 0:pn], scalar=scale,
                      in1=sn[:, p0:p0 + pn], op0=ALU.mult, op1=ALU.mult)
                k.dve("tensor_tensor", [t1_b, t2_b], [dst_b], out=dstT[:, t0 + p0:t0 + p0 + pn], in0=t1[:, 0:pn],
                      in1=t2[:, 0:pn], op=ALU.add)
    dump(c, "DBG_QT", qT[0][0][:, :], [128, T], qT[0][1], BF16)
    dump(c, "DBG_KT", kT[1][0][:, :], [128, TX], kT[1][1], BF16)
    for jj in range(2):
        kt, kt_b = kT[jj]
        for n0 in range(0, NTX, 4):
            nn = min(4, NTX - n0)
            pt, pb = ps_r.next()
            ptb = pt[:, :].bitcast(BF16)
            for a in range(nn):
                k.pe("transpose", [kt_b, c.cmb_b], [pb], out=ptb[:, a * 128:(a + 1) * 128],
                     in_=kt[:, (n0 + a) * 128:(n0 + a + 1) * 128], identity=c.identb)
            evac(KTM[:, n0:n0 + nn, jj * 128:(jj + 1) * 128], ptb[:, 0:nn * 128].rearrange("p (a x) -> p a x", x=128),
                 [pb], [ktm_b])
    k.barrier()
    ar.release(m1)
    C32 = ar.alloc([128, 4, 129], F32, "C32")
    Cbf = ar.alloc([128, 4, 129], BF16, "Cbf")
    cbuf = {}
    for d in range(2):
        for h in range(4):
            cbuf[(d, h)] = (Buf("c32"), Buf("cbf"))
    k.pool("memset", [], [cbuf[(d, h)][0] for d in range(2) for h in range(4)], ap=C32[:, :, :], constant=0.0)
    k.pool("memset", [], [cbuf[(d, h)][1] for d in range(2) for h in range(4)], ap=Cbf[:, :, :], constant=0.0)
    vmr = sb_ring(ar, 6, [128, 4, 129], BF16, "vma")
    for (vt, vb) in vmr.items:
        k.pool("memset", [], [vb], ap=vt[:, :, :], constant=1.0)
    vpr = sb_ring(ar, 4, [128, 4, 129], BF16, "vp4")
    ptmr = sb_ring(ar, 4, [128, 128], BF16, "ptm")
    hsr = sb_ring(ar, 4, [128, 129], F32, "hs")
    ddr = sb_ring(ar, 4, [128, 1], F32, "dd")
    hor = sb_ring(ar, 4, [128, 512], F32, "hout")
    tmpr = sb_ring(ar, 4, [128, 129], F32, "ctmp")
    ps_s = Ring([c.ps[0], c.ps[1], c.ps[2]])
    ps_o = Ring([c.ps[3], c.ps[4], c.ps[5]])
    ps_u = Ring([c.ps[6], c.ps[7]])
    seq = {0: [NT, NT + 1] + list(range(NT)), 1: [NT + 1, NT] + list(range(NT - 1, -1, -1))}
    for step in range(NT + 2):
        for d in range(2):
            n = seq[d][step]
            latent = n < NT
            tok = slice(n * 128, (n + 1) * 128)
            vt, vb = vmr.next()
            k.dma("sp", vt[:, :, 0:128], S.VM[n * 128:(n + 1) * 128, :].rearrange("p (h v) -> p h v", v=128), wr=[vb])
            vp, vp_b = vpr.next()
            k.dve("tensor_tensor", [vb, es_b], [vp_b], out=vp[:, :, :], in0=vt[:, :, :],
                  in1=eS[:, n, d * 4:(d + 1) * 4].unsqueeze(2).to_broadcast([128, 4, 129]), op=ALU.mult)
            if latent:
                ho, ho_b = hor.next()
            for h in range(4):
                jj, hh = h // 2, h % 2
                pr = slice(hh * 64, hh * 64 + 64)
                slot = d * 2 + jj
                c32_b, cbf_b = cbuf[(d, h)]
                col = d * 4 + h
                if latent:
                    q_, q_b = qT[jj]
                    kt, kt_b = kT[jj]
                    pst, psb = ps_s.next()
                    k.pe("matmul", [kt_b, q_b], [psb], out=pst[:, 0:128], lhsT=kt[pr, tok], rhs=q_[pr, tok], start=True, stop=True)
                    ptm, ptm_b = ptmr.next()
                    k.dve("tensor_tensor", [psb, c.cm_b], [ptm_b], out=ptm[:, :], in0=pst[:, 0:128],
                          in1=(c.triF if d == 0 else c.triB), op=ALU.mult)
                    po, pob = ps_o.next()
                    k.pe("matmul", [ptm_b, vp_b], [pob], out=po[:, 0:129], lhsT=ptm[:, :], rhs=vp[:, h, :], start=True, stop=False)
                    k.pe("matmul", [q_b, cbf_b], [pob], out=po[:, 0:129], lhsT=q_[pr, tok], rhs=Cbf[pr, slot, :], start=False, stop=True)
                    hs, hs_b = hsr.next()
                    k.act("activation", [pob, et_b], [hs_b], out=hs[:, :], in_=po[:, 0:129], func=ACT.Copy, scale=eT[:, n, col:col + 1])
                    dd, dd_b = ddr.next()
                    k.dve("tensor_scalar", [hs_b], [dd_b], out=dd[:, :], in0=hs[:, 128:129], scalar1=-1.0, scalar2=1.0,
                          op0=ALU.mult, op1=ALU.max)
                    k.dve("tensor_tensor", [hs_b, dd_b], [dd_b], out=dd[:, :], in0=dd[:, :], in1=hs[:, 128:129], op=ALU.max)
                    k.dve("reciprocal", [dd_b], [dd_b], out=dd[:, :], in_=dd[:, :])
                    k.dve("tensor_scalar_mul", [hs_b, dd_b], [ho_b], out=ho[:, h * 128:(h + 1) * 128], in0=hs[:, 0:128],
                          scalar1=dd[:, 0:1])
                pu, pub = ps_u.next()
                k.pe("matmul", [ktm_b, vp_b], [pub], out=pu[pr, 0:129], lhsT=KTM[:, n, h * 64:(h + 1) * 64], rhs=vp[:, h, :],
                     start=True, stop=True)
                tmp, tmp_b = tmpr.next()
                k.dve("tensor_tensor", [pub, c32_b], [tmp_b], out=tmp[pr, :], in0=pu[pr, 0:129], in1=C32[pr, slot, :], op=ALU.add)
                k.dve("tensor_scalar_mul", [tmp_b, el_b], [c32_b], out=C32[pr, slot, :], in0=tmp[pr, :], scalar1=eL[pr, n, col:col + 1])
                k.act("activation", [tmp_b, el_b], [cbf_b], out=Cbf[pr, slot, :], in_=tmp[pr, :], func=ACT.Copy,
                      scale=eL[pr, n, col:col + 1])
            if latent:
                dst = S.HF if d == 0 else S.HB
                k.dma("sp", dst[n * 128:(n + 1) * 128, :], ho[:, :], rd=[ho_b])
    k.barrier()
    ar.release(m0)


LN_EPS = 1e-5
ALPHA = 2.0 ** 0.25


def bcast_load(c, ar, src_row, n, name):
    t = ar.alloc([128, n], F32, name)
    b = Buf(name)
    c.k.dma("sp", t[:, :], src_row.partition_broadcast(128), wr=[b])
    return t, b


def stage_e(c):
    k, ar, I, S, nc, cfg = c.k, c.ar, c.I, c.S, c.nc, c.cfg
    T, NT, NE = cfg.T, cfg.NT, cfg.NE
    m0 = ar.mark()
    evac = Evac(k)
    WO = ar.alloc([128, 8, 1024], BF16, "wo")
    wo_b = Buf("wo")
    wst = sb_ring(ar, 2, [128, 1024], F32, "wost")
    for kk in range(8):
        wt, wb = wst.next()
        k.dma("sp", wt[:, :], I.w_out[kk * 128:(kk + 1) * 128, :], wr=[wb])
        evac(WO[:, kk, :], wt[:, :], [wb], [wo_b])
    WR = ar.alloc([128, 8, NE], F32, "wr")
    wr_b = Buf("wr")
    for a0 in (0, 4):
        k.dma("sp", WR[:, a0:a0 + 4, :], I.w_router[a0 * 128:(a0 + 4) * 128, :].rearrange("(a p) e -> p a e", p=128),
              wr=[wr_b], wr_add=(a0 > 0))
    g1t, g1_b = bcast_load(c, ar, I.ln1_g, 1024, "ln1g")
    b1t, b1_b = bcast_load(c, ar, I.ln1_b, 1024, "ln1b")
    mgt, mg_b = bcast_load(c, ar, I.ml_norm_g, 512, "mlg")
    P1 = ar.alloc([128, 1024], F32, "p1sc2")
    p1_b = Buf("p1")
    k.dve("tensor_scalar_add", [c.MB_b], [p1_b], out=P1[:, :], in0=c.MB[:, 4096:5120], scalar1=1.0)
    G1 = c.MB[:, 2048:3072]
    SH2 = c.MB[:, 3072:4096]
    hfr = sb_ring(ar, 2, [128, 512], F32, "hf")
    hbr = sb_ring(ar, 2, [128, 512], F32, "hb")
    omr = sb_ring(ar, 2, [128, 512], F32, "om")
    xr = sb_ring(ar, 2, [128, 1024], F32, "xe")
    mixr = sb_ring(ar, 2, [128, 8, 128], BF16, "mixT")
    hr = sb_ring(ar, 2, [128, 512], F32, "h")
    sqr = sb_ring(ar, 2, [128, 512], F32, "sq")
    sgr = sb_ring(ar, 2, [128, 512], F32, "sg")
    mlr = sb_ring(ar, 2, [128, 512], BF16, "mlb")
    st4 = sb_ring(ar, 4, [128, 4], F32, "st4")
    st1 = sb_ring(ar, 8, [128, 1], F32, "st1")
    yr = sb_ring(ar, 2, [128, 1024], F32, "y")
    scr = sb_ring(ar, 2, [128, 1024], F32, "scr")
    accr = sb_ring(ar, 2, [128, 1024], F32, "acc")
    h2r = sb_ring(ar, 2, [128, 1024], F32, "h2")
    h2br = sb_ring(ar, 2, [128, 1024], BF16, "h2b")
    h2tr = sb_ring(ar, 2, [128, 8, 128], F32, "h2T")
    lgr = sb_ring(ar, 2, [128, NE], F32, "lg")
    ps_t = Ring([c.ps[0], c.ps[1]])
    ps_m = Ring([c.ps[2], c.ps[3], c.ps[4], c.ps[5]])
    ps_r = Ring([c.ps[6], c.ps[7]])
    for i in range(NT):
        rows = slice(i * 128, (i + 1) * 128)
        hf, hf_b = hfr.next()
        hb, hb_b = hbr.next()
        om, om_b = omr.next()
        xt, x_b = xr.next()
        mixT, mx_b = mixr.next()
        k.dma("sp", hf[:, :], S.HF[rows, :], wr=[hf_b])
        k.dma("sp", hb[:, :], S.HB[rows, :], wr=[hb_b])
        k.dma("sp", om[:, :], S.OM[rows, :], wr=[om_b])
        k.dma("sp", xt[:, :], I.x[rows, :], wr=[x_b])
        k.dma("sp", mixT[:, 0:4, :], S.MIX_T[0:4, :, rows].rearrange("j p t -> p j t"), wr=[mx_b])
        h, h_b = hr.next()
        k.pool("tensor_tensor", [hf_b, hb_b], [h_b], out=h[:, :], in0=hf[:, :], in1=hb[:, :], op=ALU.add)
        h3 = h[:, :].rearrange("p (a v) -> p a v", v=128)
        mu, mu_b = st4.next()
        k.dve("tensor_reduce", [h_b], [mu_b], out=mu[:, :], in_=h3, axis=AX.X, op=ALU.add)
        k.dve("tensor_scalar_mul", [mu_b], [mu_b], out=mu[:, :], in0=mu[:, :], scalar1=1.0 / 128)
        k.dve("tensor_tensor", [h_b, mu_b], [h_b], out=h3, in0=h3, in1=mu[:, :].unsqueeze(2).to_broadcast([128, 4, 128]),
              op=ALU.subtract)
        sq, sq_b = sqr.next()
        k.pool("tensor_tensor", [h_b], [sq_b], out=sq[:, :], in0=h[:, :], in1=h[:, :], op=ALU.mult)
        var, var_b = st4.next()
        k.dve("tensor_reduce", [sq_b], [var_b], out=var[:, :], in_=sq[:, :].rearrange("p (a v) -> p a v", v=128), axis=AX.X,
              op=ALU.add)
        k.dve("tensor_scalar", [var_b], [var_b], out=var[:, :], in0=var[:, :], scalar1=1.0 / 128, scalar2=LN_EPS,
              op0=ALU.mult, op1=ALU.add)
        k.act("activation", [var_b], [var_b], out=var[:, :], in_=var[:, :], func=ACT.Sqrt)
        k.dve("reciprocal", [var_b], [var_b], out=var[:, :], in_=var[:, :])
        k.dve("tensor_tensor", [h_b, var_b], [h_b], out=h3, in0=h3, in1=var[:, :].unsqueeze(2).to_broadcast([128, 4, 128]),
              op=ALU.mult)
        sg, sg_b = sgr.next()
        k.act("activation", [om_b], [sg_b], out=sg[:, :], in_=om[:, :], func=ACT.Sigmoid)
        k.pool("tensor_tensor", [h_b, mg_b], [h_b], out=h[:, :], in0=h[:, :], in1=mgt[:, :], op=ALU.mult)
        mlb, ml_b = mlr.next()
        k.dve("tensor_tensor", [h_b, sg_b], [ml_b], out=mlb[:, :], in0=h[:, :], in1=sg[:, :], op=ALU.mult)
        pt, pb = ps_t.next()
        ptb = pt[:, :].bitcast(BF16)
        for a in range(4):
            k.pe("transpose", [ml_b, c.cmb_b], [pb], out=ptb[:, a * 128:(a + 1) * 128], in_=mlb[:, a * 128:(a + 1) * 128],
                 identity=c.identb)
        evac(mixT[:, 4:8, :], ptb[:, 0:512].rearrange("p (a x) -> p a x", x=128), [pb], [mx_b])
        y, y_b = yr.next()
        for half in range(2):
            pm, pmb = ps_m.next()
            for kk in range(8):
                k.pe("matmul", [mx_b, wo_b], [pmb], out=pm[:, :], lhsT=mixT[:, kk, :], rhs=WO[:, kk, half * 512:(half + 1) * 512],
                     start=(kk == 0), stop=(kk == 7))
            k.dve("tensor_tensor", [pmb, c.MB_b], [y_b], out=y[:, half * 512:(half + 1) * 512], in0=pm[:, :],
                  in1=G1[:, half * 512:(half + 1) * 512], op=ALU.mult)
        k.dve("scalar_tensor_tensor", [x_b, y_b], [y_b], out=y[:, :], in0=xt[:, :], scalar=ALPHA, in1=y[:, :], op0=ALU.mult,
              op1=ALU.add)
        x1, x1_b = layer_norm(c, y, y_b, g1t, g1_b, b1t, b1_b, st1, scr)
        acc, acc_b = accr.next()
        k.act("activation", [x1_b], [acc_b], out=acc[:, :], in_=x1[:, :], func=ACT.Copy, scale=ALPHA)
        k.dma("sp", S.ACC[rows, :], acc[:, :], rd=[acc_b])
        h2, h2_b = h2r.next()
        k.pool("tensor_tensor", [x1_b, p1_b], [h2_b], out=h2[:, :], in0=x1[:, :], in1=P1[:, :], op=ALU.mult)
        k.pool("tensor_tensor", [h2_b, c.MB_b], [h2_b], out=h2[:, :], in0=h2[:, :], in1=SH2, op=ALU.add)
        h2b, h2b_b = h2br.next()
        k.act("activation", [h2_b], [h2b_b], out=h2b[:, :], in_=h2[:, :], func=ACT.Copy)
        k.dma("sp", S.H2[rows, :], h2b[:, :], rd=[h2b_b])
        h2T, h2T_b = h2tr.next()
        for half in range(2):
            pt, pb = ps_t.next()
            for q in range(4):
                ch = half * 4 + q
                k.pe("transpose", [h2_b, c.cm_b], [pb], out=pt[:, q * 128:(q + 1) * 128], in_=h2[:, ch * 128:(ch + 1) * 128],
                     identity=c.ident)
            evac(h2T[:, half * 4:(half + 1) * 4, :], pt[:, :].rearrange("p (a x) -> p a x", x=128), [pb], [h2T_b])
        pr_, prb = ps_r.next()
        for kk in range(8):
            k.pe("matmul", [h2T_b, wr_b], [prb], out=pr_[:, 0:NE], lhsT=h2T[:, kk, :], rhs=WR[:, kk, :], start=(kk == 0),
                 stop=(kk == 7))
        mxv, mxv_b = st1.next()
        k.dve("tensor_reduce", [prb], [mxv_b], out=mxv[:, :], in_=pr_[:, 0:NE], axis=AX.X, op=ALU.max)
        k.dve("tensor_scalar_mul", [mxv_b], [mxv_b], out=mxv[:, :], in0=mxv[:, :], scalar1=-1.0)
        lg, lg_b = lgr.next()
        ssum, ssum_b = st1.next()
        k.act("activation", [prb, mxv_b], [lg_b, ssum_b], out=lg[:, :], in_=pr_[:, 0:NE], func=ACT.Exp, bias=mxv[:, 0:1],
              accum_out=ssum[:, 0:1])
        k.dve("reciprocal", [ssum_b], [ssum_b], out=ssum[:, :], in_=ssum[:, :])
        k.dve("tensor_scalar_mul", [lg_b, ssum_b], [c.aff_b], out=c.AFFt[:, i, :], in0=lg[:, :], scalar1=ssum[:, 0:1])
        k.dma("sp", S.AFF[rows, :], c.AFFt[:, i, :], rd=[c.aff_b])
    k.barrier()
    ar.release(m0)


def layer_norm(c, y, y_b, gt, g_b, bt, b_b, st1, scr):
    k = c.k
    mu, mu_b = st1.next()
    k.dve("tensor_reduce", [y_b], [mu_b], out=mu[:, :], in_=y[:, :], axis=AX.X, op=ALU.add)
    k.dve("tensor_scalar_mul", [mu_b], [mu_b], out=mu[:, :], in0=mu[:, :], scalar1=-1.0 / 1024)
    k.dve("tensor_scalar_add", [y_b, mu_b], [y_b], out=y[:, :], in0=y[:, :], scalar1=mu[:, 0:1])
    sc, sc_b = scr.next()
    ss, ss_b = st1.next()
    k.act("activation", [y_b], [sc_b, ss_b], out=sc[:, :], in_=y[:, :], func=ACT.Square, accum_out=ss[:, 0:1])
    k.dve("tensor_scalar", [ss_b], [ss_b], out=ss[:, :], in0=ss[:, :], scalar1=1.0 / 1024, scalar2=LN_EPS, op0=ALU.mult,
          op1=ALU.add)
    k.act("activation", [ss_b], [ss_b], out=ss[:, :], in_=ss[:, :], func=ACT.Sqrt)
    k.dve("reciprocal", [ss_b], [ss_b], out=ss[:, :], in_=ss[:, :])
    k.dve("scalar_tensor_tensor", [y_b, ss_b, g_b], [y_b], out=y[:, :], in0=y[:, :], scalar=ss[:, 0:1], in1=gt[:, :],
          op0=ALU.mult, op1=ALU.mult)
    k.dve("tensor_tensor", [y_b, b_b], [y_b], out=y[:, :], in0=y[:, :], in1=bt[:, :], op=ALU.add)
    return y, y_b


def stage_f(c):
    k, ar, I, S, nc, cfg = c.k, c.ar, c.I, c.S, c.nc, c.cfg
    T, NT, NE, NSL, CAP = cfg.T, cfg.NT, cfg.NE, cfg.NSL, cfg.CAP
    ar.release(c.mb_mark)
    m0 = ar.mark()
    NN = NT * NE
    A = c.AFFt
    a_b = c.aff_b
    io = ar.alloc([128, 128], F32, "iota")
    io_b = Buf("iota")
    k.dma("sp", io[:, :], I.iota[:, :], wr=[io_b])
    lo = ar.alloc([128, NE], F32, "lo")
    hi = ar.alloc([128, NE], F32, "hi")
    mid = ar.alloc([128, NE], F32, "mid")
    dl = ar.alloc([128, NE], F32, "dl")
    sel = ar.alloc([128, NE], F32, "sel")
    pc = ar.alloc([128, NE], BF16, "pc")
    M = ar.alloc([128, NT, NE], F32, "M")
    lo_b, hi_b, mid_b, dl_b, sel_b, pc_b, M_b = [Buf(n) for n in ("lo", "hi", "mid", "dl", "sel", "pc", "M")]
    k.dve("memset", [], [lo_b], ap=lo[:, :], constant=0.0)
    k.dve("memset", [], [hi_b], ap=hi[:, :], constant=1.0)
    psr = Ring([c.ps[0], c.ps[1]])
    for it in range(30):
        k.dve("tensor_tensor", [lo_b, hi_b], [mid_b], out=mid[:, :], in0=lo[:, :], in1=hi[:, :], op=ALU.add)
        k.dve("tensor_scalar_mul", [mid_b], [mid_b], out=mid[:, :], in0=mid[:, :], scalar1=0.5)
        k.dve("tensor_tensor", [a_b, mid_b], [M_b], out=M[:, :, :], in0=A[:, :, :],
              in1=mid[:, :].unsqueeze(1).to_broadcast([128, NT, NE]), op=ALU.is_gt)
        k.dve("tensor_reduce", [M_b], [dl_b], out=dl[:, :], in_=M[:, :, :].rearrange("p n e -> p e n"), axis=AX.X, op=ALU.add)
        k.dve("tensor_copy", [dl_b], [pc_b], out=pc[:, :], in_=dl[:, :])
        pt, pb = psr.next()
        k.pe("matmul", [pc_b, c.cmb_b], [pb], out=pt[:, 0:NE], lhsT=c.onesb, rhs=pc[:, :], start=True, stop=True)
        k.dve("tensor_single_scalar", [pb], [sel_b], out=sel[:, :], in_=pt[:, 0:NE], scalar=float(CAP), op=ALU.is_ge)
        k.dve("tensor_tensor", [mid_b, lo_b], [dl_b], out=dl[:, :], in0=mid[:, :], in1=lo[:, :], op=ALU.subtract)
        k.dve("tensor_tensor", [dl_b, sel_b], [dl_b], out=dl[:, :], in0=dl[:, :], in1=sel[:, :], op=ALU.mult)
        k.dve("tensor_tensor", [lo_b, dl_b], [lo_b], out=lo[:, :], in0=lo[:, :], in1=dl[:, :], op=ALU.add)
        k.dve("tensor_tensor", [hi_b, mid_b], [dl_b], out=dl[:, :], in0=hi[:, :], in1=mid[:, :], op=ALU.subtract)
        k.dve("tensor_tensor", [dl_b, sel_b], [dl_b], out=dl[:, :], in0=dl[:, :], in1=sel[:, :], op=ALU.mult)
        k.dve("tensor_tensor", [mid_b, dl_b], [hi_b], out=hi[:, :], in0=mid[:, :], in1=dl[:, :], op=ALU.add)
    if "stopF1" in cfg.debug:
        dump(c, "DBG_LO", lo[:, :], [128, NE], lo_b)
        k.barrier()
        ar.release(m0)
        return
    Mb = ar.alloc([128, NN], BF16, "Mb")
    Mb_b = Buf("Mb")
    k.dve("tensor_tensor", [a_b, lo_b], [Mb_b], out=Mb[:, :].rearrange("p (n e) -> p n e", e=NE), in0=A[:, :, :],
          in1=lo[:, :].unsqueeze(1).to_broadcast([128, NT, NE]), op=ALU.is_gt)
    INCL = ar.alloc([128, NN], F32, "INCL")
    incl_b = Buf("INCL")
    TA = ar.alloc([128, NN], F32, "TA")
    ta_b = Buf("TA")
    TB = ar.alloc([128, NN], F32, "TB")
    tb_b = Buf("TB")
    TOT = ar.alloc([128, NN], F32, "TOT")
    tot_b = Buf("TOT")
    for p0 in range(0, NN, 512):
        w = min(512, NN - p0)
        pt, pb = psr.next()
        k.pe("matmul", [Mb_b, c.cmb_b], [pb], out=pt[:, 0:w], lhsT=c.triFb, rhs=Mb[:, p0:p0 + w], start=True, stop=True)
        k.dve("tensor_copy", [pb], [incl_b], out=INCL[:, p0:p0 + w], in_=pt[:, 0:w])
        pt, pb = psr.next()
        k.pe("matmul", [Mb_b, c.cmb_b], [pb], out=pt[:, 0:w], lhsT=c.onesb, rhs=Mb[:, p0:p0 + w], start=True, stop=True)
        k.dve("tensor_copy", [pb], [tot_b], out=TOT[:, p0:p0 + w], in_=pt[:, 0:w])
        k.dve("tensor_copy", [tot_b], [ta_b], out=TA[:, p0:p0 + w], in_=TOT[:, p0:p0 + w])
    cur, cur_b, oth, oth_b = TA, ta_b, TB, tb_b
    s = 1
    while s < NT:
        sw = s * NE
        k.dve("tensor_copy", [cur_b], [oth_b], out=oth[:, 0:sw], in_=cur[:, 0:sw])
        k.dve("tensor_tensor", [cur_b], [oth_b], out=oth[:, sw:NN], in0=cur[:, sw:NN], in1=cur[:, 0:NN - sw], op=ALU.add)
        cur, cur_b, oth, oth_b = oth, oth_b, cur, cur_b
        s *= 2
    k.dve("tensor_tensor", [cur_b, tot_b], [cur_b], out=cur[:, :], in0=cur[:, :], in1=TOT[:, :], op=ALU.subtract)
    k.dve("tensor_tensor", [incl_b, cur_b], [incl_b], out=INCL[:, :], in0=INCL[:, :], in1=cur[:, :], op=ALU.add)
    if "stopF2" in cfg.debug:
        dump(c, "DBG_INCL", INCL[:, :], [128, NN], incl_b)
        k.barrier()
        ar.release(m0)
        return
    K1 = NSL + 1
    K128 = ar.alloc([128, K1], F32, "K128")
    k128_b = Buf("K128")
    k.dve("tensor_scalar_mul", [io_b], [k128_b], out=K128[:, :], in0=io[:, 0:K1], scalar1=128.0)
    Ge = ar.alloc([128, NN, K1], F32, "Ge")
    ge_b = Buf("Ge")
    k.dve("tensor_tensor", [incl_b, k128_b], [ge_b], out=Ge[:, :, :], in0=INCL[:, :].unsqueeze(2).to_broadcast([128, NN, K1]),
          in1=K128[:, :].unsqueeze(1).to_broadcast([128, NN, K1]), op=ALU.is_ge)
    Bv = ar.alloc([128, NN], F32, "Bv")
    bv_b = Buf("Bv")
    k.dve("tensor_reduce", [ge_b], [bv_b], out=Bv[:, :], in_=Ge[:, :, 1:K1], axis=AX.X, op=ALU.add)
    k.dve("scalar_tensor_tensor", [bv_b, incl_b], [bv_b], out=Bv[:, :], in0=Bv[:, :], scalar=-128.0, in1=INCL[:, :],
          op0=ALU.mult, op1=ALU.add)
    Am = ar.alloc([128, NN, NSL], BF16, "Am")
    am_b = Buf("Am")
    Al = ar.alloc([128, NN, NSL], BF16, "Al")
    al_b = Buf("Al")
    k.dve("tensor_tensor", [ge_b], [am_b], out=Am[:, :, :], in0=Ge[:, :, 0:NSL], in1=Ge[:, :, 1:K1], op=ALU.subtract)
    k.dve("tensor_scalar", [ge_b], [al_b], out=Al[:, :, :], in0=Ge[:, :, 0:NSL], scalar1=-1.0, scalar2=1.0, op0=ALU.mult,
          op1=ALU.add)
    dump(c, "DBG_INCL", INCL[:, :], [128, NN], incl_b)
    if "stopF3" in cfg.debug:
        dump(c, "DBG_BV", Bv[:, :], [128, NN], bv_b)
        k.barrier()
        ar.release(m0)
        return
    thr = sb_ring(ar, 4, [128, 128], BF16, "Th")
    ps4 = Ring([c.ps[2], c.ps[3], c.ps[4], c.ps[5]])
    for e in range(NE):
        pt, pb = ps4.next()
        for n in range(NT):
            th, th_b = thr.next()
            col = n * NE + e
            eng = k.dve
            eng("tensor_scalar", [io_b, bv_b], [th_b], out=th[:, :], in0=io[:, :], scalar1=Bv[:, col:col + 1], scalar2=None,
                op0=ALU.is_ge)
            k.pe("matmul", [th_b, am_b], [pb], out=pt[:, 0:NSL], lhsT=th[:, :], rhs=Am[:, col, :], start=(n == 0), stop=False)
            k.pe("matmul", [c.cmb_b, al_b], [pb], out=pt[:, 0:NSL], lhsT=c.onesb, rhs=Al[:, col, :], start=False,
                 stop=(n == NT - 1))
        k.dve("tensor_scalar_min", [pb], [c.idx_b], out=c.IDX[:, e, :], in0=pt[:, 0:NSL], scalar1=float(T - 1))
    dump(c, "DBG_IDX", c.IDX[:, :, :], [128, NE, NSL], c.idx_b, U32)
    k.barrier()
    ar.release(m0)


def stage_g(c):
    k, ar, I, S, nc, cfg = c.k, c.ar, c.I, c.S, c.nc, c.cfg
    T, NT, NE, NSL, CAP, NFC, DFF = cfg.T, cfg.NT, cfg.NE, cfg.NSL, cfg.CAP, cfg.NFC, cfg.DFF
    m0 = ar.mark()
    evac = Evac(k)
    FG = 2
    NG = NFC // FG
    HW_ = min(int(os.environ.get('KHW', 512)), CAP)
    NH = CAP // HW_
    xer = sb_ring(ar, 1, [128, NSL, 1024], BF16, "XE")
    xtr = sb_ring(ar, 1, [128, 8, CAP], BF16, "XT")
    g16r = sb_ring(ar, 2, [128, NSL, NE], F32, "g16")
    wgs = sb_ring(ar, 2, [128, 8, FG * 128], F32, "wgs")
    wus = sb_ring(ar, 2, [128, 8, FG * 128], F32, "wus")
    wds = sb_ring(ar, 1, [128, FG, 1024], F32, "wds")
    wgb = sb_ring(ar, 2, [128, 8, FG * 128], BF16, "wgb")
    wub = sb_ring(ar, 2, [128, 8, FG * 128], BF16, "wub")
    WD = ar.alloc([128, NFC, 1024], BF16, "WD")
    wd_b = Buf("WD")
    HT = ar.alloc([128, NFC, CAP], BF16, "HT")
    ht_b = Buf("HT")
    sgr = sb_ring(ar, 2, [128, HW_], F32, "sg")
    ysr = sb_ring(ar, 1, [128, 1024], F32, "ys")
    acc_b = Buf("ACCdram")
    ps_t = Ring([c.ps[0], c.ps[1]])
    ps_g = Ring([c.ps[2], c.ps[3]])
    ps_u = Ring([c.ps[4], c.ps[5]])
    ps_d = Ring([c.ps[6], c.ps[7]])
    cast_n = [0]

    def cast(out_ap, in_ap, rd, wr):
        cast_n[0] += 1
        m = cast_n[0] % 3
        if m == 0:
            k.act("activation", rd, wr, out=out_ap, in_=in_ap, func=ACT.Copy)
        elif m == 1:
            k.dve("tensor_copy", rd, wr, out=out_ap, in_=in_ap)
        else:
            k.pool("tensor_copy", rd, wr, out=out_ap, in_=in_ap)

    XE, xe_b = xer.next()
    XT, xt_b = xtr.next()
    g16s = {}

    def gather(e):
        g16, g16_b = g16r.next()
        g16s[e] = (g16, g16_b)
        for kq in range(NSL):
            off = bass.IndirectOffsetOnAxis(ap=c.IDX[:, e, kq:kq + 1], axis=0)
            k.op("pool", ("indirect_dma_start", dict(out=XE[:, kq, :], out_offset=None, in_=S.H2[:, :], in_offset=off)),
                 [c.idx_b], [xe_b], dma=True, wr_add=(kq > 0))
            off2 = bass.IndirectOffsetOnAxis(ap=c.IDX[:, e, kq:kq + 1], axis=0)
            k.op("pool", ("indirect_dma_start", dict(out=g16[:, kq, :], out_offset=None, in_=S.AFF[:, :], in_offset=off2)),
                 [c.idx_b], [g16_b], dma=True, wr_add=(kq > 0))

    def transposes():
        for kq in range(NSL):
            for half in range(2):
                pt, pb = ps_t.next()
                ptb = pt[:, :].bitcast(BF16)
                for q in range(4):
                    ch = half * 4 + q
                    k.pe("transpose", [xe_b, c.cmb_b], [pb], out=ptb[:, q * 128:(q + 1) * 128],
                         in_=XE[:, kq, ch * 128:(ch + 1) * 128], identity=c.identb)
                evac(XT[:, half * 4:(half + 1) * 4, kq * 128:(kq + 1) * 128],
                     ptb[:, 0:512].rearrange("p (a x) -> p a x", x=128), [pb], [xt_b])

    gather(0)
    transposes()
    for e in range(NE):
        g16, g16_b = g16s[e]
        if e + 1 < NE:
            gather(e + 1)
        for g in range(NG):
            f0 = g * FG * 128
            ws, ws_b = wgs.next()
            for a0 in (0, 4):
                k.dma("sp", ws[:, a0:a0 + 4, :], I.weg[e, a0 * 128:(a0 + 4) * 128, f0:f0 + FG * 128].rearrange(
                    "(a p) f -> p a f", p=128), wr=[ws_b], wr_add=(a0 > 0))
            wg, wg_b = wgb.next()
            cast(wg[:, :, :], ws[:, :, :], [ws_b], [wg_b])
            ws2, ws2_b = wus.next()
            for a0 in (0, 4):
                k.dma("sp", ws2[:, a0:a0 + 4, :], I.weu[e, a0 * 128:(a0 + 4) * 128, f0:f0 + FG * 128].rearrange(
                    "(a p) f -> p a f", p=128), wr=[ws2_b], wr_add=(a0 > 0))
            wu, wu_b = wub.next()
            cast(wu[:, :, :], ws2[:, :, :], [ws2_b], [wu_b])
            ws3, ws3_b = wds.next()
            k.dma("sp", ws3[:, :, :], I.wed[e, f0:f0 + FG * 128, :].rearrange("(a p) d -> p a d", p=128), wr=[ws3_b])
            cast(WD[:, g * FG:(g + 1) * FG, :], ws3[:, :, :], [ws3_b], [wd_b])
            for fl in range(FG):
                fc = g * FG + fl
                for hf in range(NH):
                    sl = slice(hf * HW_, (hf + 1) * HW_)
                    pg, pgb = ps_g.next()
                    pu, pub = ps_u.next()
                    for kk in range(8):
                        k.pe("matmul", [wg_b, xt_b], [pgb], out=pg[:, 0:HW_], lhsT=wg[:, kk, fl * 128:(fl + 1) * 128],
                             rhs=XT[:, kk, sl], start=(kk == 0), stop=(kk == 7))
                    for kk in range(8):
                        k.pe("matmul", [wu_b, xt_b], [pub], out=pu[:, 0:HW_], lhsT=wu[:, kk, fl * 128:(fl + 1) * 128],
                             rhs=XT[:, kk, sl], start=(kk == 0), stop=(kk == 7))
                    sg, sg_b = sgr.next()
                    k.act("activation", [pgb], [sg_b], out=sg[:, :], in_=pg[:, 0:HW_], func=ACT.Silu)
                    k.dve("tensor_tensor", [sg_b, pub], [ht_b], out=HT[:, fc, sl], in0=sg[:, :], in1=pu[:, 0:HW_], op=ALU.mult)
        for kq in range(NSL):
            ys, ys_b = ysr.next()
            for dh in range(2):
                pd, pdb = ps_d.next()
                for fc in range(NFC):
                    k.pe("matmul", [ht_b, wd_b], [pdb], out=pd[:, :], lhsT=HT[:, fc, kq * 128:(kq + 1) * 128],
                         rhs=WD[:, fc, dh * 512:(dh + 1) * 512], start=(fc == 0), stop=(fc == NFC - 1))
                k.dve("scalar_tensor_tensor", [pdb, g16_b, c.g2_b], [ys_b], out=ys[:, dh * 512:(dh + 1) * 512], in0=pd[:, :],
                      scalar=g16[:, kq, e:e + 1], in1=c.G2t[:, dh * 512:(dh + 1) * 512], op0=ALU.mult, op1=ALU.mult)
            off = bass.IndirectOffsetOnAxis(ap=c.IDX[:, e, kq:kq + 1], axis=0)
            k.op("pool", ("indirect_dma_start", dict(out=S.ACC[:, :], out_offset=off, in_=ys[:, :], in_offset=None,
                                                     compute_op=ALU.add)),
                 [ys_b, c.idx_b], [acc_b], dma=True)
        if e + 1 < NE:
            transposes()
    k.barrier()
    ar.release(m0)


def stage_h(c):
    k, ar, I, S, nc, cfg = c.k, c.ar, c.I, c.S, c.nc, c.cfg
    NT = cfg.NT
    m0 = ar.mark()
    g2t, g2_b = bcast_load(c, ar, I.ln2_g, 1024, "ln2g")
    b2t, b2_b = bcast_load(c, ar, I.ln2_b, 1024, "ln2b")
    yr = sb_ring(ar, 3, [128, 1024], F32, "yh")
    scr = sb_ring(ar, 2, [128, 1024], F32, "scrh")
    st1 = sb_ring(ar, 8, [128, 1], F32, "st1h")
    for i in range(NT):
        rows = slice(i * 128, (i + 1) * 128)
        y, y_b = yr.next()
        k.dma("sp", y[:, :], S.ACC[rows, :], wr=[y_b])
        layer_norm(c, y, y_b, g2t, g2_b, b2t, b2_b, st1, scr)
        k.dma("sp", c.out[rows, :], y[:, :], rd=[y_b])
    k.barrier()
    ar.release(m0)


_CONST_CACHE = {}


def _consts(T):
    if T in _CONST_CACHE:
        return _CONST_CACHE[T]
    cos, sin = _rope_tables(T)
    ident = np.eye(128, dtype=np.float32)
    perm = _perm_matrix()
    s = np.arange(128)[:, None]
    t = np.arange(128)[None, :]
    triF = (s <= t).astype(np.float32)
    triB = (s >= t).astype(np.float32)
    ones = np.ones((128, 128), np.float32)
    cmat = np.stack([ident, perm, triF, triB, ones]).astype(np.float32)
    iota = np.tile(np.arange(128, dtype=np.float32)[None, :], (128, 1))
    _CONST_CACHE[T] = dict(ropec=cos, ropes=sin, cmat=cmat, iota=iota)
    return _CONST_CACHE[T]


def prep_shared(cfg, inp):
    f = lambda a: np.ascontiguousarray(np.asarray(a, dtype=np.float32))
    sh = {}
    sh["w_ada"] = f(inp["w_ada"][0])
    sh["b_ada"] = f(inp["b_ada"][0]).reshape(1, -1)
    sh["w_in"] = f(inp["w_in"][0])
    sh["b_gate"] = f(inp["b_gate"][0]).reshape(1, 16)
    cq = f(inp["conv_qk"][0])
    sh["convw"] = np.ascontiguousarray(cq.T.reshape(4, 128, 5).transpose(1, 0, 2))
    sh["nabias"] = _na_bias_layout(f(inp["na_rel_bias"][0]))
    sh["ml_norm_g"] = f(inp["ml_norm_g"][0]).reshape(1, -1)
    sh["w_out"] = f(inp["w_out"][0])
    sh["ln1_g"] = f(inp["ln1_g"][0]).reshape(1, -1)
    sh["ln1_b"] = f(inp["ln1_b"][0]).reshape(1, -1)
    sh["w_router"] = f(inp["w_router"][0])
    sh["weg"] = f(inp["w_expert_gate"][0])
    sh["weu"] = f(inp["w_expert_up"][0])
    sh["wed"] = f(inp["w_expert_down"][0])
    sh["ln2_g"] = f(inp["ln2_g"][0]).reshape(1, -1)
    sh["ln2_b"] = f(inp["ln2_b"][0]).reshape(1, -1)
    sh.update(_consts(cfg.T))
    return sh


def prep_core(cfg, inp, sh, b):
    f = lambda a: np.ascontiguousarray(np.asarray(a, dtype=np.float32))
    m = dict(sh)
    m["x"] = f(inp["x"][b])
    m["ctx"] = f(inp["ctx"][b])
    cv = np.concatenate([f(inp["c"][b]).reshape(8, 128).T, f(inp["c_ctx"]).reshape(8, 128).T], axis=1)
    m["cvec"] = np.ascontiguousarray(cv)
    return m


_PROG_CACHE = {}


def kernel(**inputs):
    cfg = Cfg()
    if "full" not in _PROG_CACHE:
        _PROG_CACHE["full"] = build_program(cfg)
    nc = _PROG_CACHE["full"]
    sh = prep_shared(cfg, inputs)
    B = 8
    in_maps = [prep_core(cfg, inputs, sh, b) for b in range(B)]
    res = run_bass_kernel_spmd(nc, in_maps, core_ids=list(range(B)))
    return np.stack([np.asarray(r["out"], dtype=np.float32) for r in res.results], axis=0)
```

```python
import os
import numpy as np
import ml_dtypes
import concourse.bass as bass
import concourse.mybir as mybir
from concourse.bass_utils import run_bass_kernel_spmd

F32 = mybir.dt.float32
BF16 = mybir.dt.bfloat16
I32 = mybir.dt.int32
U32 = mybir.dt.uint32
ALU = mybir.AluOpType
ACT = mybir.ActivationFunctionType
AX = mybir.AxisListType


class Cfg:
    def __init__(self, T=8192, DFF=2816, NE=16, debug=()):
        self.T = T
        self.DFF = DFF
        self.NE = NE
        self.D = 1024
        self.GW = 64
        self.LC = 256
        self.TX = T + self.LC
        self.NT = T // 128
        self.NTX = self.TX // 128
        self.ROWS = T // 64
        self.CAP = 2 * T // NE
        self.NSL = self.CAP // 128
        self.NFC = DFF // 128
        self.DIN = 3088
        self.debug = set(debug)


class Buf:
    __slots__ = ("name", "w", "r")

    def __init__(self, name=""):
        self.name = name
        self.w = None
        self.r = []


class Prog:
    ENGS = ("pe", "act", "dve", "pool", "sp")

    def __init__(self, nc, stack, n_dma_sems=20):
        self.nc = nc
        self.lists = {e: [] for e in self.ENGS}
        self.esem = {e: stack.enter_context(nc.semaphore("es_" + e)) for e in self.ENGS}
        self.cnt = {e: 0 for e in self.ENGS}
        self.seen = {e: {} for e in self.ENGS}
        self.dsem = {}
        self.dtot = {}
        self.drr = {}
        for q in ("sp", "pool", "act"):
            self.dsem[q] = [stack.enter_context(nc.semaphore("ds_%s%d" % (q, i))) for i in range(n_dma_sems)]
            self.dtot[q] = [0] * n_dma_sems
            self.drr[q] = 0
        self.nops = 0

    def _need(self, eng, tok, waits):
        if tok is None:
            return
        kind, key, val = tok
        sk = (kind, key)
        if self.seen[eng].get(sk, 0) >= val:
            return
        if waits.get(sk, 0) < val:
            waits[sk] = val

    def _sem(self, sk):
        kind, key = sk
        if kind == "E":
            return self.esem[key]
        return self.dsem[key[0]][key[1]]

    def op(self, eng, fn, rd=(), wr=(), sig=True, dma=False, acc=False, wr_add=False):
        waits = {}
        for b in rd:
            for t in (b.w or ()):
                if t[0] == "E" and t[1] == eng and eng == "pe":
                    continue
                self._need(eng, t, waits)
        for b in wr:
            if not wr_add:
                for t in (b.w or ()):
                    if not (t[0] == "E" and t[1] == eng):
                        self._need(eng, t, waits)
            for t in b.r:
                if not (t[0] == "E" and t[1] == eng and not dma):
                    self._need(eng, t, waits)
        lst = self.lists[eng]
        if dma:
            q = eng
            i = self.drr[q]
            self.drr[q] = (i + 1) % len(self.dsem[q])
            if self.dtot[q][i] > 0:
                self._need(eng, ("D", (q, i), self.dtot[q][i]), waits)
            self.dtot[q][i] += 16
            tok = ("D", (q, i), self.dtot[q][i])
            for sk, v in waits.items():
                lst.append(("w", self._sem(sk), v))
                self.seen[eng][sk] = v
            lst.append(("o", fn, self.dsem[q][i], 16))
        else:
            for sk, v in waits.items():
                lst.append(("w", self._sem(sk), v))
                self.seen[eng][sk] = v
            if True:
                self.cnt[eng] += 1
                tok = ("E", eng, self.cnt[eng])
                lst.append(("o", fn, self.esem[eng], 1))
            else:
                tok = ("E", eng, self.cnt[eng] + 1)
                lst.append(("o", fn, None, 0))
        for b in rd:
            b.r.append(tok)
            if len(b.r) > 24:
                b.r = b.r[-24:] if False else b.r
        for b in wr:
            if wr_add and b.w:
                b.w = b.w + [tok]
            else:
                b.w = [tok]
                b.r = []
        self.nops += 1
        return tok

    def pe(self, meth, rd=(), wr=(), **kw):
        return self.op("pe", (meth, kw), rd, wr)

    def act(self, meth, rd=(), wr=(), **kw):
        return self.op("act", (meth, kw), rd, wr)

    def dve(self, meth, rd=(), wr=(), **kw):
        return self.op("dve", (meth, kw), rd, wr)

    def pool(self, meth, rd=(), wr=(), **kw):
        return self.op("pool", (meth, kw), rd, wr)

    def dma(self, q, out, in_, rd=(), wr=(), wr_add=False, **kw):
        return self.op(q, ("dma_start", dict(out=out, in_=in_, **kw)), rd, wr, dma=True, wr_add=wr_add)

    def barrier(self, bufs=()):
        toks = []
        for e in self.ENGS:
            if self.cnt[e] > 0:
                toks.append(("E", e, self.cnt[e]))
        for q in self.dsem:
            for i, t in enumerate(self.dtot[q]):
                if t > 0:
                    toks.append(("D", (q, i), t))
        for e in self.ENGS:
            for (kind, key, val) in toks:
                sk = (kind, key)
                if kind == "E" and key == e:
                    continue
                if self.seen[e].get(sk, 0) >= val:
                    continue
                self.lists[e].append(("w", self._sem(sk), val))
                self.seen[e][sk] = val
        for b in bufs:
            b.w = None
            b.r = []

    def finish(self):
        nc = self.nc
        self.barrier()
        lists = self.lists

        def replay(engine, items):
            for it in items:
                if it[0] == "w":
                    engine.wait_ge(it[1], it[2])
                else:
                    f = it[1]
                    ins = getattr(engine, f[0])(**f[1]) if isinstance(f, tuple) else f(engine)
                    if it[2] is not None:
                        ins.then_inc(it[2], it[3])

        with nc.Block() as block:
            @block.tensor
            def _(e):
                replay(e, lists["pe"])

            @block.scalar
            def _(e):
                replay(e, lists["act"])

            @block.vector
            def _(e):
                replay(e, lists["dve"])

            @block.gpsimd
            def _(e):
                replay(e, lists["pool"])

            @block.sync
            def _(e):
                replay(e, lists["sp"])


class Arena:
    def __init__(self, nc):
        self.nc = nc
        self.base = (nc.sbuf_base + 63) // 64 * 64
        self.top = nc.sbuf_top
        self.cur = self.base
        self.n = 0

    def alloc(self, shape, dtype, name="t"):
        esz = {F32: 4, BF16: 2, I32: 4, U32: 4}[dtype]
        per = int(np.prod(shape[1:])) * esz
        per = (per + 63) // 64 * 64
        off = self.cur
        assert off + per <= self.top, "SBUF arena overflow: %s %s need %d have %d" % (name, shape, per, self.top - off)
        self.cur += per
        self.n += 1
        return self.nc.alloc_sbuf_tensor_at("%s_%d" % (name, self.n), list(shape), dtype, offset=off)

    def mark(self):
        return self.cur

    def release(self, m):
        self.cur = m


def _na_bias_layout(bias_table):
    H = bias_table.shape[0]
    NEG = np.float32(-30000.0)
    out = np.full((H, 3, 6, 128, 256), NEG, np.float32)
    c = np.arange(64)
    cs = np.clip(c - 8, 0, 48)
    for gt in range(3):
        for ql in range(4):
            for kl in range(12 if gt == 1 else 8):
                if gt == 0:
                    r, kr, rs = ql, kl, 0
                elif gt == 1:
                    r, kr, rs = 4 + ql, kl, ql
                else:
                    r, kr, rs = 4 + ql, kl, 0
                if not (rs <= kr < rs + 8):
                    continue
                dr = kr - r + 7
                for qc in range(64):
                    kcs = np.arange(cs[qc], cs[qc] + 16)
                    dc = kcs - qc + 15
                    tile = kr // 2
                    keyp = (kr % 2) * 64 + kcs
                    out[:, gt, tile, keyp, ql * 64 + qc] = bias_table[:, dr, :][:, dc]
    return out


def _rope_tables(T):
    nf = 16
    inv = (1.0 / (10000.0 ** (np.arange(nf, dtype=np.float32) / nf))).astype(np.float32)
    t = np.arange(T)
    rows = (t // 64).astype(np.float32)
    cols = (t % 64).astype(np.float32)
    cos = np.zeros((128, T), np.float32)
    sin = np.zeros((128, T), np.float32)
    for p in range(128):
        d = p % 64
        pos = rows if d < 32 else cols
        j = d % 16
        ang = (pos * inv[j]).astype(np.float32)
        cos[p] = np.cos(ang)
        sgn = -1.0 if (d % 32) < 16 else 1.0
        sin[p] = sgn * np.sin(ang)
    return cos, sin


def _perm_matrix():
    P = np.zeros((128, 128), np.float32)
    for m in range(128):
        d = m % 64
        base = m - d
        pd = d + 16 if (d % 32) < 16 else d - 16
        P[base + pd, m] = 1.0
    return P


class Ring:
    def __init__(self, items):
        self.items = items
        self.i = 0

    def next(self):
        it = self.items[self.i]
        self.i = (self.i + 1) % len(self.items)
        return it


def sb_ring(ar, n, shape, dtype, name):
    return Ring([(ar.alloc(shape, dtype, name), Buf(name)) for _ in range(n)])


class Ctx:
    pass


def build_program(cfg):
    from contextlib import ExitStack
    nc = bass.Bass("TRN2", target_bir_lowering=False)
    T, TX, NT, NTX, D, DFF, NE = cfg.T, cfg.TX, cfg.NT, cfg.NTX, cfg.D, cfg.DFF, cfg.NE
    c = Ctx()
    c.cfg = cfg
    c.nc = nc

    def din(name, shape, dt=F32):
        return nc.dram_tensor(name, list(shape), dt, kind="ExternalInput").ap()

    def dscr(name, shape, dt=F32):
        kind = "ExternalOutput" if name in cfg.debug else "Internal"
        return nc.dram_tensor(name, list(shape), dt, kind=kind).ap()

    I = Ctx()
    c.I = I
    I.x = din("x", [T, D])
    I.ctx = din("ctx", [cfg.LC, D])
    I.cvec = din("cvec", [128, 16])
    I.w_ada = din("w_ada", [D, 6 * D])
    I.b_ada = din("b_ada", [1, 6 * D])
    I.w_in = din("w_in", [D, cfg.DIN])
    I.b_gate = din("b_gate", [1, 16])
    I.convw = din("convw", [128, 4, 5])
    I.nabias = din("nabias", [8, 3, 6, 128, 256])
    I.ml_norm_g = din("ml_norm_g", [1, 512])
    I.w_out = din("w_out", [D, D])
    I.ln1_g = din("ln1_g", [1, D])
    I.ln1_b = din("ln1_b", [1, D])
    I.w_router = din("w_router", [D, NE])
    I.weg = din("weg", [NE, D, DFF])
    I.weu = din("weu", [NE, D, DFF])
    I.wed = din("wed", [NE, DFF, D])
    I.ln2_g = din("ln2_g", [1, D])
    I.ln2_b = din("ln2_b", [1, D])
    I.ropec = din("ropec", [128, T])
    I.ropes = din("ropes", [128, T])
    I.cmat = din("cmat", [5, 128, 128])
    I.iota = din("iota", [128, 128])
    out = nc.dram_tensor("out", [T, D], F32, kind="ExternalOutput").ap()
    c.out = out

    S = Ctx()
    c.S = S
    S.QA_T = dscr("QA_T", [4, 128, T], BF16)
    S.KA_T = dscr("KA_T", [4, 128, TX], BF16)
    S.VA = dscr("VA", [TX, 512], BF16)
    S.QM_T = dscr("QM_T", [2, 128, T])
    S.KM_T = dscr("KM_T", [2, 128, TX])
    S.VM = dscr("VM", [TX, 512], BF16)
    S.OM = dscr("OM", [T, 512])
    S.GT = dscr("GT", [TX, 16])
    S.MIX_T = dscr("MIX_T", [8, 128, T], BF16)
    S.HF = dscr("HF", [T, 512])
    S.HB = dscr("HB", [T, 512])
    S.ACC = dscr("ACC", [T, D])
    S.H2 = dscr("H2", [T, D], BF16)
    S.AFF = dscr("AFF", [T, NE])

    with ExitStack() as stack:
        k = Prog(nc, stack)
        c.k = k
        ar = Arena(nc)
        c.ar = ar
        c.ps = [(nc.alloc_psum_tensor("ps%d" % i, [128, 512], F32), Buf("ps%d" % i)) for i in range(8)]
        c.cm = ar.alloc([128, 5, 128], F32, "cmat")
        c.cm_b = Buf("cmat")
        k.dma("sp", c.cm[:, :, :], I.cmat.rearrange("a p q -> p a q"), wr=[c.cm_b])
        c.cmb = ar.alloc([128, 5, 128], BF16, "cmatb")
        c.cmb_b = Buf("cmatb")
        k.dve("tensor_copy", [c.cm_b], [c.cmb_b], out=c.cmb[:, :, :], in_=c.cm[:, :, :])
        c.ident, c.perm, c.triF, c.triB, c.ones = [c.cm[:, i, :] for i in range(5)]
        c.identb, c.permb, c.triFb, c.triBb, c.onesb = [c.cmb[:, i, :] for i in range(5)]

        c.modp = ar.alloc([128, 4, 8], F32, "modp")
        c.modp_b = Buf("modp")
        c.G2t = ar.alloc([128, 1024], F32, "G2t")
        c.g2_b = Buf("G2t")
        c.AFFt = ar.alloc([128, cfg.NT, cfg.NE], F32, "AFFt")
        c.aff_b = Buf("AFFt")
        c.IDX = ar.alloc([128, cfg.NE, cfg.NSL], U32, "IDX")
        c.idx_b = Buf("IDX")
        c.mb_mark = ar.mark()
        for nm, fn in (("A", stage_a), ("B", stage_b), ("C", stage_c), ("D", stage_d), ("E", stage_e),
                       ("F", stage_f), ("G", stage_g), ("H", stage_h)):
            fn(c)
            if any(d_.startswith("stop" + nm) for d_ in cfg.debug):
                break
        k.finish()
    return nc


def dump(c, name, ap, shape, buf, dt=F32):
    if name in c.cfg.debug:
        d = c.nc.dram_tensor(name, list(shape), dt, kind="ExternalOutput").ap()
        c.k.dma("sp", d, ap, rd=[buf])


class Evac:
    def __init__(self, k):
        self.k = k
        self.n = 0

    def __call__(self, out_ap, in_ap, rd, wr, scale=None, eng=None):
        k = self.k
        self.n += 1
        use_act = (self.n % 2 == 0) if eng is None else (eng == "act")
        if use_act:
            if scale is None:
                k.act("activation", rd, wr, out=out_ap, in_=in_ap, func=ACT.Copy)
            else:
                k.act("activation", rd, wr, out=out_ap, in_=in_ap, func=ACT.Copy, scale=scale)
        else:
            if scale is None:
                k.dve("tensor_copy", rd, wr, out=out_ap, in_=in_ap)
            else:
                k.dve("tensor_scalar_mul", rd, wr, out=out_ap, in0=in_ap, scalar1=scale)


def stage_a(c):
    k, ar, I, nc = c.k, c.ar, c.I, c.nc
    D = 1024
    c.MB = ar.alloc([128, 6 * D], F32, "MB")
    c.MB_b = Buf("MB")
    m0 = ar.mark()
    MBC = ar.alloc([128, 2 * D], F32, "MBC")
    MBC_b = Buf("MBC")
    cv = ar.alloc([128, 16], F32, "cv")
    cv_b = Buf()
    sv = ar.alloc([128, 16], F32, "sv")
    sv_b = Buf()
    SR = ar.alloc([128, 16, 128], F32, "SR")
    SR_b = Buf()
    bab = ar.alloc([128, 6 * D], F32, "bab")
    bab_b = Buf()
    wring = sb_ring(ar, 3, [128, 2048], F32, "wada")
    k.dma("sp", cv[:, :], I.cvec[:, :], wr=[cv_b])
    k.dma("sp", bab[:, :], I.b_ada.partition_broadcast(128), wr=[bab_b])
    k.act("activation", [cv_b], [sv_b], out=sv[:, :], in_=cv[:, :], func=ACT.Silu)
    k.dve("tensor_copy", [sv_b], [SR_b], out=SR[:, :, :], in_=sv[:, :].unsqueeze(2).to_broadcast([128, 16, 128]))
    for piece in range(3):
        nb = 8 if piece == 0 else 4
        for kk in range(8):
            wt, wb = wring.next()
            k.dma("sp", wt[:, :], I.w_ada[kk * 128:(kk + 1) * 128, piece * 2048:(piece + 1) * 2048], wr=[wb])
            for j in range(nb):
                pt, pb = c.ps[j]
                lhs = SR[:, kk, :] if j < 4 else SR[:, 8 + kk, :]
                jj = j % 4
                k.pe("matmul", [SR_b, wb], [pb], out=pt[:, :], lhsT=lhs, rhs=wt[:, jj * 512:(jj + 1) * 512],
                     start=(kk == 0), stop=(kk == 7))
        for j in range(nb):
            pt, pb = c.ps[j]
            jj = j % 4
            col = piece * 2048 + jj * 512
            if j < 4:
                k.dve("tensor_tensor", [pb, bab_b], [c.MB_b], out=c.MB[:, col:col + 512], in0=pt[:, :],
                      in1=bab[:, col:col + 512], op=ALU.add)
            else:
                k.dve("tensor_tensor", [pb, bab_b], [MBC_b], out=MBC[:, col:col + 512], in0=pt[:, :],
                      in1=bab[:, col:col + 512], op=ALU.add)
    srcs = [(c.MB, c.MB_b, 1024, 0, 1.0), (c.MB, c.MB_b, 0, 1, 0.0), (MBC, MBC_b, 1024, 2, 1.0), (MBC, MBC_b, 0, 3, 0.0)]
    n = 0
    for (src, sb, off, slot, addc) in srcs:
        for half in range(2):
            pt, pb = c.ps[n % 8]
            n += 1
            for q in range(4):
                ch = half * 4 + q
                k.pe("transpose", [sb, c.cm_b], [pb], out=pt[:, q * 128:(q + 1) * 128],
                     in_=src[:, off + ch * 128: off + (ch + 1) * 128], identity=c.ident)
            k.dve("tensor_scalar_add", [pb], [c.modp_b], out=c.modp[:, slot, half * 4:(half + 1) * 4],
                  in0=pt[:, :].rearrange("p (q t) -> p q t", t=128)[:, :, 0], scalar1=addc)
    k.dve("tensor_copy", [c.MB_b], [c.g2_b], out=c.G2t[:, :], in_=c.MB[:, 5120:6144])
    dump(c, "MB", c.MB[:, :], [128, 6144], c.MB_b)
    dump(c, "MODP", c.modp[:, :, :], [128, 4, 8], c.modp_b)
    k.barrier()
    ar.release(m0)


def stage_b(c):
    k, ar, I, S, nc, cfg = c.k, c.ar, c.I, c.S, c.nc, c.cfg
    T, TX, NT, NTX = cfg.T, cfg.TX, cfg.NT, cfg.NTX
    m0 = ar.mark()
    W = ar.alloc([128, 8, cfg.DIN], BF16, "win")
    W_b = Buf("win")
    wst = sb_ring(ar, 2, [128, cfg.DIN], F32, "winst")
    evac = Evac(k)
    for kk in range(8):
        wt, wb = wst.next()
        k.dma("sp", wt[:, :], I.w_in[kk * 128:(kk + 1) * 128, :], wr=[wb])
        evac(W[:, kk, :], wt[:, :], [wb], [W_b])
    xring = sb_ring(ar, 3, [128, 1024], F32, "xin")
    xmring = sb_ring(ar, 2, [128, 8, 512], BF16, "xm")
    st_bf = sb_ring(ar, 4, [128, 512], BF16, "stbf")
    st_f = sb_ring(ar, 4, [128, 512], F32, "stf")
    st_g = sb_ring(ar, 3, [128, 16], F32, "stg")
    ps_tp = Ring([c.ps[0], c.ps[1]])
    ps_tm = Ring([c.ps[2], c.ps[3], c.ps[4]])
    ps_fm = Ring([c.ps[5], c.ps[6], c.ps[7]])
    ngroups = (NTX + 3) // 4
    for g in range(ngroups):
        tiles = list(range(4 * g, min(4 * g + 4, NTX)))
        ntok = 128 * len(tiles)
        xm, xm_b = xmring.next()
        is_ctx = tiles[0] >= NT
        sl_sc, sl_sh = (2, 3) if is_ctx else (0, 1)
        for ti, i in enumerate(tiles):
            xt, xb = xring.next()
            src = I.ctx[(i - NT) * 128:(i - NT + 1) * 128, :] if is_ctx else I.x[i * 128:(i + 1) * 128, :]
            k.dma("sp", xt[:, :], src, wr=[xb])
            for half in range(2):
                pt, pb = ps_tp.next()
                for q in range(4):
                    ch = half * 4 + q
                    k.pe("transpose", [xb, c.cm_b], [pb], out=pt[:, q * 128:(q + 1) * 128],
                         in_=xt[:, ch * 128:(ch + 1) * 128], identity=c.ident)
                for q in range(4):
                    ch = half * 4 + q
                    o_ap = xm[:, ch, ti * 128:(ti + 1) * 128]
                    i_ap = pt[:, q * 128:(q + 1) * 128]
                    s1 = c.modp[:, sl_sc, ch:ch + 1]
                    s2 = c.modp[:, sl_sh, ch:ch + 1]
                    if q % 2 == 0:
                        k.dve("tensor_scalar", [pb, c.modp_b], [xm_b], out=o_ap, in0=i_ap, scalar1=s1, scalar2=s2,
                              op0=ALU.mult, op1=ALU.add)
                    else:
                        k.act("activation", [pb, c.modp_b], [xm_b], out=o_ap, in_=i_ap, func=ACT.Identity,
                              bias=s2, scale=s1)
            tsl = slice(ti * 128, (ti + 1) * 128)
            rows = slice(i * 128, (i + 1) * 128)
            for (c0, ncol, kind) in ((1024, 512, "va"), (2048, 512, "vm"), (2560, 512, "om"), (3072, 16, "gt")):
                if kind == "om" and is_ctx:
                    continue
                pt, pb = ps_tm.next()
                for kk in range(8):
                    k.pe("matmul", [xm_b, W_b], [pb], out=pt[:, 0:ncol], lhsT=xm[:, kk, tsl],
                         rhs=W[:, kk, c0:c0 + ncol], start=(kk == 0), stop=(kk == 7))
                if kind in ("va", "vm"):
                    st, stb = st_bf.next()
                    evac(st[:, :], pt[:, :], [pb], [stb])
                    dst = S.VA if kind == "va" else S.VM
                    k.dma("sp", dst[rows, :], st[:, :], rd=[stb])
                elif kind == "om":
                    st, stb = st_f.next()
                    evac(st[:, :], pt[:, :], [pb], [stb])
                    k.dma("sp", S.OM[rows, :], st[:, :], rd=[stb])
                else:
                    st, stb = st_g.next()
                    evac(st[:, :], pt[:, 0:16], [pb], [stb])
                    k.dma("sp", S.GT[rows, :], st[:, :], rd=[stb])
        tok0 = tiles[0] * 128
        for ci in range(12):
            kind = ("qa", "ka", "qm", "km")[0 if ci < 4 else 1 if ci < 8 else 2 if ci < 10 else 3]
            if is_ctx and kind in ("qa", "qm"):
                continue
            c0 = {"qa": 0, "ka": 512, "qm": 1536, "km": 1792}[kind]
            j = ci if ci < 4 else ci - 4 if ci < 8 else ci - 8 if ci < 10 else ci - 10
            pt, pb = ps_fm.next()
            for kk in range(8):
                k.pe("matmul", [xm_b, W_b], [pb], out=pt[:, 0:ntok], lhsT=W[:, kk, c0 + j * 128:c0 + (j + 1) * 128],
                     rhs=xm[:, kk, 0:ntok], start=(kk == 0), stop=(kk == 7))
            if kind in ("qa", "ka"):
                st, stb = st_bf.next()
                evac(st[:, 0:ntok], pt[:, 0:ntok], [pb], [stb], scale=(0.125 if kind == "qa" else None))
                dst = S.QA_T if kind == "qa" else S.KA_T
                k.dma("sp", dst[j, :, tok0:tok0 + ntok], st[:, 0:ntok], rd=[stb])
            else:
                st, stb = st_f.next()
                evac(st[:, 0:ntok], pt[:, 0:ntok], [pb], [stb])
                dst = S.QM_T if kind == "qm" else S.KM_T
                k.dma("sp", dst[j, :, tok0:tok0 + ntok], st[:, 0:ntok], rd=[stb])
    k.barrier()
    ar.release(m0)


def stage_c(c):
    k, ar, I, S, nc, cfg = c.k, c.ar, c.I, c.S, c.nc, c.cfg
    T, TX, NT, NTX = cfg.T, cfg.TX, cfg.NT, cfg.NTX
    G = cfg.ROWS // 4
    m0 = ar.mark()
    qring = sb_ring(ar, 2, [128, T], BF16, "qT")
    kring = sb_ring(ar, 2, [128, TX], BF16, "kT")
    vring = sb_ring(ar, 2, [128, NTX, 128], BF16, "Vp")
    bias = [(ar.alloc([128, 3, 6, 256], F32, "nab"), Buf("nab")) for _ in range(2)]
    ptring = sb_ring(ar, 3, [128, 8, 256], BF16, "PT")
    tmpring = sb_ring(ar, 3, [128, 256], F32, "stmp")
    rdring = sb_ring(ar, 2, [128, 256], F32, "rden")
    attring = sb_ring(ar, 2, [128, 256], BF16, "attT")
    ps_s = Ring([c.ps[i] for i in range(5)])
    ps_o = Ring([c.ps[5], c.ps[6], c.ps[7]])
    for j in range(4):
        qT, q_b = qring.next()
        kT, k_b = kring.next()
        V, v_b = vring.next()
        k.dma("sp", qT[:, :], S.QA_T[j], wr=[q_b])
        k.dma("sp", kT[:, :], S.KA_T[j], wr=[k_b])
        for n0 in range(0, NTX, 4):
            n1 = min(NTX, n0 + 4)
            k.dma("sp", V[:, n0:n1, :], S.VA[n0 * 128:n1 * 128, j * 128:(j + 1) * 128].rearrange("(n p) c -> p n c", p=128),
                  wr=[v_b], wr_add=(n0 > 0))
        for hh in range(2):
            first = True
            for t_ in range(3):
                for a0 in (0, 3):
                    k.dma("sp", bias[hh][0][:, t_, a0:a0 + 3, :], I.nabias[2 * j + hh, t_, a0:a0 + 3].rearrange("a p q -> p a q"),
                          wr=[bias[hh][1]], wr_add=(not first))
                    first = False
        for g in range(G):
            q0 = g * 256
            if g == 0:
                gt, t0, nl = 0, 0, 4
            elif g == G - 1:
                gt, t0, nl = 2, NT - 4, 4
            else:
                gt, t0, nl = 1, 2 * g - 2, 6
            tiles = [t0 + a for a in range(nl)] + [NT, NT + 1]
            att, att_b = attring.next()
            for hh in range(2):
                pr = slice(hh * 64, hh * 64 + 64)
                bt, bb = bias[hh]
                PT, pt_b = ptring.next()
                for a, tile in enumerate(tiles):
                    pst, psb = ps_s.next()
                    k.pe("matmul", [k_b, q_b], [psb], out=pst[:, 0:256], lhsT=kT[pr, tile * 128:(tile + 1) * 128],
                         rhs=qT[pr, q0:q0 + 256], start=True, stop=True)
                    if a < nl:
                        tmp, tmp_b = tmpring.next()
                        k.dve("tensor_tensor", [psb, bb], [tmp_b], out=tmp[:, :], in0=pst[:, 0:256],
                              in1=bt[:, gt, a, :], op=ALU.add)
                        k.act("activation", [tmp_b], [pt_b], out=PT[:, a, :], in_=tmp[:, :], func=ACT.Exp)
                    else:
                        k.act("activation", [psb], [pt_b], out=PT[:, a, :], in_=pst[:, 0:256], func=ACT.Exp)
                po, pob = ps_o.next()
                na = len(tiles)
                for a, tile in enumerate(tiles):
                    k.pe("matmul", [v_b, pt_b], [pob], out=po[pr, 0:256], lhsT=V[:, tile, pr], rhs=PT[:, a, :],
                         start=(a == 0), stop=(a == na - 1))
                for a, tile in enumerate(tiles):
                    k.pe("matmul", [c.cmb_b, pt_b], [pob], out=po[pr, 256:512], lhsT=c.onesb[:, pr], rhs=PT[:, a, :],
                         start=(a == 0), stop=(a == na - 1))
                rd, rd_b = rdring.next()
                k.dve("reciprocal", [pob], [rd_b], out=rd[pr, :], in_=po[pr, 256:512])
                k.dve("tensor_tensor", [pob, rd_b], [att_b], out=att[pr, :], in0=po[pr, 0:256], in1=rd[pr, :], op=ALU.mult)
            k.dma("sp", S.MIX_T[j, :, q0:q0 + 256], att[:, :], rd=[att_b])
    k.barrier()
    ar.release(m0)


def stage_d(c):
    k, ar, I, S, nc, cfg = c.k, c.ar, c.I, c.S, c.nc, c.cfg
    T, TX, NT, NTX = cfg.T, cfg.TX, cfg.NT, cfg.NTX
    m0 = ar.mark()
    evac = Evac(k)
    GTt = ar.alloc([128, NTX, 16], F32, "gt")
    g_b = Buf("gt")
    bg = ar.alloc([128, 16], F32, "bg")
    bg_b = Buf("bg")
    for n0 in range(0, NTX, 4):
        n1 = min(NTX, n0 + 4)
        k.dma("sp", GTt[:, n0:n1, :], S.GT[n0 * 128:n1 * 128, :].rearrange("(n p) g -> p n g", p=128), wr=[g_b],
              wr_add=(n0 > 0))
    k.dma("sp", bg[:, :], I.b_gate.partition_broadcast(128), wr=[bg_b])
    k.dve("tensor_tensor", [g_b, bg_b], [g_b], out=GTt[:, :, :], in0=GTt[:, :, :],
          in1=bg[:, :].unsqueeze(1).to_broadcast([128, NTX, 16]), op=ALU.add)
    LF = ar.alloc([128, NTX, 8], F32, "LF")
    lf_b = Buf("LF")
    IG = ar.alloc([128, NTX, 8], F32, "IG")
    ig_b = Buf("IG")
    for d in range(2):
        k.act("activation", [g_b], [lf_b], out=LF[:, :, d * 4:(d + 1) * 4], in_=GTt[:, :, 8 * d + 4:8 * d + 8],
              func=ACT.Exp, scale=-1.0)
        k.dve("tensor_copy", [g_b], [ig_b], out=IG[:, :, d * 4:(d + 1) * 4], in_=GTt[:, :, 8 * d:8 * d + 4])
    k.act("activation", [lf_b], [lf_b], out=LF[:, :, :], in_=LF[:, :, :], func=ACT.Ln, bias=1.0)
    k.dve("tensor_scalar_mul", [lf_b], [lf_b], out=LF[:, :, :], in0=LF[:, :, :], scalar1=-1.0)
    BC = ar.alloc([128, NTX, 8], F32, "BC")
    bc_b = Buf("BC")
    eT = ar.alloc([128, NTX, 8], F32, "eT")
    et_b = Buf("eT")
    eS = ar.alloc([128, NTX, 8], F32, "eS")
    es_b = Buf("eS")
    eL = ar.alloc([128, NTX, 8], F32, "eL")
    el_b = Buf("eL")
    NC4 = NTX * 4
    for d in range(2):
        pt, pb = c.ps[d]
        k.pe("matmul", [lf_b, c.cm_b], [pb], out=pt[:, 0:NC4], lhsT=(c.triF if d == 0 else c.triB),
             rhs=LF[:, :, d * 4:(d + 1) * 4], start=True, stop=True)
        k.dve("tensor_copy", [pb], [bc_b], out=BC[:, :, d * 4:(d + 1) * 4],
              in_=pt[:, 0:NC4].rearrange("p (n h) -> p n h", h=4))
    pt, pb = c.ps[2]
    pt2, pb2 = c.ps[3]
    half = NTX * 8 // 2
    LF2 = LF[:, :, :].rearrange("p n h -> p (n h)")
    k.pe("matmul", [lf_b, c.cm_b], [pb], out=pt[:, 0:half], lhsT=c.ones, rhs=LF2[:, 0:half], start=True, stop=True)
    k.pe("matmul", [lf_b, c.cm_b], [pb2], out=pt2[:, 0:half], lhsT=c.ones, rhs=LF2[:, half:2 * half], start=True, stop=True)
    eL2 = eL[:, :, :].rearrange("p n h -> p (n h)")
    k.act("activation", [pb], [el_b], out=eL2[:, 0:half], in_=pt[:, 0:half], func=ACT.Exp)
    k.act("activation", [pb2], [el_b], out=eL2[:, half:2 * half], in_=pt2[:, 0:half], func=ACT.Exp)
    k.act("activation", [bc_b], [et_b], out=eT[:, :, :], in_=BC[:, :, :], func=ACT.Exp)
    k.dve("tensor_tensor", [ig_b, bc_b], [es_b], out=eS[:, :, :], in0=IG[:, :, :], in1=BC[:, :, :], op=ALU.subtract)
    k.act("activation", [es_b], [es_b], out=eS[:, :, :], in_=eS[:, :, :], func=ACT.Exp)
    dump(c, "DBG_BC", BC[:, :, :], [128, NTX, 8], bc_b)
    qT = [(ar.alloc([128, T], BF16, "mqT"), Buf("mqT")) for _ in range(2)]
    kT = [(ar.alloc([128, TX], BF16, "mkT"), Buf("mkT")) for _ in range(2)]
    KTM = ar.alloc([128, NTX, 256], BF16, "ktm")
    ktm_b = Buf("ktm")
    cw = ar.alloc([128, 4, 5], F32, "cw")
    cw_b = Buf("cw")
    k.dma("sp", cw[:, :, :], I.convw[:, :, :], wr=[cw_b])
    m1 = ar.mark()
    BLK = min(int(os.environ.get('KBLK', 1024)), T)
    rawr = sb_ring(ar, 2, [128, BLK + 4], F32, "raw")
    accr = sb_ring(ar, 2, [128, BLK], F32, "acc")
    sr = sb_ring(ar, 2, [128, BLK], F32, "sil")
    cosr = sb_ring(ar, 2, [128, BLK], F32, "cos")
    sinr = sb_ring(ar, 2, [128, BLK], F32, "sin")
    t1r = sb_ring(ar, 2, [128, 512], F32, "t1")
    t2r = sb_ring(ar, 2, [128, 512], F32, "t2")
    ps_r = Ring([c.ps[4], c.ps[5], c.ps[6], c.ps[7]])
    for ch in range(4):
        isq = ch < 2
        jj = ch % 2
        src = S.QM_T[jj] if isq else S.KM_T[jj]
        dstT, dst_b = (qT[jj] if isq else kT[jj])
        scale = 0.125 if isq else 1.0
        segs = [(t0, min(BLK, T - t0), 0, T) for t0 in range(0, T, BLK)]
        if not isq:
            segs.append((T, cfg.LC, T, TX))
        for (t0, n, lo, hi) in segs:
            raw, raw_b = rawr.next()
            a0 = max(lo, t0 - 2)
            a1 = min(hi, t0 + n + 2)
            if a0 > t0 - 2 or a1 < t0 + n + 2:
                k.pool("memset", [], [raw_b], ap=raw[:, 0:n + 4], constant=0.0)
            k.dma("sp", raw[:, a0 - (t0 - 2):a1 - (t0 - 2)], src[:, a0:a1], wr=[raw_b])
            acc, acc_b = accr.next()
            k.dve("tensor_scalar_mul", [raw_b, cw_b], [acc_b], out=acc[:, 0:n], in0=raw[:, 0:n], scalar1=cw[:, ch, 0:1])
            for j in range(1, 5):
                k.dve("scalar_tensor_tensor", [raw_b, cw_b, acc_b], [acc_b], out=acc[:, 0:n], in0=raw[:, j:j + n],
                      scalar=cw[:, ch, j:j + 1], in1=acc[:, 0:n], op0=ALU.mult, op1=ALU.add)
            if lo == T:
                k.act("activation", [acc_b], [dst_b], out=dstT[:, t0:t0 + n], in_=acc[:, 0:n], func=ACT.Silu)
                continue
            s_, s_b = sr.next()
            k.act("activation", [acc_b], [s_b], out=s_[:, 0:n], in_=acc[:, 0:n], func=ACT.Silu)
            cs, cs_b = cosr.next()
            sn, sn_b = sinr.next()
            k.dma("sp", cs[:, 0:n], I.ropec[:, t0:t0 + n], wr=[cs_b])
            k.dma("sp", sn[:, 0:n], I.ropes[:, t0:t0 + n], wr=[sn_b])
            for p0 in range(0, n, 512):
                pn = min(512, n - p0)
                pt, pb = ps_r.next()
                k.pe("matmul", [s_b, c.cm_b], [pb], out=pt[:, 0:pn], lhsT=c.perm, rhs=s_[:, p0:p0 + pn], start=True, stop=True)
                t1, t1_b = t1r.next()
                t2, t2_b = t2r.next()
                k.dve("scalar_tensor_tensor", [s_b, cs_b], [t1_b], out=t1[:, 0:pn], in0=s_[:, p0:p0 + pn], scalar=scale,
                       in1=cs[:, p0:p0 + pn], op0=ALU.mult, op1=ALU.mult)
                k.dve("scalar_tensor_tensor", [pb, sn_b], [t2_b], out=t2[:, 0:pn], in0=pt[:, 0:pn], scalar=scale,
                      in1=sn[:, p0:p0 + pn], op0=ALU.mult, op1=ALU.mult)
                k.dve("tensor_tensor", [t1_b, t2_b], [dst_b], out=dstT[:, t0 + p0:t0 + p0 + pn], in0=t1[:, 0:pn],
                      in1=t2[:, 0:pn], op=ALU.add)
    dump(c, "DBG_QT", qT[0][0][:, :], [128, T], qT[0][1], BF16)
    dump(c, "DBG_KT", kT[1][0][:, :], [128, TX], kT[1][1], BF16)
    for jj in range(2):
        kt, kt_b = kT[jj]
        for n0 in range(0, NTX, 4):
            nn = min(4, NTX - n0)
            pt, pb = ps_r.next()
            ptb = pt[:, :].bitcast(BF16)
            for a in range(nn):
                k.pe("transpose", [kt_b, c.cmb_b], [pb], out=ptb[:, a * 128:(a + 1) * 128],
                     in_=kt[:, (n0 + a) * 128:(n0 + a + 1) * 128], identity=c.identb)
            evac(KTM[:, n0:n0 + nn, jj * 128:(jj + 1) * 128], ptb[:, 0:nn * 128].rearrange("p (a x) -> p a x", x=128),
                 [pb], [ktm_b])
    k.barrier()
    ar.release(m1)
    C32 = ar.alloc([128, 4, 129], F32, "C32")
    Cbf = ar.alloc([128, 4, 129], BF16, "Cbf")
    cbuf = {}
    for d in range(2):
        for h in range(4):
            cbuf[(d, h)] = (Buf("c32"), Buf("cbf"))
    k.pool("memset", [], [cbuf[(d, h)][0] for d in range(2) for h in range(4)], ap=C32[:, :, :], constant=0.0)
    k.pool("memset", [], [cbuf[(d, h)][1] for d in range(2) for h in range(4)], ap=Cbf[:, :, :], constant=0.0)
    vmr = sb_ring(ar, 6, [128, 4, 129], BF16, "vma")
    for (vt, vb) in vmr.items:
        k.pool("memset", [], [vb], ap=vt[:, :, :], constant=1.0)
    vpr = sb_ring(ar, 4, [128, 4, 129], BF16, "vp4")
    ptmr = sb_ring(ar, 4, [128, 128], BF16, "ptm")
    hsr = sb_ring(ar, 4, [128, 129], F32, "hs")
    ddr = sb_ring(ar, 4, [128, 1], F32, "dd")
    hor = sb_ring(ar, 4, [128, 512], F32, "hout")
    tmpr = sb_ring(ar, 4, [128, 129], F32, "ctmp")
    ps_s = Ring([c.ps[0], c.ps[1], c.ps[2]])
    ps_o = Ring([c.ps[3], c.ps[4], c.ps[5]])
    ps_u = Ring([c.ps[6], c.ps[7]])
    seq = {0: [NT, NT + 1] + list(range(NT)), 1: [NT + 1, NT] + list(range(NT - 1, -1, -1))}
    for step in range(NT + 2):
        for d in range(2):
            n = seq[d][step]
            latent = n < NT
            tok = slice(n * 128, (n + 1) * 128)
            vt, vb = vmr.next()
            k.dma("sp", vt[:, :, 0:128], S.VM[n * 128:(n + 1) * 128, :].rearrange("p (h v) -> p h v", v=128), wr=[vb])
            vp, vp_b = vpr.next()
            k.dve("tensor_tensor", [vb, es_b], [vp_b], out=vp[:, :, :], in0=vt[:, :, :],
                  in1=eS[:, n, d * 4:(d + 1) * 4].unsqueeze(2).to_broadcast([128, 4, 129]), op=ALU.mult)
            if latent:
                ho, ho_b = hor.next()
            for h in range(4):
                jj, hh = h // 2, h % 2
                pr = slice(hh * 64, hh * 64 + 64)
                slot = d * 2 + jj
                c32_b, cbf_b = cbuf[(d, h)]
                col = d * 4 + h
                if latent:
                    q_, q_b = qT[jj]
                    kt, kt_b = kT[jj]
                    pst, psb = ps_s.next()
                    k.pe("matmul", [kt_b, q_b], [psb], out=pst[:, 0:128], lhsT=kt[pr, tok], rhs=q_[pr, tok], start=True, stop=True)
                    ptm, ptm_b = ptmr.next()
                    k.dve("tensor_tensor", [psb, c.cm_b], [ptm_b], out=ptm[:, :], in0=pst[:, 0:128],
                          in1=(c.triF if d == 0 else c.triB), op=ALU.mult)
                    po, pob = ps_o.next()
                    k.pe("matmul", [ptm_b, vp_b], [pob], out=po[:, 0:129], lhsT=ptm[:, :], rhs=vp[:, h, :], start=True, stop=False)
                    k.pe("matmul", [q_b, cbf_b], [pob], out=po[:, 0:129], lhsT=q_[pr, tok], rhs=Cbf[pr, slot, :], start=False, stop=True)
                    hs, hs_b = hsr.next()
                    k.act("activation", [pob, et_b], [hs_b], out=hs[:, :], in_=po[:, 0:129], func=ACT.Copy, scale=eT[:, n, col:col + 1])
                    dd, dd_b = ddr.next()
                    k.dve("tensor_scalar", [hs_b], [dd_b], out=dd[:, :], in0=hs[:, 128:129], scalar1=-1.0, scalar2=1.0,
                          op0=ALU.mult, op1=ALU.max)
                    k.dve("tensor_tensor", [hs_b, dd_b], [dd_b], out=dd[:, :], in0=dd[:, :], in1=hs[:, 128:129], op=ALU.max)
                    k.dve("reciprocal", [dd_b], [dd_b], out=dd[:, :], in_=dd[:, :])
                    k.dve("tensor_scalar_mul", [hs_b, dd_b], [ho_b], out=ho[:, h * 128:(h + 1) * 128], in0=hs[:, 0:128],
                          scalar1=dd[:, 0:1])
                pu, pub = ps_u.next()
                k.pe("matmul", [ktm_b, vp_b], [pub], out=pu[pr, 0:129], lhsT=KTM[:, n, h * 64:(h + 1) * 64], rhs=vp[:, h, :],
                     start=True, stop=True)
                tmp, tmp_b = tmpr.next()
                k.dve("tensor_tensor", [pub, c32_b], [tmp_b], out=tmp[pr, :], in0=pu[pr, 0:129], in1=C32[pr, slot, :], op=ALU.add)
                k.dve("tensor_scalar_mul", [tmp_b, el_b], [c32_b], out=C32[pr, slot, :], in0=tmp[pr, :], scalar1=eL[pr, n, col:col + 1])
                k.act("activation", [tmp_b, el_b], [cbf_b], out=Cbf[pr, slot, :], in_=tmp[pr, :], func=ACT.Copy,
                      scale=eL[pr, n, col:col + 1])
            if latent:
                dst = S.HF if d == 0 else S.HB
                k.dma("sp", dst[n * 128:(n + 1) * 128, :], ho[:, :], rd=[ho_b])
    k.barrier()
    ar.release(m0)


LN_EPS = 1e-5
ALPHA = 2.0 ** 0.25


def bcast_load(c, ar, src_row, n, name):
    t = ar.alloc([128, n], F32, name)
    b = Buf(name)
    c.k.dma("sp", t[:, :], src_row.partition_broadcast(128), wr=[b])
    return t, b


def stage_e(c):
    k, ar, I, S, nc, cfg = c.k, c.ar, c.I, c.S, c.nc, c.cfg
    T, NT, NE = cfg.T, cfg.NT, cfg.NE
    m0 = ar.mark()
    evac = Evac(k)
    WO = ar.alloc([128, 8, 1024], BF16, "wo")
    wo_b = Buf("wo")
    wst = sb_ring(ar, 2, [128, 1024], F32, "wost")
    for kk in range(8):
        wt, wb = wst.next()
        k.dma("sp", wt[:, :], I.w_out[kk * 128:(kk + 1) * 128, :], wr=[wb])
        evac(WO[:, kk, :], wt[:, :], [wb], [wo_b])
    WR = ar.alloc([128, 8, NE], F32, "wr")
    wr_b = Buf("wr")
    for a0 in (0, 4):
        k.dma("sp", WR[:, a0:a0 + 4, :], I.w_router[a0 * 128:(a0 + 4) * 128, :].rearrange("(a p) e -> p a e", p=128),
              wr=[wr_b], wr_add=(a0 > 0))
    g1t, g1_b = bcast_load(c, ar, I.ln1_g, 1024, "ln1g")
    b1t, b1_b = bcast_load(c, ar, I.ln1_b, 1024, "ln1b")
    mgt, mg_b = bcast_load(c, ar, I.ml_norm_g, 512, "mlg")
    P1 = ar.alloc([128, 1024], F32, "p1sc2")
    p1_b = Buf("p1")
    k.dve("tensor_scalar_add", [c.MB_b], [p1_b], out=P1[:, :], in0=c.MB[:, 4096:5120], scalar1=1.0)
    G1 = c.MB[:, 2048:3072]
    SH2 = c.MB[:, 3072:4096]
    hfr = sb_ring(ar, 2, [128, 512], F32, "hf")
    hbr = sb_ring(ar, 2, [128, 512], F32, "hb")
    omr = sb_ring(ar, 2, [128, 512], F32, "om")
    xr = sb_ring(ar, 2, [128, 1024], F32, "xe")
    mixr = sb_ring(ar, 2, [128, 8, 128], BF16, "mixT")
    hr = sb_ring(ar, 2, [128, 512], F32, "h")
    sqr = sb_ring(ar, 2, [128, 512], F32, "sq")
    sgr = sb_ring(ar, 2, [128, 512], F32, "sg")
    mlr = sb_ring(ar, 2, [128, 512], BF16, "mlb")
    st4 = sb_ring(ar, 4, [128, 4], F32, "st4")
    st1 = sb_ring(ar, 8, [128, 1], F32, "st1")
    yr = sb_ring(ar, 2, [128, 1024], F32, "y")
    scr = sb_ring(ar, 2, [128, 1024], F32, "scr")
    accr = sb_ring(ar, 2, [128, 1024], F32, "acc")
    h2r = sb_ring(ar, 2, [128, 1024], F32, "h2")
    h2br = sb_ring(ar, 2, [128, 1024], BF16, "h2b")
    h2tr = sb_ring(ar, 2, [128, 8, 128], F32, "h2T")
    lgr = sb_ring(ar, 2, [128, NE], F32, "lg")
    ps_t = Ring([c.ps[0], c.ps[1]])
    ps_m = Ring([c.ps[2], c.ps[3], c.ps[4], c.ps[5]])
    ps_r = Ring([c.ps[6], c.ps[7]])
    for i in range(NT):
        rows = slice(i * 128, (i + 1) * 128)
        hf, hf_b = hfr.next()
        hb, hb_b = hbr.next()
        om, om_b = omr.next()
        xt, x_b = xr.next()
        mixT, mx_b = mixr.next()
        k.dma("sp", hf[:, :], S.HF[rows, :], wr=[hf_b])
        k.dma("sp", hb[:, :], S.HB[rows, :], wr=[hb_b])
        k.dma("sp", om[:, :], S.OM[rows, :], wr=[om_b])
        k.dma("sp", xt[:, :], I.x[rows, :], wr=[x_b])
        k.dma("sp", mixT[:, 0:4, :], S.MIX_T[0:4, :, rows].rearrange("j p t -> p j t"), wr=[mx_b])
        h, h_b = hr.next()
        k.pool("tensor_tensor", [hf_b, hb_b], [h_b], out=h[:, :], in0=hf[:, :], in1=hb[:, :], op=ALU.add)
        h3 = h[:, :].rearrange("p (a v) -> p a v", v=128)
        mu, mu_b = st4.next()
        k.dve("tensor_reduce", [h_b], [mu_b], out=mu[:, :], in_=h3, axis=AX.X, op=ALU.add)
        k.dve("tensor_scalar_mul", [mu_b], [mu_b], out=mu[:, :], in0=mu[:, :], scalar1=1.0 / 128)
        k.dve("tensor_tensor", [h_b, mu_b], [h_b], out=h3, in0=h3, in1=mu[:, :].unsqueeze(2).to_broadcast([128, 4, 128]),
              op=ALU.subtract)
        sq, sq_b = sqr.next()
        k.pool("tensor_tensor", [h_b], [sq_b], out=sq[:, :], in0=h[:, :], in1=h[:, :], op=ALU.mult)
        var, var_b = st4.next()
        k.dve("tensor_reduce", [sq_b], [var_b], out=var[:, :], in_=sq[:, :].rearrange("p (a v) -> p a v", v=128), axis=AX.X,
              op=ALU.add)
        k.dve("tensor_scalar", [var_b], [var_b], out=var[:, :], in0=var[:, :], scalar1=1.0 / 128, scalar2=LN_EPS,
              op0=ALU.mult, op1=ALU.add)
        k.act("activation", [var_b], [var_b], out=var[:, :], in_=var[:, :], func=ACT.Sqrt)
        k.dve("reciprocal", [var_b], [var_b], out=var[:, :], in_=var[:, :])
        k.dve("tensor_tensor", [h_b, var_b], [h_b], out=h3, in0=h3, in1=var[:, :].unsqueeze(2).to_broadcast([128, 4, 128]),
              op=ALU.mult)
        sg, sg_b = sgr.next()
        k.act("activation", [om_b], [sg_b], out=sg[:, :], in_=om[:, :], func=ACT.Sigmoid)
        k.pool("tensor_tensor", [h_b, mg_b], [h_b], out=h[:, :], in0=h[:, :], in1=mgt[:, :], op=ALU.mult)
        mlb, ml_b = mlr.next()
        k.dve("tensor_tensor", [h_b, sg_b], [ml_b], out=mlb[:, :], in0=h[:, :], in1=sg[:, :], op=ALU.mult)
        pt, pb = ps_t.next()
        ptb = pt[:, :].bitcast(BF16)
        for a in range(4):
            k.pe("transpose", [ml_b, c.cmb_b], [pb], out=ptb[:, a * 128:(a + 1) * 128], in_=mlb[:, a * 128:(a + 1) * 128],
                 identity=c.identb)
        evac(mixT[:, 4:8, :], ptb[:, 0:512].rearrange("p (a x) -> p a x", x=128), [pb], [mx_b])
        y, y_b = yr.next()
        for half in range(2):
            pm, pmb = ps_m.next()
            for kk in range(8):
                k.pe("matmul", [mx_b, wo_b], [pmb], out=pm[:, :], lhsT=mixT[:, kk, :], rhs=WO[:, kk, half * 512:(half + 1) * 512],
                     start=(kk == 0), stop=(kk == 7))
            k.dve("tensor_tensor", [pmb, c.MB_b], [y_b], out=y[:, half * 512:(half + 1) * 512], in0=pm[:, :],
                  in1=G1[:, half * 512:(half + 1) * 512], op=ALU.mult)
        k.dve("scalar_tensor_tensor", [x_b, y_b], [y_b], out=y[:, :], in0=xt[:, :], scalar=ALPHA, in1=y[:, :], op0=ALU.mult,
              op1=ALU.add)
        x1, x1_b = layer_norm(c, y, y_b, g1t, g1_b, b1t, b1_b, st1, scr)
        acc, acc_b = accr.next()
        k.act("activation", [x1_b], [acc_b], out=acc[:, :], in_=x1[:, :], func=ACT.Copy, scale=ALPHA)
        k.dma("sp", S.ACC[rows, :], acc[:, :], rd=[acc_b])
        h2, h2_b = h2r.next()
        k.pool("tensor_tensor", [x1_b, p1_b], [h2_b], out=h2[:, :], in0=x1[:, :], in1=P1[:, :], op=ALU.mult)
        k.pool("tensor_tensor", [h2_b, c.MB_b], [h2_b], out=h2[:, :], in0=h2[:, :], in1=SH2, op=ALU.add)
        h2b, h2b_b = h2br.next()
        k.act("activation", [h2_b], [h2b_b], out=h2b[:, :], in_=h2[:, :], func=ACT.Copy)
        k.dma("sp", S.H2[rows, :], h2b[:, :], rd=[h2b_b])
        h2T, h2T_b = h2tr.next()
        for half in range(2):
            pt, pb = ps_t.next()
            for q in range(4):
                ch = half * 4 + q
                k.pe("transpose", [h2_b, c.cm_b], [pb], out=pt[:, q * 128:(q + 1) * 128], in_=h2[:, ch * 128:(ch + 1) * 128],
                     identity=c.ident)
            evac(h2T[:, half * 4:(half + 1) * 4, :], pt[:, :].rearrange("p (a x) -> p a x", x=128), [pb], [h2T_b])
        pr_, prb = ps_r.next()
        for kk in range(8):
            k.pe("matmul", [h2T_b, wr_b], [prb], out=pr_[:, 0:NE], lhsT=h2T[:, kk, :], rhs=WR[:, kk, :], start=(kk == 0),
                 stop=(kk == 7))
        mxv, mxv_b = st1.next()
        k.dve("tensor_reduce", [prb], [mxv_b], out=mxv[:, :], in_=pr_[:, 0:NE], axis=AX.X, op=ALU.max)
        k.dve("tensor_scalar_mul", [mxv_b], [mxv_b], out=mxv[:, :], in0=mxv[:, :], scalar1=-1.0)
        lg, lg_b = lgr.next()
        ssum, ssum_b = st1.next()
        k.act("activation", [prb, mxv_b], [lg_b, ssum_b], out=lg[:, :], in_=pr_[:, 0:NE], func=ACT.Exp, bias=mxv[:, 0:1],
              accum_out=ssum[:, 0:1])
        k.dve("reciprocal", [ssum_b], [ssum_b], out=ssum[:, :], in_=ssum[:, :])
        k.dve("tensor_scalar_mul", [lg_b, ssum_b], [c.aff_b], out=c.AFFt[:, i, :], in0=lg[:, :], scalar1=ssum[:, 0:1])
        k.dma("sp", S.AFF[rows, :], c.AFFt[:, i, :], rd=[c.aff_b])
    k.barrier()
    ar.release(m0)


def layer_norm(c, y, y_b, gt, g_b, bt, b_b, st1, scr):
    k = c.k
    mu, mu_b = st1.next()
    k.dve("tensor_reduce", [y_b], [mu_b], out=mu[:, :], in_=y[:, :], axis=AX.X, op=ALU.add)
    k.dve("tensor_scalar_mul", [mu_b], [mu_b], out=mu[:, :], in0=mu[:, :], scalar1=-1.0 / 1024)
    k.dve("tensor_scalar_add", [y_b, mu_b], [y_b], out=y[:, :], in0=y[:, :], scalar1=mu[:, 0:1])
    sc, sc_b = scr.next()
    ss, ss_b = st1.next()
    k.act("activation", [y_b], [sc_b, ss_b], out=sc[:, :], in_=y[:, :], func=ACT.Square, accum_out=ss[:, 0:1])
    k.dve("tensor_scalar", [ss_b], [ss_b], out=ss[:, :], in0=ss[:, :], scalar1=1.0 / 1024, scalar2=LN_EPS, op0=ALU.mult,
          op1=ALU.add)
    k.act("activation", [ss_b], [ss_b], out=ss[:, :], in_=ss[:, :], func=ACT.Sqrt)
    k.dve("reciprocal", [ss_b], [ss_b], out=ss[:, :], in_=ss[:, :])
    k.dve("scalar_tensor_tensor", [y_b, ss_b, g_b], [y_b], out=y[:, :], in0=y[:, :], scalar=ss[:, 0:1], in1=gt[:, :],
          op0=ALU.mult, op1=ALU.mult)
    k.dve("tensor_tensor", [y_b, b_b], [y_b], out=y[:, :], in0=y[:, :], in1=bt[:, :], op=ALU.add)
    return y, y_b


def stage_f(c):
    k, ar, I, S, nc, cfg = c.k, c.ar, c.I, c.S, c.nc, c.cfg
    T, NT, NE, NSL, CAP = cfg.T, cfg.NT, cfg.NE, cfg.NSL, cfg.CAP
    ar.release(c.mb_mark)
    m0 = ar.mark()
    NN = NT * NE
    A = c.AFFt
    a_b = c.aff_b
    io = ar.alloc([128, 128], F32, "iota")
    io_b = Buf("iota")
    k.dma("sp", io[:, :], I.iota[:, :], wr=[io_b])
    lo = ar.alloc([128, NE], F32, "lo")
    hi = ar.alloc([128, NE], F32, "hi")
    mid = ar.alloc([128, NE], F32, "mid")
    dl = ar.alloc([128, NE], F32, "dl")
    sel = ar.alloc([128, NE], F32, "sel")
    pc = ar.alloc([128, NE], BF16, "pc")
    M = ar.alloc([128, NT, NE], F32, "M")
    lo_b, hi_b, mid_b, dl_b, sel_b, pc_b, M_b = [Buf(n) for n in ("lo", "hi", "mid", "dl", "sel", "pc", "M")]
    k.dve("memset", [], [lo_b], ap=lo[:, :], constant=0.0)
    k.dve("memset", [], [hi_b], ap=hi[:, :], constant=1.0)
    psr = Ring([c.ps[0], c.ps[1]])
    for it in range(30):
        k.dve("tensor_tensor", [lo_b, hi_b], [mid_b], out=mid[:, :], in0=lo[:, :], in1=hi[:, :], op=ALU.add)
        k.dve("tensor_scalar_mul", [mid_b], [mid_b], out=mid[:, :], in0=mid[:, :], scalar1=0.5)
        k.dve("tensor_tensor", [a_b, mid_b], [M_b], out=M[:, :, :], in0=A[:, :, :],
              in1=mid[:, :].unsqueeze(1).to_broadcast([128, NT, NE]), op=ALU.is_gt)
        k.dve("tensor_reduce", [M_b], [dl_b], out=dl[:, :], in_=M[:, :, :].rearrange("p n e -> p e n"), axis=AX.X, op=ALU.add)
        k.dve("tensor_copy", [dl_b], [pc_b], out=pc[:, :], in_=dl[:, :])
        pt, pb = psr.next()
        k.pe("matmul", [pc_b, c.cmb_b], [pb], out=pt[:, 0:NE], lhsT=c.onesb, rhs=pc[:, :], start=True, stop=True)
        k.dve("tensor_single_scalar", [pb], [sel_b], out=sel[:, :], in_=pt[:, 0:NE], scalar=float(CAP), op=ALU.is_ge)
        k.dve("tensor_tensor", [mid_b, lo_b], [dl_b], out=dl[:, :], in0=mid[:, :], in1=lo[:, :], op=ALU.subtract)
        k.dve("tensor_tensor", [dl_b, sel_b], [dl_b], out=dl[:, :], in0=dl[:, :], in1=sel[:, :], op=ALU.mult)
        k.dve("tensor_tensor", [lo_b, dl_b], [lo_b], out=lo[:, :], in0=lo[:, :], in1=dl[:, :], op=ALU.add)
        k.dve("tensor_tensor", [hi_b, mid_b], [dl_b], out=dl[:, :], in0=hi[:, :], in1=mid[:, :], op=ALU.subtract)
        k.dve("tensor_tensor", [dl_b, sel_b], [dl_b], out=dl[:, :], in0=dl[:, :], in1=sel[:, :], op=ALU.mult)
        k.dve("tensor_tensor", [mid_b, dl_b], [hi_b], out=hi[:, :], in0=mid[:, :], in1=dl[:, :], op=ALU.add)
    if "stopF1" in cfg.debug:
        dump(c, "DBG_LO", lo[:, :], [128, NE], lo_b)
        k.barrier()
        ar.release(m0)
        return
    Mb = ar.alloc([128, NN], BF16, "Mb")
    Mb_b = Buf("Mb")
    k.dve("tensor_tensor", [a_b, lo_b], [Mb_b], out=Mb[:, :].rearrange("p (n e) -> p n e", e=NE), in0=A[:, :, :],
          in1=lo[:, :].unsqueeze(1).to_broadcast([128, NT, NE]), op=ALU.is_gt)
    INCL = ar.alloc([128, NN], F32, "INCL")
    incl_b = Buf("INCL")
    TA = ar.alloc([128, NN], F32, "TA")
    ta_b = Buf("TA")
    TB = ar.alloc([128, NN], F32, "TB")
    tb_b = Buf("TB")
    TOT = ar.alloc([128, NN], F32, "TOT")
    tot_b = Buf("TOT")
    for p0 in range(0, NN, 512):
        w = min(512, NN - p0)
        pt, pb = psr.next()
        k.pe("matmul", [Mb_b, c.cmb_b], [pb], out=pt[:, 0:w], lhsT=c.triFb, rhs=Mb[:, p0:p0 + w], start=True, stop=True)
        k.dve("tensor_copy", [pb], [incl_b], out=INCL[:, p0:p0 + w], in_=pt[:, 0:w])
        pt, pb = psr.next()
        k.pe("matmul", [Mb_b, c.cmb_b], [pb], out=pt[:, 0:w], lhsT=c.onesb, rhs=Mb[:, p0:p0 + w], start=True, stop=True)
        k.dve("tensor_copy", [pb], [tot_b], out=TOT[:, p0:p0 + w], in_=pt[:, 0:w])
        k.dve("tensor_copy", [tot_b], [ta_b], out=TA[:, p0:p0 + w], in_=TOT[:, p0:p0 + w])
    cur, cur_b, oth, oth_b = TA, ta_b, TB, tb_b
    s = 1
    while s < NT:
        sw = s * NE
        k.dve("tensor_copy", [cur_b], [oth_b], out=oth[:, 0:sw], in_=cur[:, 0:sw])
        k.dve("tensor_tensor", [cur_b], [oth_b], out=oth[:, sw:NN], in0=cur[:, sw:NN], in1=cur[:, 0:NN - sw], op=ALU.add)
        cur, cur_b, oth, oth_b = oth, oth_b, cur, cur_b
        s *= 2
    k.dve("tensor_tensor", [cur_b, tot_b], [cur_b], out=cur[:, :], in0=cur[:, :], in1=TOT[:, :], op=ALU.subtract)
    k.dve("tensor_tensor", [incl_b, cur_b], [incl_b], out=INCL[:, :], in0=INCL[:, :], in1=cur[:, :], op=ALU.add)
    if "stopF2" in cfg.debug:
        dump(c, "DBG_INCL", INCL[:, :], [128, NN], incl_b)
        k.barrier()
        ar.release(m0)
        return
    K1 = NSL + 1
    K128 = ar.alloc([128, K1], F32, "K128")
    k128_b = Buf("K128")
    k.dve("tensor_scalar_mul", [io_b], [k128_b], out=K128[:, :], in0=io[:, 0:K1], scalar1=128.0)
    Ge = ar.alloc([128, NN, K1], F32, "Ge")
    ge_b = Buf("Ge")
    k.dve("tensor_tensor", [incl_b, k128_b], [ge_b], out=Ge[:, :, :], in0=INCL[:, :].unsqueeze(2).to_broadcast([128, NN, K1]),
          in1=K128[:, :].unsqueeze(1).to_broadcast([128, NN, K1]), op=ALU.is_ge)
    Bv = ar.alloc([128, NN], F32, "Bv")
    bv_b = Buf("Bv")
    k.dve("tensor_reduce", [ge_b], [bv_b], out=Bv[:, :], in_=Ge[:, :, 1:K1], axis=AX.X, op=ALU.add)
    k.dve("scalar_tensor_tensor", [bv_b, incl_b], [bv_b], out=Bv[:, :], in0=Bv[:, :], scalar=-128.0, in1=INCL[:, :],
          op0=ALU.mult, op1=ALU.add)
    Am = ar.alloc([128, NN, NSL], BF16, "Am")
    am_b = Buf("Am")
    Al = ar.alloc([128, NN, NSL], BF16, "Al")
    al_b = Buf("Al")
    k.dve("tensor_tensor", [ge_b], [am_b], out=Am[:, :, :], in0=Ge[:, :, 0:NSL], in1=Ge[:, :, 1:K1], op=ALU.subtract)
    k.dve("tensor_scalar", [ge_b], [al_b], out=Al[:, :, :], in0=Ge[:, :, 0:NSL], scalar1=-1.0, scalar2=1.0, op0=ALU.mult,
          op1=ALU.add)
    dump(c, "DBG_INCL", INCL[:, :], [128, NN], incl_b)
    if "stopF3" in cfg.debug:
        dump(c, "DBG_BV", Bv[:, :], [128, NN], bv_b)
        k.barrier()
        ar.release(m0)
        return
    thr = sb_ring(ar, 4, [128, 128], BF16, "Th")
    ps4 = Ring([c.ps[2], c.ps[3], c.ps[4], c.ps[5]])
    for e in range(NE):
        pt, pb = ps4.next()
        for n in range(NT):
            th, th_b = thr.next()
            col = n * NE + e
            eng = k.dve
            eng("tensor_scalar", [io_b, bv_b], [th_b], out=th[:, :], in0=io[:, :], scalar1=Bv[:, col:col + 1], scalar2=None,
                op0=ALU.is_ge)
            k.pe("matmul", [th_b, am_b], [pb], out=pt[:, 0:NSL], lhsT=th[:, :], rhs=Am[:, col, :], start=(n == 0), stop=False)
            k.pe("matmul", [c.cmb_b, al_b], [pb], out=pt[:, 0:NSL], lhsT=c.onesb, rhs=Al[:, col, :], start=False,
                 stop=(n == NT - 1))
        k.dve("tensor_scalar_min", [pb], [c.idx_b], out=c.IDX[:, e, :], in0=pt[:, 0:NSL], scalar1=float(T - 1))
    dump(c, "DBG_IDX", c.IDX[:, :, :], [128, NE, NSL], c.idx_b, U32)
    k.barrier()
    ar.release(m0)


def stage_g(c):
    k, ar, I, S, nc, cfg = c.k, c.ar, c.I, c.S, c.nc, c.cfg
    T, NT, NE, NSL, CAP, NFC, DFF = cfg.T, cfg.NT, cfg.NE, cfg.NSL, cfg.CAP, cfg.NFC, cfg.DFF
    m0 = ar.mark()
    evac = Evac(k)
    FG = 2
    NG = NFC // FG
    HW_ = min(int(os.environ.get('KHW', 512)), CAP)
    NH = CAP // HW_
    xer = sb_ring(ar, 1, [128, NSL, 1024], BF16, "XE")
    xtr = sb_ring(ar, 1, [128, 8, CAP], BF16, "XT")
    g16r = sb_ring(ar, 2, [128, NSL, NE], F32, "g16")
    wgs = sb_ring(ar, 2, [128, 8, FG * 128], F32, "wgs")
    wus = sb_ring(ar, 2, [128, 8, FG * 128], F32, "wus")
    wds = sb_ring(ar, 2, [128, FG, 1024], F32, "wds")
    wgb = sb_ring(ar, 2, [128, 8, FG * 128], BF16, "wgb")
    wub = sb_ring(ar, 2, [128, 8, FG * 128], BF16, "wub")
    WD = ar.alloc([128, NFC, 1024], BF16, "WD")
    wd_b = Buf("WD")
    HT = ar.alloc([128, NFC, CAP], BF16, "HT")
    ht_b = Buf("HT")
    sgr = sb_ring(ar, 2, [128, HW_], F32, "sg")
    ysr = sb_ring(ar, 1, [128, 1024], F32, "ys")
    acc_b = Buf("ACCdram")
    ps_t = Ring([c.ps[0], c.ps[1]])
    ps_g = Ring([c.ps[2], c.ps[3]])
    ps_u = Ring([c.ps[4], c.ps[5]])
    ps_d = Ring([c.ps[6], c.ps[7]])
    cast_n = [0]

    def cast(out_ap, in_ap, rd, wr):
        cast_n[0] += 1
        m = cast_n[0] % 3
        if m == 0:
            k.act("activation", rd, wr, out=out_ap, in_=in_ap, func=ACT.Copy)
        elif m == 1:
            k.dve("tensor_copy", rd, wr, out=out_ap, in_=in_ap)
        else:
            k.pool("tensor_copy", rd, wr, out=out_ap, in_=in_ap)

    XE, xe_b = xer.next()
    XT, xt_b = xtr.next()
    g16s = {}

    def gather(e):
        g16, g16_b = g16r.next()
        g16s[e] = (g16, g16_b)
        for kq in range(NSL):
            off = bass.IndirectOffsetOnAxis(ap=c.IDX[:, e, kq:kq + 1], axis=0)
            k.op("pool", ("indirect_dma_start", dict(out=XE[:, kq, :], out_offset=None, in_=S.H2[:, :], in_offset=off)),
                 [c.idx_b], [xe_b], dma=True, wr_add=(kq > 0))
            off2 = bass.IndirectOffsetOnAxis(ap=c.IDX[:, e, kq:kq + 1], axis=0)
            k.op("pool", ("indirect_dma_start", dict(out=g16[:, kq, :], out_offset=None, in_=S.AFF[:, :], in_offset=off2)),
                 [c.idx_b], [g16_b], dma=True, wr_add=(kq > 0))

    def transposes():
        for kq in range(NSL):
            for half in range(2):
                pt, pb = ps_t.next()
                ptb = pt[:, :].bitcast(BF16)
                for q in range(4):
                    ch = half * 4 + q
                    k.pe("transpose", [xe_b, c.cmb_b], [pb], out=ptb[:, q * 128:(q + 1) * 128],
                         in_=XE[:, kq, ch * 128:(ch + 1) * 128], identity=c.identb)
                evac(XT[:, half * 4:(half + 1) * 4, kq * 128:(kq + 1) * 128],
                     ptb[:, 0:512].rearrange("p (a x) -> p a x", x=128), [pb], [xt_b])

    gather(0)
    transposes()
    for e in range(NE):
        g16, g16_b = g16s[e]
        if e + 1 < NE:
            gather(e + 1)
        for g in range(NG):
            f0 = g * FG * 128
            ws, ws_b = wgs.next()
            for a0 in (0, 4):
                k.dma("sp", ws[:, a0:a0 + 4, :], I.weg[e, a0 * 128:(a0 + 4) * 128, f0:f0 + FG * 128].rearrange(
                    "(a p) f -> p a f", p=128), wr=[ws_b], wr_add=(a0 > 0))
            wg, wg_b = wgb.next()
            cast(wg[:, :, :], ws[:, :, :], [ws_b], [wg_b])
            ws2, ws2_b = wus.next()
            for a0 in (0, 4):
                k.dma("sp", ws2[:, a0:a0 + 4, :], I.weu[e, a0 * 128:(a0 + 4) * 128, f0:f0 + FG * 128].rearrange(
                    "(a p) f -> p a f", p=128), wr=[ws2_b], wr_add=(a0 > 0))
            wu, wu_b = wub.next()
            cast(wu[:, :, :], ws2[:, :, :], [ws2_b], [wu_b])
            ws3, ws3_b = wds.next()
            k.dma("sp", ws3[:, :, :], I.wed[e, f0:f0 + FG * 128, :].rearrange("(a p) d -> p a d", p=128), wr=[ws3_b])
            cast(WD[:, g * FG:(g + 1) * FG, :], ws3[:, :, :], [ws3_b], [wd_b])
            for fl in range(FG):
                fc = g * FG + fl
                for hf in range(NH):
                    sl = slice(hf * HW_, (hf + 1) * HW_)
                    pg, pgb = ps_g.next()
                    pu, pub = ps_u.next()
                    for kk in range(8):
                        k.pe("matmul", [wg_b, xt_b], [pgb], out=pg[:, 0:HW_], lhsT=wg[:, kk, fl * 128:(fl + 1) * 128],
                             rhs=XT[:, kk, sl], start=(kk == 0), stop=(kk == 7))
                    for kk in range(8):
                        k.pe("matmul", [wu_b, xt_b], [pub], out=pu[:, 0:HW_], lhsT=wu[:, kk, fl * 128:(fl + 1) * 128],
                             rhs=XT[:, kk, sl], start=(kk == 0), stop=(kk == 7))
                    sg, sg_b = sgr.next()
                    k.act("activation", [pgb], [sg_b], out=sg[:, :], in_=pg[:, 0:HW_], func=ACT.Silu)
                    k.dve("tensor_tensor", [sg_b, pub], [ht_b], out=HT[:, fc, sl], in0=sg[:, :], in1=pu[:, 0:HW_], op=ALU.mult)
        for kq in range(NSL):
            ys, ys_b = ysr.next()
            for dh in range(2):
                pd, pdb = ps_d.next()
                for fc in range(NFC):
                    k.pe("matmul", [ht_b, wd_b], [pdb], out=pd[:, :], lhsT=HT[:, fc, kq * 128:(kq + 1) * 128],
                         rhs=WD[:, fc, dh * 512:(dh + 1) * 512], start=(fc == 0), stop=(fc == NFC - 1))
                k.dve("scalar_tensor_tensor", [pdb, g16_b, c.g2_b], [ys_b], out=ys[:, dh * 512:(dh + 1) * 512], in0=pd[:, :],
                      scalar=g16[:, kq, e:e + 1], in1=c.G2t[:, dh * 512:(dh + 1) * 512], op0=ALU.mult, op1=ALU.mult)
            off = bass.IndirectOffsetOnAxis(ap=c.IDX[:, e, kq:kq + 1], axis=0)
            k.op("pool", ("indirect_dma_start", dict(out=S.ACC[:, :], out_offset=off, in_=ys[:, :], in_offset=None,
                                                     compute_op=ALU.add)),
                 [ys_b, c.idx_b], [acc_b], dma=True)
        if e + 1 < NE:
            transposes()
    k.barrier()
    ar.release(m0)


def stage_h(c):
    k, ar, I, S, nc, cfg = c.k, c.ar, c.I, c.S, c.nc, c.cfg
    NT = cfg.NT
    m0 = ar.mark()
    g2t, g2_b = bcast_load(c, ar, I.ln2_g, 1024, "ln2g")
    b2t, b2_b = bcast_load(c, ar, I.ln2_b, 1024, "ln2b")
    yr = sb_ring(ar, 3, [128, 1024], F32, "yh")
    scr = sb_ring(ar, 2, [128, 1024], F32, "scrh")
    st1 = sb_ring(ar, 8, [128, 1], F32, "st1h")
    for i in range(NT):
        rows = slice(i * 128, (i + 1) * 128)
        y, y_b = yr.next()
        k.dma("sp", y[:, :], S.ACC[rows, :], wr=[y_b])
        layer_norm(c, y, y_b, g2t, g2_b, b2t, b2_b, st1, scr)
        k.dma("sp", c.out[rows, :], y[:, :], rd=[y_b])
    k.barrier()
    ar.release(m0)


_CONST_CACHE = {}


def _consts(T):
    if T in _CONST_CACHE:
        return _CONST_CACHE[T]
    cos, sin = _rope_tables(T)
    ident = np.eye(128, dtype=np.float32)
    perm = _perm_matrix()
    s = np.arange(128)[:, None]
    t = np.arange(128)[None, :]
    triF = (s <= t).astype(np.float32)
    triB = (s >= t).astype(np.float32)
    ones = np.ones((128, 128), np.float32)
    cmat = np.stack([ident, perm, triF, triB, ones]).astype(np.float32)
    iota = np.tile(np.arange(128, dtype=np.float32)[None, :], (128, 1))
    _CONST_CACHE[T] = dict(ropec=cos, ropes=sin, cmat=cmat, iota=iota)
    return _CONST_CACHE[T]


def prep_shared(cfg, inp):
    f = lambda a: np.ascontiguousarray(np.asarray(a, dtype=np.float32))
    sh = {}
    sh["w_ada"] = f(inp["w_ada"][0])
    sh["b_ada"] = f(inp["b_ada"][0]).reshape(1, -1)
    sh["w_in"] = f(inp["w_in"][0])
    sh["b_gate"] = f(inp["b_gate"][0]).reshape(1, 16)
    cq = f(inp["conv_qk"][0])
    sh["convw"] = np.ascontiguousarray(cq.T.reshape(4, 128, 5).transpose(1, 0, 2))
    sh["nabias"] = _na_bias_layout(f(inp["na_rel_bias"][0]))
    sh["ml_norm_g"] = f(inp["ml_norm_g"][0]).reshape(1, -1)
    sh["w_out"] = f(inp["w_out"][0])
    sh["ln1_g"] = f(inp["ln1_g"][0]).reshape(1, -1)
    sh["ln1_b"] = f(inp["ln1_b"][0]).reshape(1, -1)
    sh["w_router"] = f(inp["w_router"][0])
    sh["weg"] = f(inp["w_expert_gate"][0])
    sh["weu"] = f(inp["w_expert_up"][0])
    sh["wed"] = f(inp["w_expert_down"][0])
    sh["ln2_g"] = f(inp["ln2_g"][0]).reshape(1, -1)
    sh["ln2_b"] = f(inp["ln2_b"][0]).reshape(1, -1)
    sh.update(_consts(cfg.T))
    return sh


def prep_core(cfg, inp, sh, b):
    f = lambda a: np.ascontiguousarray(np.asarray(a, dtype=np.float32))
    m = dict(sh)
    m["x"] = f(inp["x"][b])
    m["ctx"] = f(inp["ctx"][b])
    cv = np.concatenate([f(inp["c"][b]).reshape(8, 128).T, f(inp["c_ctx"]).reshape(8, 128).T], axis=1)
    m["cvec"] = np.ascontiguousarray(cv)
    return m


_PROG_CACHE = {}


def kernel(**inputs):
    cfg = Cfg()
    if "full" not in _PROG_CACHE:
        _PROG_CACHE["full"] = build_program(cfg)
    nc = _PROG_CACHE["full"]
    sh = prep_shared(cfg, inputs)
    B = 8
    in_maps = [prep_core(cfg, inputs, sh, b) for b in range(B)]
    res = run_bass_kernel_spmd(nc, in_maps, core_ids=list(range(B)))
    return np.stack([np.asarray(r["out"], dtype=np.float32) for r in res.results], axis=0)
```
